# Optimizing a Trainium2 kernel written in Bass

```python
import math
import jax
import jax.numpy as jnp
from jax import lax
import numpy as np

D_MODEL = 2048
BATCH = 8
SEQ = 2048
DEPTH = 2

GRID_W = 64
CTX_LEN = 256

D_SSM = D_MODEL
SSM_HEAD_DIM = 64
SSM_HEADS = D_SSM // SSM_HEAD_DIM
SSM_GROUPS = 4
SSM_STATE = 128
CONV_WIDTH = 5
CONV_DIM = D_SSM + 2 * SSM_GROUPS * SSM_STATE
SSD_CHUNK = 128

D_ATT = D_MODEL
ATT_HEAD_DIM = 64
ATT_HEADS = D_ATT // (2 * ATT_HEAD_DIM)
Q_BLOCK = 128
ROPE_BASE = 10000.0

IN_COLS = D_SSM + CONV_DIM + 2 * SSM_HEADS + 3 * D_ATT + 2 * D_MODEL

MOE_GROUPS = 4
EXPERTS_PER_GROUP = 8
N_EXPERTS = MOE_GROUPS * EXPERTS_PER_GROUP
MOE_TOP_K = 2
EXPERT_FF = D_MODEL // 2
MOE_BLOCK = 256

ALPHA = (2.0 * DEPTH) ** 0.25
BETA = (8.0 * DEPTH) ** -0.25
LN_EPS = 1e-6
RMS_EPS = 1e-5

kernel_name = "hybrid_ssd_diffattn_hmoe_dit"

F32 = jnp.float32


def layer_norm(t, g=None, b=None):
    tf = t.astype(F32)
    mu = jnp.mean(tf, axis=-1, keepdims=True)
    var = jnp.mean(jnp.square(tf - mu), axis=-1, keepdims=True)
    y = (tf - mu) * lax.rsqrt(var + LN_EPS)
    if g is not None:
        y = y * g.astype(F32) + b.astype(F32)
    return y.astype(t.dtype)


def modulate(t, shift, scale):
    return t * (1.0 + scale) + shift


def grid_positions(seq_len):
    rows = seq_len // GRID_W
    row = jnp.repeat(jnp.arange(rows, dtype=F32), GRID_W)
    col = jnp.tile(jnp.arange(GRID_W, dtype=F32), rows)
    return row, col


def split_proj(p):
    sizes = (D_SSM, CONV_DIM, 2 * SSM_HEADS, D_ATT, D_ATT, D_ATT, 2 * D_MODEL)
    idx = [int(i) for i in np.cumsum(sizes)[:-1]]
    return jnp.split(p, idx, axis=-1)


def dwconv_centred(t, w, b):
    pad = (CONV_WIDTH - 1) // 2
    out = lax.conv_general_dilated(
        t, w[:, None, :].astype(t.dtype), window_strides=(1,),
        padding=((pad, pad),), dimension_numbers=('NWC', 'WIO', 'NWC'),
        feature_group_count=t.shape[-1])
    return out + b.astype(t.dtype)


def ssd_inputs(xbc, dt_raw, conv_w, conv_b, dt_bias, a_log):
    xbc = jax.nn.silu(dwconv_centred(xbc, conv_w, conv_b))
    xs, bm, cm = jnp.split(xbc, [D_SSM, D_SSM + SSM_GROUPS * SSM_STATE], axis=-1)
    b_, s_ = xs.shape[:2]
    xs = xs.reshape(b_, s_, SSM_HEADS, SSM_HEAD_DIM)
    bm = bm.reshape(b_, s_, SSM_GROUPS, SSM_STATE)
    cm = cm.reshape(b_, s_, SSM_GROUPS, SSM_STATE)
    dt = jax.nn.softplus((dt_raw.reshape(b_, s_, 2, SSM_HEADS) + dt_bias).astype(F32))
    a = -jnp.exp(a_log.astype(F32))
    return xs, bm, cm, dt, a


def segsum(a):
    t = a.shape[-1]
    cs = jnp.cumsum(a, axis=-1)
    diff = cs[..., :, None] - cs[..., None, :]
    return jnp.where(jnp.tril(jnp.ones((t, t), bool)), diff, -jnp.inf)


def ssd_chunked(xdt, adt, bm, cm, h0, with_y):
    b_, s_ = xdt.shape[:2]
    nc = s_ // SSD_CHUNK
    hg = SSM_HEADS // SSM_GROUPS
    xc = xdt.astype(F32).reshape(b_, nc, SSD_CHUNK, SSM_GROUPS, hg, SSM_HEAD_DIM)
    bc = bm.astype(F32).reshape(b_, nc, SSD_CHUNK, SSM_GROUPS, SSM_STATE)
    cc = cm.astype(F32).reshape(b_, nc, SSD_CHUNK, SSM_GROUPS, SSM_STATE)
    a = adt.astype(F32).reshape(b_, nc, SSD_CHUNK, SSM_GROUPS, hg).transpose(0, 3, 4, 1, 2)
    a_cs = jnp.cumsum(a, axis=-1)
    decay_to_end = jnp.exp(a_cs[..., -1:] - a_cs)
    chunk_states = jnp.einsum('bclgn,bghcl,bclghp->bcghpn', bc, decay_to_end, xc)
    h0 = h0.astype(F32).reshape(b_, SSM_GROUPS, hg, SSM_HEAD_DIM, SSM_STATE)
    chunk_states = jnp.concatenate([h0[:, None], chunk_states], axis=1)
    a_tot = jnp.pad(a_cs[..., -1], ((0, 0), (0, 0), (0, 0), (1, 0)))
    decay_chunk = jnp.exp(segsum(a_tot))
    states = jnp.einsum('bghzc,bcghpn->bzghpn', decay_chunk, chunk_states)
    final = states[:, -1].reshape(b_, SSM_HEADS, SSM_HEAD_DIM, SSM_STATE)
    if not with_y:
        return None, final
    seg = jnp.exp(segsum(a))
    cb = jnp.einsum('bclgn,bcsgn->bcgls', cc, bc)
    y_diag = jnp.einsum('bcgls,bghcls,bcsghp->bclghp', cb, seg, xc)
    y_off = jnp.einsum('bclgn,bcghpn,bghcl->bclghp', cc, states[:, :-1], jnp.exp(a_cs))
    return (y_diag + y_off).reshape(b_, s_, SSM_HEADS, SSM_HEAD_DIM), final


def maybe_flip(t, rev):
    return jnp.flip(t, axis=1) if rev else t


def bidir_ssd(lat, cin, d_skip, with_ctx):
    xs, bm, cm, dt, a = lat
    cxs, cbm, ccm, cdt, _ = cin
    b_ = xs.shape[0]
    dsk = d_skip.astype(F32)[:, None]
    y = dsk * xs.astype(F32)
    y_ctx = dsk * cxs.astype(F32) if with_ctx else None
    for direction in range(2):
        rev = direction == 1
        h0 = jnp.zeros((b_, SSM_HEADS, SSM_HEAD_DIM, SSM_STATE), F32)
        yc, hc = ssd_chunked(maybe_flip(cxs * cdt[:, :, direction, :, None], rev),
                             maybe_flip(cdt[:, :, direction] * a[direction], rev),
                             maybe_flip(cbm, rev), maybe_flip(ccm, rev), h0, with_ctx)
        yl, _ = ssd_chunked(maybe_flip(xs * dt[:, :, direction, :, None], rev),
                            maybe_flip(dt[:, :, direction] * a[direction], rev),
                            maybe_flip(bm, rev), maybe_flip(cm, rev), hc, True)
        y = y + maybe_flip(yl, rev)
        if with_ctx:
            y_ctx = y_ctx + maybe_flip(yc, rev)
    return y, y_ctx


def gated_rmsnorm(y, z, g):
    b_, s_ = y.shape[:2]
    yz = y.reshape(b_, s_, SSM_GROUPS, -1) * jax.nn.silu(z.astype(F32)).reshape(b_, s_, SSM_GROUPS, -1)
    yz = yz * lax.rsqrt(jnp.mean(jnp.square(yz), axis=-1, keepdims=True) + RMS_EPS)
    return (yz.reshape(b_, s_, D_SSM) * g.astype(F32)).astype(z.dtype)


def axial_rope(t, row, col):
    d = t.shape[-1]
    half = d // 2
    nf = half // 2
    inv = ROPE_BASE ** (-(jnp.arange(nf, dtype=F32) / nf))

    def rot(u, pos):
        ang = pos[:, None] * inv[None, :]
        cos = jnp.cos(ang)[:, None, None, :]
        sin = jnp.sin(ang)[:, None, None, :]
        u1, u2 = u[..., :nf], u[..., nf:]
        return jnp.concatenate([u1 * cos - u2 * sin, u1 * sin + u2 * cos], axis=-1)

    out = jnp.concatenate([rot(t[..., :half], row), rot(t[..., half:], col)], axis=-1)
    return out.astype(t.dtype)


def diff_combine(scores, v, lam):
    p = jax.nn.softmax(scores.astype(F32) * (ATT_HEAD_DIM ** -0.5), axis=-1)
    pd = p[:, :, 0] - lam * p[:, :, 1]
    return jnp.einsum('bhqk,bkhe->bqhe', pd.astype(v.dtype), v)


def head_rmsnorm(o, g):
    of = o.astype(F32)
    return of * lax.rsqrt(jnp.mean(jnp.square(of), axis=-1, keepdims=True) + RMS_EPS) * g.astype(F32)


def diff_attention(q, k, v, cq, ck, cv, lam, subln_g, lam_init, row, col, with_ctx):
    b_, s_ = q.shape[:2]
    s_ctx = cq.shape[1]

    def qk_heads(t):
        return t.reshape(t.shape[:2] + (ATT_HEADS, 2, ATT_HEAD_DIM))

    def v_heads(t):
        return t.reshape(t.shape[:2] + (ATT_HEADS, 2 * ATT_HEAD_DIM))

    q, k, cq, ck = qk_heads(q), qk_heads(k), qk_heads(cq), qk_heads(ck)
    v, cv = v_heads(v), v_heads(cv)
    q_rot, k_rot = axial_rope(q, row, col), axial_rope(k, row, col)
    v_all = jnp.concatenate([cv, v], axis=1)
    n_blk = s_ // Q_BLOCK

    def to_blocks(t):
        return jnp.moveaxis(t.reshape((b_, n_blk, Q_BLOCK) + t.shape[2:]), 1, 0)

    def query_block(args):
        qr, qn = args
        s_lat = jnp.einsum('bqhmd,bkhmd->bhmqk', qr, k_rot)
        s_cx = jnp.einsum('bqhmd,bkhmd->bhmqk', qn, ck)
        return diff_combine(jnp.concatenate([s_cx, s_lat], axis=-1), v_all, lam)

    o = lax.map(query_block, (to_blocks(q_rot), to_blocks(q)))
    o = jnp.moveaxis(o, 0, 1).reshape(b_, s_, ATT_HEADS, 2 * ATT_HEAD_DIM)
    y = (head_rmsnorm(o, subln_g) * (1.0 - lam_init)).reshape(b_, s_, D_ATT).astype(v.dtype)
    y_ctx = None
    if with_ctx:
        o_c = diff_combine(jnp.einsum('bqhmd,bkhmd->bhmqk', cq, ck), cv, lam)
        y_ctx = (head_rmsnorm(o_c, subln_g) * (1.0 - lam_init)).reshape(b_, s_ctx, D_ATT).astype(v.dtype)
    return y, y_ctx


def gated_merge(y_ssm, y_att, gates, w_br_ssm, w_br_att, w_out):
    g_ssm, g_att = jnp.split(gates, 2, axis=-1)
    m = jax.nn.sigmoid(g_ssm) * (y_ssm @ w_br_ssm) + jax.nn.sigmoid(g_att) * (y_att @ w_br_att)
    return m @ w_out


def token_mixer(u, u_ctx, row, col, w_in, conv_w, conv_b, dt_bias, a_log, d_skip, norm_g,
                lq1, lk1, lq2, lk2, subln_g, w_br_ssm, w_br_att, w_out, lam_init, with_ctx):
    z, xbc, dt_raw, q, k, v, gates = split_proj(u @ w_in)
    cz, cxbc, cdt, cq, ck, cv, cgates = split_proj(u_ctx @ w_in)
    lat = ssd_inputs(xbc, dt_raw, conv_w, conv_b, dt_bias, a_log)
    cin = ssd_inputs(cxbc, cdt, conv_w, conv_b, dt_bias, a_log)
    y_s, y_s_ctx = bidir_ssd(lat, cin, d_skip, with_ctx)
    y_s = gated_rmsnorm(y_s, z, norm_g)
    lam = (jnp.exp(jnp.sum(lq1.astype(F32) * lk1.astype(F32)))
           - jnp.exp(jnp.sum(lq2.astype(F32) * lk2.astype(F32))) + lam_init)
    y_a, y_a_ctx = diff_attention(q, k, v, cq, ck, cv, lam, subln_g, lam_init, row, col, with_ctx)
    o = gated_merge(y_s, y_a, gates, w_br_ssm, w_br_att, w_out)
    o_ctx = None
    if with_ctx:
        y_s_ctx = gated_rmsnorm(y_s_ctx, cz, norm_g)
        o_ctx = gated_merge(y_s_ctx, y_a_ctx, cgates, w_br_ssm, w_br_att, w_out)
    return o, o_ctx


def hier_moe(t, w_rg, b_rg, w_re, b_re, w_gate, w_up, w_down):
    n_tok = t.shape[0]
    pg = jax.nn.softmax((t @ w_rg + b_rg).astype(F32), axis=-1)
    p_grp, grp = lax.top_k(pg, 1)
    le = (t @ w_re + b_re).astype(F32).reshape(n_tok, MOE_GROUPS, EXPERTS_PER_GROUP)
    le = jnp.take_along_axis(le, grp[:, :, None], axis=1)[:, 0]
    p_exp, e_loc = lax.top_k(jax.nn.softmax(le, axis=-1), MOE_TOP_K)
    wts = p_grp * p_exp / jnp.sum(p_exp, axis=-1, keepdims=True)
    expert = grp * EXPERTS_PER_GROUP + e_loc
    n_assign = n_tok * MOE_TOP_K
    e_flat = expert.reshape(-1)
    tok_flat = jnp.repeat(jnp.arange(n_tok, dtype=jnp.int32), MOE_TOP_K)
    w_flat = wts.reshape(-1)
    order = jnp.argsort(e_flat)
    e_s, tok_s, w_s = e_flat[order], tok_flat[order], w_flat[order]
    counts = jnp.zeros((N_EXPERTS,), jnp.int32).at[e_flat].add(1)
    starts = jnp.cumsum(counts) - counts
    padded = (counts + MOE_BLOCK - 1) // MOE_BLOCK * MOE_BLOCK
    pstarts = jnp.cumsum(padded) - padded
    dest = pstarts[e_s] + jnp.arange(n_assign, dtype=jnp.int32) - starts[e_s]
    n_blocks = -(-n_assign // MOE_BLOCK) + N_EXPERTS
    slot_tok = jnp.zeros((n_blocks * MOE_BLOCK,), jnp.int32).at[dest].set(tok_s)
    slot_w = jnp.zeros((n_blocks * MOE_BLOCK,), F32).at[dest].set(w_s)
    block_start = jnp.arange(n_blocks, dtype=jnp.int32) * MOE_BLOCK
    block_exp = jnp.minimum(
        jnp.sum(block_start[:, None] >= (pstarts + padded)[None, :], axis=1), N_EXPERTS - 1)

    def expert_block(args):
        idx, e = args
        xb = t[idx]
        hdn = jax.nn.silu(xb @ w_gate[e]) * (xb @ w_up[e])
        return hdn @ w_down[e]

    y_slots = lax.map(expert_block, (slot_tok.reshape(n_blocks, MOE_BLOCK), block_exp))
    y_slots = y_slots.reshape(-1, t.shape[-1]) * slot_w[:, None].astype(t.dtype)
    return jnp.zeros_like(t).at[slot_tok].add(y_slots)


def setup_inputs(seed: int = 0) -> dict:
    key = jax.random.key(seed)
    ks = jax.random.split(key, 32)
    L = DEPTH

    def nrm(k, shape, scale):
        return jax.random.normal(k, shape, F32) * scale

    dt0 = jnp.exp(jax.random.uniform(ks[8], (L, 2, SSM_HEADS), F32, math.log(1e-3), math.log(1e-1)))
    return {
        "x": nrm(ks[0], (BATCH, SEQ, D_MODEL), 1.0),
        "c": nrm(ks[1], (BATCH, D_MODEL), 1.0),
        "ctx": nrm(ks[2], (BATCH, CTX_LEN, D_MODEL), 1.0),
        "c_ctx": nrm(ks[3], (D_MODEL,), 1.0),
        "w_ada": nrm(ks[4], (L, D_MODEL, 6 * D_MODEL), D_MODEL ** -0.5),
        "b_ada": nrm(ks[5], (L, 6 * D_MODEL), 0.01),
        "w_in": nrm(ks[6], (L, D_MODEL, IN_COLS), D_MODEL ** -0.5),
        "conv_w": nrm(ks[7], (L, CONV_WIDTH, CONV_DIM), CONV_WIDTH ** -0.5),
        "conv_b": nrm(ks[9], (L, CONV_DIM), 0.01),
        "ssm_dt_bias": dt0 + jnp.log(-jnp.expm1(-dt0)),
        "ssm_a_log": jnp.log(jax.random.uniform(ks[10], (L, 2, SSM_HEADS), F32, 1.0, 16.0)),
        "ssm_d": 1.0 + nrm(ks[11], (L, SSM_HEADS), 0.1),
        "ssm_norm_g": 1.0 + nrm(ks[12], (L, D_SSM), 0.1),
        "lam_q1": nrm(ks[13], (L, ATT_HEAD_DIM), 0.1),
        "lam_k1": nrm(ks[14], (L, ATT_HEAD_DIM), 0.1),
        "lam_q2": nrm(ks[15], (L, ATT_HEAD_DIM), 0.1),
        "lam_k2": nrm(ks[16], (L, ATT_HEAD_DIM), 0.1),
        "attn_subln_g": 1.0 + nrm(ks[17], (L, 2 * ATT_HEAD_DIM), 0.1),
        "w_br_ssm": nrm(ks[18], (L, D_SSM, D_MODEL), D_SSM ** -0.5),
        "w_br_att": nrm(ks[19], (L, D_ATT, D_MODEL), D_ATT ** -0.5),
        "w_out": nrm(ks[20], (L, D_MODEL, D_MODEL), BETA * D_MODEL ** -0.5),
        "ln1_g": 1.0 + nrm(ks[21], (L, D_MODEL), 0.1),
        "ln1_b": nrm(ks[22], (L, D_MODEL), 0.01),
        "w_router_group": nrm(ks[23], (L, D_MODEL, MOE_GROUPS), D_MODEL ** -0.5),
        "b_router_group": nrm(ks[24], (L, MOE_GROUPS), 0.01),
        "w_router_expert": nrm(ks[25], (L, D_MODEL, N_EXPERTS), D_MODEL ** -0.5),
        "b_router_expert": nrm(ks[26], (L, N_EXPERTS), 0.01),
        "w_exp_gate": nrm(ks[27], (L, N_EXPERTS, D_MODEL, EXPERT_FF), D_MODEL ** -0.5),
        "w_exp_up": nrm(ks[28], (L, N_EXPERTS, D_MODEL, EXPERT_FF), D_MODEL ** -0.5),
        "w_exp_down": nrm(ks[29], (L, N_EXPERTS, EXPERT_FF, D_MODEL), BETA * EXPERT_FF ** -0.5),
        "ln2_g": 1.0 + nrm(ks[30], (L, D_MODEL), 0.1),
        "ln2_b": nrm(ks[31], (L, D_MODEL), 0.01),
    }


def reference(x, c, ctx, c_ctx, w_ada, b_ada, w_in, conv_w, conv_b, ssm_dt_bias, ssm_a_log,
              ssm_d, ssm_norm_g, lam_q1, lam_k1, lam_q2, lam_k2, attn_subln_g, w_br_ssm,
              w_br_att, w_out, ln1_g, ln1_b, w_router_group, b_router_group, w_router_expert,
              b_router_expert, w_exp_gate, w_exp_up, w_exp_down, ln2_g, ln2_b):
    row, col = grid_positions(x.shape[1])
    h, h_ctx = x, ctx
    sc = jax.nn.silu(c)
    sc_ctx = jax.nn.silu(c_ctx)
    for l in range(DEPTH):
        last = l == DEPTH - 1
        lam_init = 0.8 - 0.6 * math.exp(-0.3 * l)
        mod = (sc @ w_ada[l] + b_ada[l])[:, None, :]
        mod_ctx = sc_ctx @ w_ada[l] + b_ada[l]
        sh1, s1, g1, sh2, s2, g2 = jnp.split(mod, 6, axis=-1)
        csh1, cs1, cg1, csh2, cs2, cg2 = jnp.split(mod_ctx, 6, axis=-1)

        u = modulate(layer_norm(h), sh1, s1)
        u_ctx = modulate(layer_norm(h_ctx), csh1, cs1)
        o, o_ctx = token_mixer(u, u_ctx, row, col, w_in[l], conv_w[l], conv_b[l], ssm_dt_bias[l],
                               ssm_a_log[l], ssm_d[l], ssm_norm_g[l], lam_q1[l], lam_k1[l],
                               lam_q2[l], lam_k2[l], attn_subln_g[l], w_br_ssm[l], w_br_att[l],
                               w_out[l], lam_init, not last)
        h = layer_norm(ALPHA * h + g1 * o, ln1_g[l], ln1_b[l])

        u = modulate(layer_norm(h), sh2, s2)
        moe_w = (w_router_group[l], b_router_group[l], w_router_expert[l], b_router_expert[l],
                 w_exp_gate[l], w_exp_up[l], w_exp_down[l])
        if last:
            y = hier_moe(u.reshape(-1, D_MODEL), *moe_w).reshape(h.shape)
        else:
            h_ctx = layer_norm(ALPHA * h_ctx + cg1 * o_ctx, ln1_g[l], ln1_b[l])
            u_ctx = modulate(layer_norm(h_ctx), csh2, cs2)
            n_ctx = u_ctx.shape[0] * u_ctx.shape[1]
            y_all = hier_moe(jnp.concatenate([u_ctx.reshape(-1, D_MODEL), u.reshape(-1, D_MODEL)], axis=0), *moe_w)
            y_ctx = y_all[:n_ctx].reshape(h_ctx.shape)
            y = y_all[n_ctx:].reshape(h.shape)
            h_ctx = layer_norm(ALPHA * h_ctx + cg2 * y_ctx, ln2_g[l], ln2_b[l])
        h = layer_norm(ALPHA * h + g2 * y, ln2_g[l], ln2_b[l])
    return h
```

```python
import numpy as np
import concourse.bass as bass
import concourse.mybir as mybir
from concourse.bass_utils import run_bass_kernel_spmd

F32 = mybir.dt.float32
BF16 = mybir.dt.bfloat16
I32 = mybir.dt.int32
AF = mybir.ActivationFunctionType
ALU = mybir.AluOpType
AX = mybir.AxisListType

NDS = 8


class Buf:
    __slots__ = ("name", "w", "r")

    def __init__(self, name=""):
        self.name = name
        self.w = None
        self.r = []


class V:
    __slots__ = ("ap", "bufs")

    def __init__(self, ap, bufs):
        self.ap = ap
        self.bufs = bufs


class Tl:
    def __init__(self, t, name=""):
        self.t = t
        self.buf = Buf(name)

    def __getitem__(self, idx):
        return V(self.t[idx], [self.buf])

    def v(self, ap):
        return V(ap, [self.buf])


class Sched:
    def __init__(self, nc):
        self.nc = nc
        self.prog = {e: [] for e in ("pe", "dve", "act", "pool", "sp")}
        self.sem = {}
        self.cnt = {}
        for e in ("pe", "dve", "act", "pool"):
            self.sem[e] = nc.alloc_semaphore("s_" + e)
            self.cnt[e] = 0
        self.dq = {}
        for q in ("sp", "act", "pool"):
            ks = []
            for i in range(NDS):
                k = "d_%s%d" % (q, i)
                self.sem[k] = nc.alloc_semaphore(k)
                self.cnt[k] = 0
                ks.append(k)
            self.dq[q] = [ks, 0]
        self.seen = {e: {} for e in self.prog}
        self.ninst = 0
        self.arena_off = 16512
        self.nalloc = 0
        self.psum_t = nc.alloc_psum_tensor("psum_all", [128, 8 * 512], F32)
        self.psum_bufs = [Buf("bank%d" % i) for i in range(8)]

    def arena_reset(self, off=16512):
        self.arena_off = off

    def sb(self, name, shape, dt):
        esz = 2 if dt == BF16 else 4
        n = 1
        for s in shape[1:]:
            n *= s
        nbytes = (n * esz + 63) // 64 * 64
        off = self.arena_off
        assert off + nbytes <= 229344, ("SBUF arena overflow", name, off, nbytes)
        self.arena_off = off + nbytes
        self.nalloc += 1
        t = self.nc.alloc_sbuf_tensor_at("%s_%d" % (name, self.nalloc), list(shape), dt, offset=off)
        return Tl(t, name)

    def bank(self, i, n=1, dt=F32):
        ap = self.psum_t[:, i * 512:(i + n) * 512]
        if dt == BF16:
            ap = ap.bitcast(BF16)
        return V(ap, self.psum_bufs[i:i + n])

    def barrier(self):
        toks = [(k, v) for k, v in self.cnt.items() if v > 0]
        for e in self.prog:
            need = [(k, v) for (k, v) in toks if self.seen[e].get(k, 0) < v and not (k == e)]
            for k, v in need:
                self.seen[e][k] = v
            sem = self.sem

            def emit(eng, need=need):
                for k, v in need:
                    eng.wait_ge(sem[k], v)

            self.prog[e].append(emit)


    def dram(self, name, shape, dt, kind="Internal"):
        return Tl(self.nc.dram_tensor(name, list(shape), dt, kind=kind).ap(), name)

    def _deps(self, e, reads, writes):
        need = {}

        def add(tok):
            if tok is None:
                return
            k, v = tok
            if k == "pe" and e == "pe":
                return
            if self.seen[e].get(k, 0) >= v:
                return
            if need.get(k, 0) < v:
                need[k] = v

        for b in reads:
            add(b.w)
        for b in writes:
            add(b.w)
            for t in b.r:
                add(t)
        for k, v in need.items():
            self.seen[e][k] = v
        return list(need.items())

    def _record(self, tok, reads, writes):
        for b in writes:
            b.w = tok
            b.r = []
        for b in reads:
            if b in writes:
                continue
            b.r.append(tok)
            if len(b.r) > 24:
                mx = {}
                for k, v in b.r:
                    if mx.get(k, 0) < v:
                        mx[k] = v
                b.r = list(mx.items())

    def op(self, e, fn, reads, writes):
        rb = [b for x in reads for b in x.bufs]
        wb = [b for x in writes for b in x.bufs]
        waits = self._deps(e, rb, wb)
        self.cnt[e] += 1
        tok = (e, self.cnt[e])
        sem = self.sem
        se = sem[e]

        def emit(eng, waits=waits, fn=fn, se=se):
            for k, v in waits:
                eng.wait_ge(sem[k], v)
            fn(eng).then_inc(se, 1)

        self.prog[e].append(emit)
        self._record(tok, rb, wb)
        self.ninst += 1
        return tok

    def dma(self, q, out, in_, **kw):
        rb = list(in_.bufs)
        wb = list(out.bufs)
        waits = self._deps(q, rb, wb)
        ks, rr = self.dq[q]
        k = ks[rr % NDS]
        self.dq[q][1] = rr + 1
        if self.cnt[k] > 0 and self.seen[q].get(k, 0) < self.cnt[k]:
            waits = [w for w in waits if w[0] != k] + [(k, self.cnt[k])]
            self.seen[q][k] = self.cnt[k]
        self.cnt[k] += 16
        tok = (k, self.cnt[k])
        sem = self.sem
        oap, iap = out.ap, in_.ap

        def emit(eng, waits=waits, sk=sem[k]):
            for kk, v in waits:
                eng.wait_ge(sem[kk], v)
            eng.dma_start(out=oap, in_=iap, **kw).then_inc(sk, 16)

        self.prog[q].append(emit)
        self._record(tok, rb, wb)
        self.ninst += 1
        return tok

    def wait_all(self, e, toks):
        sem = self.sem
        toks = [t for t in toks if t is not None]

        def emit(eng):
            for k, v in toks:
                eng.wait_ge(sem[k], v)

        self.prog[e].append(emit)

    def mm(self, out, lhsT, rhs, start=True, stop=True, extra_reads=()):
        return self.op("pe", lambda eng: eng.matmul(out.ap, lhsT.ap, rhs.ap, start=start, stop=stop),
                       [lhsT, rhs] + list(extra_reads), [out])

    def tr(self, out, in_, ident):
        return self.op("pe", lambda eng: eng.transpose(out.ap, in_.ap, ident.ap), [in_, ident], [out])

    def act(self, out, in_, func, bias=None, scale=1.0, accum_out=None, e="act"):
        reads = [in_]
        kw = {}
        if bias is not None:
            if isinstance(bias, V):
                reads.append(bias)
                kw["bias"] = bias.ap
            else:
                kw["bias"] = bias
        if isinstance(scale, V):
            reads.append(scale)
            kw["scale"] = scale.ap
        else:
            kw["scale"] = scale
        writes = [out]
        if accum_out is not None:
            writes.append(accum_out)
            kw["accum_out"] = accum_out.ap
        return self.op("act", lambda eng: eng.activation(out.ap, in_.ap, func, **kw), reads, writes)

    def tt(self, e, out, a, b, op):
        return self.op(e, lambda eng: eng.tensor_tensor(out.ap, a.ap, b.ap, op), [a, b], [out])

    def ts(self, e, out, a, s1, s2, op0, op1=None, accum_out=None):
        reads = [a]
        s1a = s1.ap if isinstance(s1, V) else s1
        s2a = s2.ap if isinstance(s2, V) else s2
        if isinstance(s1, V):
            reads.append(s1)
        if isinstance(s2, V):
            reads.append(s2)
        writes = [out]
        kw = {}
        if accum_out is not None:
            writes.append(accum_out)
            kw["accum_out"] = accum_out.ap
        if op1 is None:
            return self.op(e, lambda eng: eng.tensor_scalar(out.ap, a.ap, s1a, None, op0, **kw), reads, writes)
        return self.op(e, lambda eng: eng.tensor_scalar(out.ap, a.ap, s1a, s2a, op0, op1, **kw), reads, writes)

    def stt(self, e, out, a, s, b, op0, op1):
        reads = [a, b]
        sa = s.ap if isinstance(s, V) else s
        if isinstance(s, V):
            reads.append(s)
        return self.op(e, lambda eng: eng.scalar_tensor_tensor(out.ap, a.ap, sa, b.ap, op0, op1), reads, [out])

    def copy(self, e, out, in_):
        if e == "act":
            return self.op("act", lambda eng: eng.copy(out.ap, in_.ap), [in_], [out])
        return self.op(e, lambda eng: eng.tensor_copy(out.ap, in_.ap), [in_], [out])

    def memset(self, e, out, val):
        return self.op(e, lambda eng: eng.memset(out.ap, val), [], [out])

    def finish(self, final_tokens):
        nc = self.nc
        self.wait_all("sp", final_tokens)
        prog = self.prog
        with nc.Block() as blk:
            @blk.sync
            def _(eng):
                for f in prog["sp"]:
                    f(eng)

            @blk.tensor
            def _(eng):
                for f in prog["pe"]:
                    f(eng)

            @blk.vector
            def _(eng):
                for f in prog["dve"]:
                    f(eng)

            @blk.scalar
            def _(eng):
                for f in prog["act"]:
                    f(eng)

            @blk.gpsimd
            def _(eng):
                for f in prog["pool"]:
                    f(eng)


NL = 2
T = 2304
D = 2048
NT = 18
KC = 16
NCTX = 256
IN_COLS = 15424
OFF_Z, OFF_XBC, OFF_DT, OFF_Q, OFF_K, OFF_V, OFF_G = 0, 2048, 5120, 5184, 7232, 9280, 11328
LN_EPS = 1e-6
RMS_EPS = 1e-5
ALPHA = (2.0 * NL) ** 0.25
TG = [(0, 512), (512, 512), (1024, 512), (1536, 512), (2048, 256)]


def bcast_rows(ap1d, n=128):
    return ap1d.partition_broadcast(n)


class Ctx:
    pass


def declare_io(S, lim=99):
    nc = S.nc
    g = Ctx()
    need = {"w_br_ssm": 6, "w_br_att": 6, "w_out": 6, "w_eg": 7, "w_eu": 7, "w_ed": 7, "w_in": 2}

    def inp(name, shape, dt=F32):
        if need.get(name, 0) > lim:
            return None
        return S.dram(name, shape, dt, kind="ExternalInput")

    g.xin = inp("xin", [T, D])
    g.cT = inp("cT", [128, 32])
    g.w_ada = inp("w_ada", [NL, D, 6 * D])
    g.b_ada = inp("b_ada", [NL, 6 * D])
    g.w_in = inp("w_in", [NL, D, IN_COLS])
    g.convw = inp("convw", [NL, 128, 24 * 5])
    g.convb = inp("convb", [NL, 128, 24])
    g.dtb = inp("dtb", [NL, 64])
    g.alog = inp("alog", [NL, 64])
    g.ssmd = inp("ssmd", [NL, 32])
    g.normg = inp("normg", [NL, D])
    g.lamv = inp("lamv", [NL, 4 * 64])
    g.subg = inp("subg", [NL, 128, 1])
    g.w_br_ssm = inp("w_br_ssm", [NL, D, D])
    g.w_br_att = inp("w_br_att", [NL, D, D])
    g.w_out = inp("w_out", [NL, D, D])
    g.ln1g = inp("ln1g", [NL, D])
    g.ln1b = inp("ln1b", [NL, D])
    g.ln2g = inp("ln2g", [NL, D])
    g.ln2b = inp("ln2b", [NL, D])
    g.wr = inp("wr", [NL, D, 36])
    g.br = inp("br", [NL, 36])
    g.w_eg = inp("w_eg", [NL, 32, D, 1024])
    g.w_eu = inp("w_eu", [NL, 32, D, 1024])
    g.w_ed = inp("w_ed", [NL, 32, 1024, D])
    g.costab = inp("costab", [T, 32])
    g.sintab = inp("sintab", [T, 32])
    g.identf = inp("identf", [128, 128])
    g.tri = inp("tri", [2, 128, 128])
    g.negm = inp("negm", [2, 128, 128])
    g.out = S.dram("out", [2048, D], F32, kind="ExternalOutput")
    g.MOD = S.dram("MOD", [NL, 2, 6 * D], F32)
    g.H = S.dram("H", [T, D], F32)
    g.Z = S.dram("Z", [T, D], F32)
    g.XBCT = S.dram("XBCT", [3072, T], F32)
    g.DTR = S.dram("DTR", [T, 64], F32)
    g.QT = S.dram("QT", [16, 128, T], BF16)
    g.QN = S.dram("QN", [16, 128, T], BF16)
    g.KT = S.dram("KT", [16, 128, T], BF16)
    g.Vd = S.dram("Vd", [T, D], BF16)
    g.GT = S.dram("GT", [4096, T], F32)
    g.dbg = {}
    return g


def load_consts(S, g):
    c = Ctx()
    c.identf = S.sb("identf", [128, 128], F32)
    c.identb = S.sb("identb", [128, 128], BF16)
    c.ones_b = S.sb("ones_b", [128, 128], BF16)
    c.ones_f = S.sb("ones_f", [128, 128], F32)
    S.dma("sp", c.identf[:], g.identf[:])
    S.copy("dve", c.identb[:], c.identf[:])
    S.memset("dve", c.ones_b[:], 1.0)
    S.memset("dve", c.ones_f[:], 1.0)
    c.eps_ln = S.sb("eps_ln", [128, 1], F32)
    c.eps_rms = S.sb("eps_rms", [128, 1], F32)
    S.memset("dve", c.eps_ln[:], LN_EPS)
    S.memset("dve", c.eps_rms[:], RMS_EPS)
    return c


def phase_mod(S, g, c, l):
    base = S.arena_off
    cT = S.sb("cT", [128, 32], F32)
    scT = S.sb("scT", [128, 32], BF16)
    S.dma("sp", cT[:], g.cT[:])
    S.act(scT[:], cT[:], AF.Silu)
    wb = [S.sb("wada%d" % i, [128, KC, 512], BF16) for i in range(2)]
    bb = [S.sb("bada%d" % i, [2, 512], F32) for i in range(2)]
    ob = [S.sb("oada%d" % i, [2, 512], F32) for i in range(2)]
    wv = g.w_ada.t[l].rearrange("(kc p) n -> p kc n", p=128)
    for nb in range(24):
        w = wb[nb % 2]
        S.dma("pool", w[:], g.w_ada.v(wv[:, :, nb * 512:(nb + 1) * 512]))
        S.dma("sp", bb[nb % 2][:], g.b_ada.v(g.b_ada.t[l, nb * 512:(nb + 1) * 512].partition_broadcast(2)))
        ps = S.bank(nb % 2)
        pv = V(ps.ap[0:2, :], ps.bufs)
        for kc in range(KC):
            S.mm(pv, scT[:, kc * 2:(kc + 1) * 2], w[:, kc, :], start=(kc == 0), stop=(kc == KC - 1))
        S.tt("dve", ob[nb % 2][:], pv, bb[nb % 2][:], ALU.add)
        S.dma("sp", g.MOD.v(g.MOD.t[l, :, nb * 512:(nb + 1) * 512]), ob[nb % 2][:])
    S.barrier()
    S.arena_reset(base)


def load_mod_tiles(S, g, l, seg, plus_one):
    res = []
    for row in range(2):
        t = S.sb("mod%d_%d" % (seg, row), [128, D], F32)
        S.dma("sp", t[:], g.MOD.v(g.MOD.t[l, row, seg * D:(seg + 1) * D].partition_broadcast(128)))
        if plus_one:
            S.ts("pool", t[:], t[:], 1.0, None, ALU.add)
        res.append(t)
    return res


def layer_norm_tile(S, c, xt, xn, stats, mv, rstd, nmr):
    for j in range(4):
        S.op("dve", lambda e, j=j: e.bn_stats(stats.t[:, j, :], xt.t[:, j * 512:(j + 1) * 512]),
             [xt[:]], [stats[:]])
    S.op("dve", lambda e: e.bn_aggr(mv.t[:], stats.t[:]), [stats[:]], [mv[:]])
    S.act(rstd[:], mv[:, 1:2], AF.Sqrt, bias=c.eps_ln[:])
    S.op("dve", lambda e: e.reciprocal(rstd.t[:], rstd.t[:]), [rstd[:]], [rstd[:]])
    S.ts("dve", nmr[:], mv[:, 0:1], -1.0, None, ALU.mult)
    S.tt("dve", nmr[:], nmr[:], rstd[:], ALU.mult)
    S.act(xn[:], xt[:], AF.Identity, bias=nmr[:], scale=rstd[:])


def phase_ln_mod(S, g, c, l, UT, seg_shift, seg_scale, src, router=None, flush=None):
    base = S.arena_off
    sc = load_mod_tiles(S, g, l, seg_scale, True)
    sh = load_mod_tiles(S, g, l, seg_shift, False)
    xts = [S.sb("xt%d" % i, [128, D], F32) for i in range(2)]
    xn = S.sb("xn", [128, D], F32)
    ub = S.sb("ub", [128, D], BF16)
    stats = S.sb("stats", [128, 4, 6], F32)
    mv = S.sb("mv", [128, 2], F32)
    rstd = S.sb("rstd", [128, 1], F32)
    nmr = S.sb("nmr", [128, 1], F32)
    S.dma("sp", xts[0][:], src.v(src.t[0:128, :]))
    for t in range(NT):
        xt = xts[t % 2]
        if t + 1 < NT:
            S.dma("sp", xts[(t + 1) % 2][:], src.v(src.t[(t + 1) * 128:(t + 2) * 128, :]))
        row = 1 if t < 2 else 0
        layer_norm_tile(S, c, xt, xn, stats, mv, rstd, nmr)
        S.tt("dve", xn[:], xn[:], sc[row][:], ALU.mult)
        S.tt("pool", xn[:], xn[:], sh[row][:], ALU.add)
        S.copy("act", ub[:], xn[:])
        if router is not None:
            router_tile(S, g, c, router, xn, t)
        for j in range(4):
            pb = S.bank(6 + (j % 2), dt=BF16)
            for i in range(4):
                kc = j * 4 + i
                S.tr(V(pb.ap[:, i * 128:(i + 1) * 128], pb.bufs), ub[:, kc * 128:(kc + 1) * 128], c.identb[:])
            src_v = V(pb.ap[:, 0:512].rearrange("p (a b) -> p a b", a=4), pb.bufs)
            dst_v = UT[:, j * 4:(j + 1) * 4, t * 128:(t + 1) * 128]
            S.copy("act" if j % 2 == 0 else "dve", dst_v, src_v)
    if flush is not None:
        for kc in range(KC):
            S.dma("sp", flush.v(flush.t[kc * 128:(kc + 1) * 128, :]), UT[:, kc, :])
    S.barrier()
    S.arena_reset(base)


def phase_inproj(S, g, c, l, UT):
    base = S.arena_off
    wbuf = [S.sb("win%d" % i, [128, KC, 512], BF16) for i in range(2)]
    stg = [S.sb("stg%d" % i, [128, 512], F32) for i in range(3)]
    stgb = [S.sb("stgb%d" % i, [128, 512], BF16) for i in range(2)]
    fstg = [S.sb("fstg%d" % i, [128, T], F32) for i in range(2)]
    rot = S.sb("rot", [128, 512], BF16)
    unr = S.sb("unr", [128, 512], BF16)
    tmp = [S.sb("rt%d" % i, [128, 256], F32) for i in range(4)]
    hst = S.sb("hst", [128, 4, T], BF16)
    hsn = S.sb("hsn", [128, 4, T], BF16)
    cosb = S.sb("cosb", [128, NT, 32], F32)
    sinb = S.sb("sinb", [128, NT, 32], F32)
    S.dma("sp", cosb[:], g.costab.v(g.costab.t.rearrange("(t p) f -> p t f", p=128)))
    S.dma("sp", sinb[:], g.sintab.v(g.sintab.t.rearrange("(t p) f -> p t f", p=128)))
    wv = g.w_in.t[l].rearrange("(kc p) n -> p kc n", p=128)

    blocks = []
    for i in range(4):
        blocks.append(("z", OFF_Z + i * 512, 512, i))
    for i in range(6):
        blocks.append(("xbc", OFF_XBC + i * 512, 512, i))
    blocks.append(("dt", OFF_DT, 64, 0))
    for i in range(4):
        blocks.append(("q", OFF_Q + i * 512, 512, i))
    for i in range(4):
        blocks.append(("qn", OFF_Q + i * 512, 512, i))
    for i in range(4):
        blocks.append(("k", OFF_K + i * 512, 512, i))
    for i in range(4):
        blocks.append(("v", OFF_V + i * 512, 512, i))
    for i in range(8):
        blocks.append(("g", OFF_G + i * 512, 512, i))

    def load_w(bi):
        kind, c0, ncol, i = blocks[bi]
        w = wbuf[bi % 2]
        S.dma("pool", w[:, :, 0:ncol], g.w_in.v(wv[:, :, c0:c0 + ncol]))

    load_w(0)
    pcnt = 0
    scnt = 0
    fcnt = 0
    for bi, (kind, c0, ncol, i) in enumerate(blocks):
        if bi + 1 < len(blocks):
            load_w(bi + 1)
        w = wbuf[bi % 2]
        if kind in ("z", "dt", "q", "qn", "k", "v"):
            for t in range(NT):
                ps = S.bank(pcnt % 6)
                pcnt += 1
                pv = V(ps.ap[:, 0:ncol], ps.bufs)
                for kc in range(KC):
                    S.mm(pv, UT[:, kc, t * 128:(t + 1) * 128], w[:, kc, 0:ncol], start=(kc == 0), stop=(kc == KC - 1))
                if kind == "z":
                    s = stg[scnt % 3]
                    scnt += 1
                    S.copy("act" if t % 2 == 0 else "dve", s[:], pv)
                    S.dma("sp", g.Z.v(g.Z.t[t * 128:(t + 1) * 128, i * 512:(i + 1) * 512]), s[:])
                elif kind == "dt":
                    s = stg[scnt % 3]
                    scnt += 1
                    S.copy("act", s[:, 0:64], pv)
                    S.dma("sp", g.DTR.v(g.DTR.t[t * 128:(t + 1) * 128, :]), s[:, 0:64])
                elif kind == "v":
                    s = stgb[t % 2]
                    S.copy("act" if t % 2 == 0 else "dve", s[:], pv)
                    S.dma("sp", g.Vd.v(g.Vd.t[t * 128:(t + 1) * 128, i * 512:(i + 1) * 512]), s[:])
                else:
                    pr = ps.ap[:, 0:512].rearrange("p (a h u f) -> p a h u f", a=8, h=2, u=2)
                    u1 = V(pr[:, :, :, 0, :], ps.bufs)
                    u2 = V(pr[:, :, :, 1, :], ps.bufs)
                    tt_ = 0 if kind == "qn" else t
                    cosv = V(cosb.t[:, tt_, :].rearrange("p (h f) -> p h f", h=2).unsqueeze(1).to_broadcast([128, 8, 2, 16]), [cosb.buf])
                    sinv = V(sinb.t[:, tt_, :].rearrange("p (h f) -> p h f", h=2).unsqueeze(1).to_broadcast([128, 8, 2, 16]), [sinb.buf])
                    rr = rot.t[:, :].rearrange("p (a h u f) -> p a h u f", a=8, h=2, u=2)
                    o1 = V(rr[:, :, :, 0, :], [rot.buf])
                    o2 = V(rr[:, :, :, 1, :], [rot.buf])
                    tv = [V(x.t[:, :].rearrange("p (a h f) -> p a h f", a=8, h=2), [x.buf]) for x in tmp]
                    S.tt("dve", tv[0], u1, cosv, ALU.mult)
                    S.tt("dve", tv[1], u2, sinv, ALU.mult)
                    S.tt("pool", o1, tv[0], tv[1], ALU.subtract)
                    S.tt("dve", tv[2], u1, sinv, ALU.mult)
                    S.tt("dve", tv[3], u2, cosv, ALU.mult)
                    S.tt("pool", o2, tv[2], tv[3], ALU.add)
                    srcs = [(rot, hst)]
                    for si, (srct, dstt) in enumerate(srcs):
                        pb = S.bank(6 + si, dt=BF16)
                        for hh in range(4):
                            S.tr(V(pb.ap[:, hh * 128:(hh + 1) * 128], pb.bufs), srct[:, hh * 128:(hh + 1) * 128], c.identb[:])
                        S.copy("act", dstt[:, :, t * 128:(t + 1) * 128],
                               V(pb.ap[:, 0:512].rearrange("p (a b) -> p a b", a=4), pb.bufs))
            if kind in ("q", "k", "qn"):
                dst = {"q": g.QT, "k": g.KT, "qn": g.QN}[kind]
                for hh in range(4):
                    S.dma("sp", dst.v(dst.t[i * 4 + hh]), hst[:, hh, :])
        else:
            for m in range(4):
                fs = fstg[fcnt % 2]
                fcnt += 1
                for (t0, tn) in TG:
                    ps = S.bank(pcnt % 6)
                    pcnt += 1
                    pv = V(ps.ap[:, 0:tn], ps.bufs)
                    for kc in range(KC):
                        S.mm(pv, w[:, kc, m * 128:(m + 1) * 128], UT[:, kc, t0:t0 + tn], start=(kc == 0), stop=(kc == KC - 1))
                    if kind == "g":
                        S.act(fs[:, t0:t0 + tn], pv, AF.Sigmoid)
                    else:
                        S.copy("dve" if (pcnt % 2) else "act", fs[:, t0:t0 + tn], pv)
                dst = g.GT if kind == "g" else g.XBCT
                r0 = i * 512 + m * 128
                S.dma("sp", dst.v(dst.t[r0:r0 + 128, :]), fs[:])
    S.barrier()
    S.arena_reset(base)


def declare_ssd_scratch(S, g):
    g.Xtok = S.dram("Xtok", [T, D], BF16)
    g.Btok = S.dram("Btok", [T, 512], BF16)
    g.BT = S.dram("BT", [512, T], BF16)
    g.CT = S.dram("CT", [512, T], BF16)
    g.ST = S.dram("ST", [2 * NT * 128, D], BF16)
    g.YST = S.dram("YST", [D, T], BF16)
    g.YAT = S.dram("YAT", [D, T], BF16)
    g.YDBG = S.dram("YDBG", [T, D], F32)
    g.DTVd = S.dram("DTVd", [128, NT * 64], F32)
    g.ADTd = S.dram("ADTd", [128, NT * 64], F32)


def phase_ssdprep(S, g, c, l, DTV, ADT):
    base = S.arena_off
    XC = S.sb("XC", [128, 20, T], BF16)
    cw = S.sb("convw", [128, 120], F32)
    cb = S.sb("convb", [128, 24], F32)
    S.dma("sp", cw[:], g.convw.v(g.convw.t[l]))
    S.dma("sp", cb[:], g.convb.v(g.convb.t[l]))
    xin = [S.sb("cin%d" % i, [128, T], F32) for i in range(2)]
    acc = S.sb("cacc", [128, T], F32)
    cstg = [S.sb("cstg%d" % i, [128, T], BF16) for i in range(2)]
    S.dma("sp", xin[0][:], g.XBCT.v(g.XBCT.t[0:128, :]))
    segs = [(0, NCTX), (NCTX, T)]
    for cc in range(24):
        xi = xin[cc % 2]
        if cc + 1 < 24:
            S.dma("sp", xin[(cc + 1) % 2][:], g.XBCT.v(g.XBCT.t[(cc + 1) * 128:(cc + 2) * 128, :]))
        S.act(acc[:], xi[:], AF.Identity, bias=cb[:, cc:cc + 1], scale=cw[:, cc * 5 + 2:cc * 5 + 3])
        for j in (0, 1, 3, 4):
            s = j - 2
            for (a, b) in segs:
                lo = max(a, a - s)
                hi = min(b, b - s)
                S.stt("dve", acc[:, lo:hi], xi[:, lo + s:hi + s], cw[:, cc * 5 + j:cc * 5 + j + 1],
                      acc[:, lo:hi], ALU.mult, ALU.add)
        if cc < 20:
            S.act(XC[:, cc, :], acc[:], AF.Silu)
            if cc >= 16:
                S.dma("sp", g.BT.v(g.BT.t[(cc - 16) * 128:(cc - 15) * 128, :]), XC[:, cc, :])
        else:
            st = cstg[cc % 2]
            S.act(st[:], acc[:], AF.Silu)
            S.dma("sp", g.CT.v(g.CT.t[(cc - 20) * 128:(cc - 19) * 128, :]), st[:])
    xt = [S.sb("xtok%d" % i, [128, 2560], BF16) for i in range(2)]
    for t in range(NT):
        o = xt[t % 2]
        for j in range(5):
            pb = S.bank(6 + (j % 2), dt=BF16)
            for i in range(4):
                S.tr(V(pb.ap[:, i * 128:(i + 1) * 128], pb.bufs), XC[:, j * 4 + i, t * 128:(t + 1) * 128], c.identb[:])
            S.copy("act" if j % 2 == 0 else "dve", o[:, j * 512:(j + 1) * 512], V(pb.ap[:, 0:512], pb.bufs))
        S.dma("sp", g.Xtok.v(g.Xtok.t[t * 128:(t + 1) * 128, :]), o[:, 0:2048])
        S.dma("sp", g.Btok.v(g.Btok.t[t * 128:(t + 1) * 128, :]), o[:, 2048:2560])
    dtr = S.sb("dtr", [128, NT, 64], F32)
    tb = S.sb("dtbb", [128, 64], F32)
    al = S.sb("alb", [128, 64], F32)
    t1 = S.sb("dt1", [128, NT, 64], F32)
    S.dma("sp", dtr[:], g.DTR.v(g.DTR.t.rearrange("(t p) f -> p t f", p=128)))
    S.dma("sp", tb[:], g.dtb.v(g.dtb.t[l].partition_broadcast(128)))
    S.dma("sp", al[:], g.alog.v(g.alog.t[l].partition_broadcast(128)))
    tbb = V(tb.t[:, :].unsqueeze(1).to_broadcast([128, NT, 64]), [tb.buf])
    S.tt("dve", dtr[:], dtr[:], tbb, ALU.add)
    S.act(t1[:], dtr[:], AF.Abs)
    S.act(t1[:], t1[:], AF.Exp, scale=-1.0)
    S.act(t1[:], t1[:], AF.Ln, bias=c.ones_f[:, 0:1])
    S.stt("dve", DTV[:], dtr[:], 0.0, t1[:], ALU.max, ALU.add)
    S.act(al[:], al[:], AF.Exp)
    S.ts("dve", al[:], al[:], -1.0, None, ALU.mult)
    alb = V(al.t[:, :].unsqueeze(1).to_broadcast([128, NT, 64]), [al.buf])
    S.tt("dve", ADT[:], DTV[:], alb, ALU.mult)
    if "DTVd" in DBG_EXT:
        S.dma("sp", g.DTVd[:], V(DTV.t[:, :, :].rearrange("p a b -> p (a b)"), [DTV.buf]))
        S.dma("sp", g.ADTd[:], V(ADT.t[:, :, :].rearrange("p a b -> p (a b)"), [ADT.buf]))
    S.barrier()
    S.arena_reset(base)


def phase_ssd_states(S, g, c, l, DTV, ADT):
    base = S.arena_off
    tri = [S.sb("tri%d" % d, [128, 128], F32) for d in range(2)]
    for d in range(2):
        S.dma("sp", tri[d][:], g.tri.v(g.tri.t[d]))
    St = S.sb("Sstate", [128, D], F32)
    Sb = [S.sb("Sbf%d" % i, [128, D], BF16) for i in range(2)]
    xs = [S.sb("xs%d" % i, [128, D], BF16) for i in range(2)]
    bs = [S.sb("bs%d" % i, [128, 512], BF16) for i in range(2)]
    xdd = S.sb("xdd", [128, D], BF16)
    cs = S.sb("cs", [128, 32], F32)
    df = S.sb("df", [128, 32], F32)
    dte = S.sb("dte", [128, 32], F32)
    dch = S.sb("dch", [128, 32], F32)
    it = 0
    for d in range(2):
        order = list(range(NT)) if d == 0 else [1, 0] + list(range(NT - 1, 1, -1))
        S.memset("pool", St[:], 0.0)
        for ch in order:
            x = xs[it % 2]
            b = bs[it % 2]
            sb_ = Sb[it % 2]
            it += 1
            S.dma("sp", x[:], g.Xtok.v(g.Xtok.t[ch * 128:(ch + 1) * 128, :]))
            S.dma("sp", b[:], g.Btok.v(g.Btok.t[ch * 128:(ch + 1) * 128, :]))
            adt = ADT[:, ch, d * 32:(d + 1) * 32]
            pm = S.bank(4)
            pcs = V(pm.ap[:, 0:32], pm.bufs)
            pend = V(pm.ap[:, 32:64], pm.bufs)
            S.mm(pcs, tri[d][:], adt)
            S.mm(pend, c.ones_f[:], adt)
            S.copy("act", cs[:], pcs)
            S.tt("dve", df[:], pend, cs[:], ALU.subtract)
            S.act(df[:], df[:], AF.Exp)
            S.tt("dve", dte[:], df[:], DTV[:, ch, d * 32:(d + 1) * 32], ALU.mult)
            S.act(dch[:], pend, AF.Exp)
            xv = V(x.t[:, :].rearrange("p (h q) -> p h q", h=32), [x.buf])
            xo = V(xdd.t[:, :].rearrange("p (h q) -> p h q", h=32), [xdd.buf])
            S.tt("dve", xo, xv, V(dte.t[:, :].unsqueeze(2).to_broadcast([128, 32, 64]), [dte.buf]), ALU.mult)
            S.copy("act", sb_[:], St[:])
            r0 = (d * NT + ch) * 128
            S.dma("sp", g.ST.v(g.ST.t[r0:r0 + 128, :]), sb_[:])
            ps = S.bank(0, 4)
            for gi in range(4):
                S.mm(V(ps.ap[:, gi * 512:(gi + 1) * 512], ps.bufs[gi:gi + 1]), b[:, gi * 128:(gi + 1) * 128], xdd[:, gi * 512:(gi + 1) * 512])
            sv = V(St.t[:, :].rearrange("p (h q) -> p h q", h=32), [St.buf])
            S.tt("pool", sv, sv, V(dch.t[:, :].unsqueeze(2).to_broadcast([128, 32, 64]), [dch.buf]), ALU.mult)
            S.tt("dve", St[:], St[:], ps, ALU.add)
    S.barrier()
    S.arena_reset(base)


def phase_ssd_y(S, g, c, l, DTV, ADT):
    base = S.arena_off
    YST = S.sb("YSTres", [128, KC, T], BF16)
    tri = [S.sb("tri%d" % d, [128, 128], F32) for d in range(2)]
    negm = [S.sb("negm%d" % d, [128, 128], F32) for d in range(2)]
    for d in range(2):
        S.dma("sp", tri[d][:], g.tri.v(g.tri.t[d]))
        S.dma("sp", negm[d][:], g.negm.v(g.negm.t[d]))
    BT = S.sb("BTres", [128, 4, T], BF16)
    CT = S.sb("CTres", [128, 4, T], BF16)
    for gi in range(4):
        S.dma("sp", BT[:, gi, :], g.BT.v(g.BT.t[gi * 128:(gi + 1) * 128, :]))
        S.dma("sp", CT[:, gi, :], g.CT.v(g.CT.t[gi * 128:(gi + 1) * 128, :]))
    dsk = S.sb("dsk", [128, 32], F32)
    S.dma("sp", dsk[:], g.ssmd.v(g.ssmd.t[l].partition_broadcast(128)))
    ng = S.sb("normg", [128, D], F32)
    S.dma("sp", ng[:], g.normg.v(g.normg.t[l].partition_broadcast(128)))
    xs = [S.sb("yx%d" % i, [128, D], BF16) for i in range(2)]
    sts = [[S.sb("yst%d_%d" % (d, i), [128, D], BF16) for i in range(2)] for d in range(2)]
    zs = [S.sb("yz%d" % i, [128, D], F32) for i in range(2)]
    xdt = [S.sb("xdt%d" % d, [128, D], BF16) for d in range(2)]
    cs = S.sb("ycs", [128, 64], F32)
    ecs = S.sb("yecs", [128, 64], F32)
    CBT = S.sb("CBT", [128, 4, 128], F32)
    R = [S.sb("R%d" % i, [128, 4, 128], F32) for i in range(2)]
    Dm = [S.sb("Dm%d" % i, [128, 4, 128], F32) for i in range(2)]
    MT = [S.sb("MT%d" % i, [128, 4, 128], BF16) for i in range(2)]
    yacc = S.sb("yacc", [128, D], F32)
    t2 = S.sb("yt2", [128, 1024], F32)
    ybf = S.sb("ybf", [128, D], BF16)
    ss = S.sb("yss", [128, 4], F32)
    junk = S.sb("yjunk", [128, 512], F32)

    def loads(ch, i):
        S.dma("sp", xs[i][:], g.Xtok.v(g.Xtok.t[ch * 128:(ch + 1) * 128, :]))
        for d in range(2):
            r0 = (d * NT + ch) * 128
            S.dma("sp", sts[d][i][:], g.ST.v(g.ST.t[r0:r0 + 128, :]))
        S.dma("sp", zs[i][:], g.Z.v(g.Z.t[ch * 128:(ch + 1) * 128, :]))

    loads(0, 0)
    gcnt = 0
    for ch in range(NT):
        i = ch % 2
        if ch + 1 < NT:
            loads(ch + 1, (ch + 1) % 2)
        x = xs[i]
        z = zs[i]
        tsl = slice(ch * 128, (ch + 1) * 128)
        pm = S.bank(6)
        for d in range(2):
            S.mm(V(pm.ap[:, d * 32:(d + 1) * 32], pm.bufs), tri[d][:], ADT[:, ch, d * 32:(d + 1) * 32])
        S.copy("act", cs[:], V(pm.ap[:, 0:64], pm.bufs))
        S.act(ecs[:], V(pm.ap[:, 0:64], pm.bufs), AF.Exp)
        pc = S.bank(7)
        for gi in range(4):
            S.mm(V(pc.ap[:, gi * 128:(gi + 1) * 128], pc.bufs), BT[:, gi, tsl], CT[:, gi, tsl])
        S.copy("dve", V(CBT.t[:, :, :].rearrange("p a b -> p (a b)"), [CBT.buf]), pc)
        xv = V(x.t[:, :].rearrange("p (h q) -> p h q", h=32), [x.buf])
        for d in range(2):
            xo = V(xdt[d].t[:, :].rearrange("p (h q) -> p h q", h=32), [xdt[d].buf])
            dv = V(DTV.t[:, ch, d * 32:(d + 1) * 32].unsqueeze(2).to_broadcast([128, 32, 64]), [DTV.buf])
            S.tt("pool", xo, xv, dv, ALU.mult)
        for half in range(2):
            yp = S.bank(0, 2)
            for jj in range(4):
                j = half * 4 + jj
                gi = j // 2
                for d in range(2):
                    k = gcnt % 2
                    gcnt += 1
                    a4 = V(ADT.t[:, ch, d * 32 + 4 * j:d * 32 + 4 * j + 4].unsqueeze(2).to_broadcast([128, 4, 128]), [ADT.buf])
                    tb = V(tri[d].t[:, :].unsqueeze(1).to_broadcast([128, 4, 128]), [tri[d].buf])
                    S.tt("dve", R[k][:], tb, a4, ALU.mult)
                    dp = S.bank(4 + k)
                    S.mm(dp, c.ones_f[:], V(R[k].t[:, :, :].rearrange("p a b -> p (a b)"), [R[k].buf]))
                    for ii in range(4):
                        h = 4 * j + ii
                        S.stt("dve", Dm[k][:, ii, :], V(dp.ap[:, ii * 128:(ii + 1) * 128], dp.bufs),
                              cs[:, d * 32 + h:d * 32 + h + 1], negm[d][:], ALU.subtract, ALU.add)
                    S.act(Dm[k][:], Dm[k][:], AF.Exp)
                    cb = V(CBT.t[:, gi, :].unsqueeze(1).to_broadcast([128, 4, 128]), [CBT.buf])
                    S.tt("pool", MT[d][:], Dm[k][:], cb, ALU.mult)
                for ii in range(4):
                    h = 4 * j + ii
                    hl = h - half * 16
                    for d in range(2):
                        S.mm(V(yp.ap[:, hl * 64:(hl + 1) * 64], yp.bufs[hl // 8:hl // 8 + 1]), MT[d][:, ii, :],
                             xdt[d][:, h * 64:(h + 1) * 64], start=(d == 0), stop=(d == 1))
            ya = yacc[:, half * 1024:(half + 1) * 1024]
            ya3 = V(yacc.t[:, half * 1024:(half + 1) * 1024].rearrange("p (h q) -> p h q", h=16), [yacc.buf])
            for d in range(2):
                yo = S.bank(2, 2)
                for gg in range(2):
                    gi = half * 2 + gg
                    S.mm(V(yo.ap[:, gg * 512:(gg + 1) * 512], yo.bufs[gg:gg + 1]), CT[:, gi, tsl],
                         sts[d][i][:, gi * 512:(gi + 1) * 512])
                ev = V(ecs.t[:, d * 32 + half * 16:d * 32 + half * 16 + 16].unsqueeze(2).to_broadcast([128, 16, 64]), [ecs.buf])
                yo3 = V(yo.ap.rearrange("p (h q) -> p h q", h=16), yo.bufs)
                if d == 0:
                    S.tt("dve", ya3, yo3, ev, ALU.mult)
                else:
                    t23 = V(t2.t[:, :].rearrange("p (h q) -> p h q", h=16), [t2.buf])
                    S.tt("dve", t23, yo3, ev, ALU.mult)
                    S.tt("pool", ya, ya, t2[:], ALU.add)
            S.tt("dve", ya, ya, yp, ALU.add)
        if "YDBG" in DBG_EXT:
            S.dma("sp", g.YDBG.v(g.YDBG.t[ch * 128:(ch + 1) * 128, :]), yacc[:])
        y3 = V(yacc.t[:, :].rearrange("p (h q) -> p h q", h=32), [yacc.buf])
        dk = V(dsk.t[:, :].unsqueeze(2).to_broadcast([128, 32, 64]), [dsk.buf])
        xf = V(z.t[:, :].rearrange("p (h q) -> p h q", h=32), [z.buf])
        S.act(z[:], z[:], AF.Silu)
        S.tt("dve", yacc[:], yacc[:], z[:], ALU.mult)
        t3 = V(junk.t[:, :], [junk.buf])
        for q4 in range(4):
            xq = V(x.t[:, q4 * 512:(q4 + 1) * 512].rearrange("p (h q) -> p h q", h=8), [x.buf])
            dq = V(dsk.t[:, q4 * 8:(q4 + 1) * 8].unsqueeze(2).to_broadcast([128, 8, 64]), [dsk.buf])
            j3 = V(junk.t[:, :].rearrange("p (h q) -> p h q", h=8), [junk.buf])
            S.tt("pool", j3, xq, dq, ALU.mult)
            S.tt("pool", junk[:], junk[:], z[:, q4 * 512:(q4 + 1) * 512], ALU.mult)
            S.tt("dve", yacc[:, q4 * 512:(q4 + 1) * 512], yacc[:, q4 * 512:(q4 + 1) * 512], junk[:], ALU.add)
        S.memset("dve", ss[:], 0.0)
        for gi in range(4):
            S.act(junk[:], yacc[:, gi * 512:(gi + 1) * 512], AF.Square, accum_out=ss[:, gi:gi + 1])
        S.act(ss[:], ss[:], AF.Sqrt, bias=c.eps_rms[:], scale=1.0 / 512)
        S.op("dve", lambda e: e.reciprocal(ss.t[:], ss.t[:]), [ss[:]], [ss[:]])
        for gi in range(4):
            S.act(yacc[:, gi * 512:(gi + 1) * 512], yacc[:, gi * 512:(gi + 1) * 512], AF.Copy, scale=ss[:, gi:gi + 1])
        S.tt("dve", ybf[:], yacc[:], ng[:], ALU.mult)
        for j in range(4):
            pb = S.bank(6 + (j % 2), dt=BF16)
            for ii in range(4):
                kc = j * 4 + ii
                S.tr(V(pb.ap[:, ii * 128:(ii + 1) * 128], pb.bufs), ybf[:, kc * 128:(kc + 1) * 128], c.identb[:])
            S.copy("act", YST[:, j * 4:(j + 1) * 4, tsl], V(pb.ap[:, 0:512].rearrange("p (a b) -> p a b", a=4), pb.bufs))
    for kc in range(KC):
        S.dma("sp", g.YST.v(g.YST.t[kc * 128:(kc + 1) * 128, :]), YST[:, kc, :])
    S.barrier()
    S.arena_reset(base)


def phase_attn(S, g, c, l, last=False):
    import math as _m
    base = S.arena_off
    lam_init = 0.8 - 0.6 * _m.exp(-0.3 * l)
    lv = S.sb("lamv", [128, 256], F32)
    S.dma("sp", lv[:], g.lamv.v(g.lamv.t[l].partition_broadcast(128)))
    pr = S.sb("lampr", [128, 128], F32)
    sm = S.sb("lamsm", [128, 2], F32)
    neglam = S.sb("neglam", [128, 1], F32)
    gsc = S.sb("gsc", [128, 1], F32)
    S.tt("dve", pr[:, 0:64], lv[:, 0:64], lv[:, 64:128], ALU.mult)
    S.tt("dve", pr[:, 64:128], lv[:, 128:192], lv[:, 192:256], ALU.mult)
    for i in range(2):
        S.op("dve", lambda e, i=i: e.reduce_sum(sm.t[:, i:i + 1], pr.t[:, i * 64:(i + 1) * 64], AX.X), [pr[:]], [sm[:]])
    S.act(sm[:], sm[:], AF.Exp)
    S.tt("dve", neglam[:], sm[:, 1:2], sm[:, 0:1], ALU.subtract)
    S.ts("dve", neglam[:], neglam[:], -lam_init, None, ALU.add)
    S.dma("sp", gsc[:], g.subg.v(g.subg.t[l]))
    S.ts("dve", gsc[:], gsc[:], 1.0 - lam_init, None, ALU.mult)

    KTh = [S.sb("KTh%d" % i, [128, T], BF16) for i in range(2)]
    QTh = [S.sb("QTh%d" % i, [128, T], BF16) for i in range(2)]
    QNh = [S.sb("QNh%d" % i, [128, T], BF16) for i in range(2)]
    Vh = [S.sb("Vh%d" % i, [128, NT, 128], BF16) for i in range(2)]
    YA = [S.sb("YAh%d" % i, [128, T], BF16) for i in range(2)]
    psum_acc = [S.sb("pacc%d" % m, [128, 512], F32) for m in range(2)]
    PT = [S.sb("PT%d" % i, [128, 512], BF16) for i in range(6)]
    r = [S.sb("ar%d" % i, [128, 512], F32) for i in range(2)]
    tt_ = [S.sb("at%d" % i, [128, 512], F32) for i in range(2)]
    o = S.sb("ao", [128, 512], F32)
    sq = S.sb("asq", [128, 512], BF16)
    rs = S.sb("ars", [128, 512], F32)

    def loads(h, i):
        S.dma("sp", KTh[i][:], g.KT.v(g.KT.t[h]))
        S.dma("sp", QTh[i][:], g.QT.v(g.QT.t[h]))
        S.dma("sp", QNh[i][:], g.QN.v(g.QN.t[h]))
        S.dma("sp", Vh[i][:], g.Vd.v(g.Vd.t[:, h * 128:(h + 1) * 128].rearrange("(kc p) e -> p kc e", p=128)))

    loads(0, 0)
    pcnt = 0
    qgroups = [(NCTX + i * 512, 512, list(range(NT))) for i in range(4)] + ([] if last else [(0, NCTX, [0, 1])])
    for h in range(16):
        i = h % 2
        if h + 1 < 16:
            loads(h + 1, (h + 1) % 2)
        for (q0, nq, kcs) in qgroups:
            seq = [(m, ki, kc) for m in range(2) for ki, kc in enumerate(kcs)]
            LA = 3
            obv = []
            sbv = []
            for m in range(2):
                ob = S.bank(4 + m)
                sb_ = S.bank(6 + m)
                obv.append(V(ob.ap[:, 0:nq], ob.bufs))
                sbv.append(V(sb_.ap[:, 0:nq], sb_.bufs))
            pts = {}
            for si in range(len(seq) + LA):
                if si < len(seq):
                    m, ki, kc = seq[si]
                    ps_ = slice(m * 64, (m + 1) * 64)
                    st = S.bank(pcnt % 4)
                    stv = V(st.ap[:, 0:nq], st.bufs)
                    qsrc = QNh[i] if kc < 2 else QTh[i]
                    S.mm(stv, KTh[i][ps_, kc * 128:(kc + 1) * 128], qsrc[ps_, q0:q0 + nq])
                    p = PT[pcnt % 6]
                    pcnt += 1
                    S.act(p[:, 0:nq], stv, AF.Exp, scale=0.125)
                    pts[si] = p
                    if ki == 0:
                        S.copy("dve", psum_acc[m][:, 0:nq], p[:, 0:nq])
                    else:
                        S.tt("dve", psum_acc[m][:, 0:nq], psum_acc[m][:, 0:nq], p[:, 0:nq], ALU.add)
                sj = si - LA
                if sj >= 0:
                    m, ki, kc = seq[sj]
                    p = pts.pop(sj)
                    S.mm(obv[m], Vh[i][:, kc, :], p[:, 0:nq], start=(ki == 0), stop=(ki == len(kcs) - 1))
            for m in range(2):
                S.mm(sbv[m], c.ones_f[:], psum_acc[m][:, 0:nq])
                S.op("dve", lambda e, m=m, sv=sbv[m], nq=nq: e.reciprocal(r[m].t[:, 0:nq], sv.ap), [sbv[m]], [r[m][:]])
                S.tt("dve", tt_[m][:, 0:nq], obv[m], r[m][:, 0:nq], ALU.mult)
            S.stt("dve", o[:, 0:nq], tt_[1][:, 0:nq], neglam[:], tt_[0][:, 0:nq], ALU.mult, ALU.add)
            S.act(sq[:, 0:nq], o[:, 0:nq], AF.Square)
            mb = S.bank(0)
            mbv = V(mb.ap[:, 0:nq], mb.bufs)
            S.mm(mbv, c.ones_b[:], sq[:, 0:nq])
            S.act(rs[:, 0:nq], mbv, AF.Sqrt, bias=c.eps_rms[:], scale=1.0 / 128)
            S.op("dve", lambda e, nq=nq: e.reciprocal(rs.t[:, 0:nq], rs.t[:, 0:nq]), [rs[:]], [rs[:]])
            S.tt("pool", o[:, 0:nq], o[:, 0:nq], rs[:, 0:nq], ALU.mult)
            S.act(YA[i][:, q0:q0 + nq], o[:, 0:nq], AF.Copy, scale=gsc[:])
        S.dma("sp", g.YAT.v(g.YAT.t[h * 128:(h + 1) * 128, :]), YA[i][:])
    S.barrier()
    S.arena_reset(base)


def declare_merge_scratch(S, g):
    g.MTd = S.dram("MTd", [D, T], BF16)
    g.U2T = S.dram("U2T", [D, T], BF16)
    g.YM = S.dram("YM", [T, D], F32)


def phase_merge(S, g, c, l):
    base = S.arena_off
    YS = S.sb("YSr", [128, KC, T], BF16)
    YA = S.sb("YAr", [128, KC, T], BF16)
    for kc in range(KC):
        S.dma("sp", YS[:, kc, :], g.YST.v(g.YST.t[kc * 128:(kc + 1) * 128, :]))
        S.dma("sp", YA[:, kc, :], g.YAT.v(g.YAT.t[kc * 128:(kc + 1) * 128, :]))
    ws = [S.sb("wbs%d" % i, [128, KC, 128], BF16) for i in range(2)]
    wa = [S.sb("wba%d" % i, [128, KC, 128], BF16) for i in range(2)]
    gs = S.sb("ggs", [128, T], F32)
    ga = S.sb("gga", [128, T], F32)
    m1 = [S.sb("mm1_%d" % i, [128, 512], F32) for i in range(2)]
    m2 = [S.sb("mm2_%d" % i, [128, 512], F32) for i in range(2)]
    mrow = [S.sb("mrow%d" % i, [128, T], BF16) for i in range(2)]
    wsv = g.w_br_ssm.t[l].rearrange("(kc p) n -> p kc n", p=128)
    wav = g.w_br_att.t[l].rearrange("(kc p) n -> p kc n", p=128)

    def loadw(cc):
        S.dma("pool", ws[cc % 2][:], g.w_br_ssm.v(wsv[:, :, cc * 128:(cc + 1) * 128]))
        S.dma("pool", wa[cc % 2][:], g.w_br_att.v(wav[:, :, cc * 128:(cc + 1) * 128]))

    loadw(0)
    cnt = 0
    for cc in range(16):
        if cc + 1 < 16:
            loadw(cc + 1)
        S.dma("sp", gs[:], g.GT.v(g.GT.t[cc * 128:(cc + 1) * 128, :]))
        S.dma("sp", ga[:], g.GT.v(g.GT.t[D + cc * 128:D + (cc + 1) * 128, :]))
        mr = mrow[cc % 2]
        for (t0, tn) in TG:
            k = cnt % 2
            cnt += 1
            p1 = S.bank(2 * k)
            p2 = S.bank(2 * k + 1)
            p1v = V(p1.ap[:, 0:tn], p1.bufs)
            p2v = V(p2.ap[:, 0:tn], p2.bufs)
            for kc in range(KC):
                S.mm(p1v, ws[cc % 2][:, kc, :], YS[:, kc, t0:t0 + tn], start=(kc == 0), stop=(kc == KC - 1))
            for kc in range(KC):
                S.mm(p2v, wa[cc % 2][:, kc, :], YA[:, kc, t0:t0 + tn], start=(kc == 0), stop=(kc == KC - 1))
            S.tt("dve", m1[k][:, 0:tn], p1v, gs[:, t0:t0 + tn], ALU.mult)
            S.tt("dve", m2[k][:, 0:tn], p2v, ga[:, t0:t0 + tn], ALU.mult)
            S.tt("pool", mr[:, t0:t0 + tn], m1[k][:, 0:tn], m2[k][:, 0:tn], ALU.add)
        S.dma("sp", g.MTd.v(g.MTd.t[cc * 128:(cc + 1) * 128, :]), mr[:])
    S.barrier()
    S.arena_reset(base)


def bcast_tile(S, name, dram_tl, ap1d):
    t = S.sb(name, [128, D], F32)
    S.dma("sp", t[:], dram_tl.v(ap1d.partition_broadcast(128)))
    return t


def phase_outproj(S, g, c, l, src):
    base = S.arena_off
    wo = S.sb("wout", [128, KC, D], BF16)
    wov = g.w_out.t[l].rearrange("(kc p) n -> p kc n", p=128)
    for n in range(4):
        S.dma("pool", wo[:, :, n * 512:(n + 1) * 512], g.w_out.v(wov[:, :, n * 512:(n + 1) * 512]))
    g1 = load_mod_tiles(S, g, l, 2, False)
    lg = bcast_tile(S, "ln1g", g.ln1g, g.ln1g.t[l])
    lb = bcast_tile(S, "ln1b", g.ln1b, g.ln1b.t[l])
    mts = [S.sb("mt%d" % i, [128, KC, 128], BF16) for i in range(2)]
    hs = [S.sb("hh%d" % i, [128, D], F32) for i in range(2)]
    t1 = S.sb("ot1", [128, D], F32)
    xn = S.sb("oxn", [128, D], F32)
    stats = S.sb("ostats", [128, 4, 6], F32)
    mv = S.sb("omv", [128, 2], F32)
    rstd = S.sb("orstd", [128, 1], F32)
    nmr = S.sb("onmr", [128, 1], F32)
    mtv = g.MTd.t.rearrange("(cc p) t -> p cc t", p=128)

    def loads(t):
        S.dma("sp", mts[t % 2][:], g.MTd.v(mtv[:, :, t * 128:(t + 1) * 128]))
        S.dma("sp", hs[t % 2][:], src.v(src.t[t * 128:(t + 1) * 128, :]))

    loads(0)
    for t in range(NT):
        if t + 1 < NT:
            loads(t + 1)
        row = 1 if t < 2 else 0
        mt = mts[t % 2]
        h = hs[t % 2]
        po = S.bank(0, 4)
        for n in range(4):
            for cc in range(KC):
                S.mm(V(po.ap[:, n * 512:(n + 1) * 512], po.bufs[n:n + 1]), mt[:, cc, :], wo[:, cc, n * 512:(n + 1) * 512],
                     start=(cc == 0), stop=(cc == KC - 1))
        S.tt("dve", t1[:], po, g1[row][:], ALU.mult)
        S.stt("dve", t1[:], h[:], float(ALPHA), t1[:], ALU.mult, ALU.add)
        layer_norm_tile(S, c, t1, xn, stats, mv, rstd, nmr)
        S.tt("pool", xn[:], xn[:], lg[:], ALU.mult)
        S.tt("pool", xn[:], xn[:], lb[:], ALU.add)
        S.dma("sp", g.H.v(g.H.t[t * 128:(t + 1) * 128, :]), xn[:])
    S.barrier()
    S.arena_reset(base)


def router_tile(S, g, c, rt, xn, t):
    u2Tf = rt.u2Tf
    for j in range(4):
        pb = S.bank(4 + (j % 2))
        for i in range(4):
            kc = j * 4 + i
            S.tr(V(pb.ap[:, i * 128:(i + 1) * 128], pb.bufs), xn[:, kc * 128:(kc + 1) * 128], c.identf[:])
        S.copy("dve", u2Tf[:, j * 4:(j + 1) * 4, :], V(pb.ap[:, 0:512].rearrange("p (a b) -> p a b", a=4), pb.bufs))
    pl = S.bank(3)
    plv = V(pl.ap[:, 0:36], pl.bufs)
    for kc in range(KC):
        S.mm(plv, u2Tf[:, kc, :], rt.wr[:, kc, :], start=(kc == 0), stop=(kc == KC - 1))
    lg = rt.lg
    S.tt("dve", lg[:], plv, rt.brb[:], ALU.add)
    mx = rt.s[:, 0:1]
    s4 = rt.s[:, 1:2]
    pg = rt.s[:, 2:3]
    m1 = rt.s[:, 3:4]
    m2 = rt.s[:, 4:5]
    e2 = rt.s[:, 5:6]
    w1 = rt.s[:, 6:7]
    w2 = rt.s[:, 7:8]
    nmx = rt.s[:, 8:9]
    S.op("dve", lambda e: e.reduce_max(rt.s.t[:, 0:1], lg.t[:, 0:4], AX.X), [lg[:]], [rt.s[:]])
    S.ts("dve", nmx, mx, -1.0, None, ALU.mult)
    S.memset("dve", s4, 0.0)
    S.act(rt.e4[:], lg[:, 0:4], AF.Exp, bias=nmx, accum_out=s4)
    S.op("dve", lambda e: e.reciprocal(rt.s.t[:, 2:3], rt.s.t[:, 1:2]), [rt.s[:]], [rt.s[:]])
    S.ts("dve", rt.oh4[:], lg[:, 0:4], mx, None, ALU.is_equal)
    le = V(lg.t[:, 4:36].rearrange("p (g e) -> p g e", g=4), [lg.buf])
    ohb = V(rt.oh4.t[:, :].unsqueeze(2).to_broadcast([128, 4, 8]), [rt.oh4.buf])
    S.tt("dve", V(rt.t32.t[:, :].rearrange("p (g e) -> p g e", g=4), [rt.t32.buf]), le, ohb, ALU.mult)
    S.op("dve", lambda e: e.reduce_sum(rt.l8.t[:, :], rt.t32.t[:, :].rearrange("p (g e) -> p e g", g=4), AX.X), [rt.t32[:]], [rt.l8[:]])
    S.op("dve", lambda e: e.reduce_max(rt.s.t[:, 3:4], rt.l8.t[:, :], AX.X), [rt.l8[:]], [rt.s[:]])
    S.ts("dve", rt.oh1[:], rt.l8[:], m1, None, ALU.is_equal)
    S.stt("dve", rt.l8b[:], rt.oh1[:], -1.0e30, rt.l8[:], ALU.mult, ALU.add)
    S.op("dve", lambda e: e.reduce_max(rt.s.t[:, 4:5], rt.l8b.t[:, :], AX.X), [rt.l8b[:]], [rt.s[:]])
    S.ts("dve", rt.oh2[:], rt.l8b[:], m2, None, ALU.is_equal)
    S.tt("dve", e2, m2, m1, ALU.subtract)
    S.act(e2, e2, AF.Exp)
    S.ts("dve", w1, e2, 1.0, None, ALU.add)
    S.op("dve", lambda e: e.reciprocal(rt.s.t[:, 6:7], rt.s.t[:, 6:7]), [rt.s[:]], [rt.s[:]])
    S.tt("dve", w1, w1, pg, ALU.mult)
    S.tt("dve", w2, w1, e2, ALU.mult)
    S.ts("dve", rt.oh1[:], rt.oh1[:], w1, None, ALU.mult)
    S.stt("dve", rt.w8[:], rt.oh2[:], w2, rt.oh1[:], ALU.mult, ALU.add)
    w8b = V(rt.w8.t[:, :].unsqueeze(1).to_broadcast([128, 4, 8]), [rt.w8.buf])
    S.tt("dve", V(rt.WR.t[:, t, :].rearrange("p (g e) -> p g e", g=4), [rt.WR.buf]), ohb, w8b, ALU.mult)


def router_setup(S, g, c, l, WR):
    rt = Ctx()
    rt.WR = WR
    rt.u2Tf = S.sb("u2Tf", [128, KC, 128], F32)
    rt.wr = S.sb("wr", [128, KC, 36], F32)
    S.dma("sp", rt.wr[:], g.wr.v(g.wr.t[l].rearrange("(kc p) n -> p kc n", p=128)))
    rt.brb = S.sb("brb", [128, 36], F32)
    S.dma("sp", rt.brb[:], g.br.v(g.br.t[l].partition_broadcast(128)))
    rt.lg = S.sb("rlg", [128, 36], F32)
    rt.s = S.sb("rs", [128, 16], F32)
    rt.e4 = S.sb("re4", [128, 4], F32)
    rt.oh4 = S.sb("roh4", [128, 4], F32)
    rt.t32 = S.sb("rt32", [128, 32], F32)
    rt.l8 = S.sb("rl8", [128, 8], F32)
    rt.l8b = S.sb("rl8b", [128, 8], F32)
    rt.oh1 = S.sb("roh1", [128, 8], F32)
    rt.oh2 = S.sb("roh2", [128, 8], F32)
    rt.w8 = S.sb("rw8", [128, 8], F32)
    return rt


def phase_moe(S, g, c, l, WR, last=False):
    base = S.arena_off
    wg = S.sb("wg", [128, KC, 1024], BF16)
    wu = S.sb("wu", [128, KC, 1024], BF16)
    wd = S.sb("wd", [128, 8, D], BF16)
    hT = S.sb("hT", [128, 8, T], BF16)
    ut = [S.sb("ut%d" % i, [128, KC, 512], BF16) for i in range(2)]
    sg = [S.sb("sg%d" % i, [128, 512], F32) for i in range(2)]
    ysb = [S.sb("ysb%d" % i, [128, D], F32) for i in range(2)]
    u2v = g.U2T.t.rearrange("(kc p) t -> p kc t", p=128)

    def load_gu(e):
        gv = g.w_eg.t[l, e].rearrange("(kc p) f -> p kc f", p=128)
        uv = g.w_eu.t[l, e].rearrange("(kc p) f -> p kc f", p=128)
        for hf in range(2):
            S.dma("pool", wg[:, :, hf * 512:(hf + 1) * 512], g.w_eg.v(gv[:, :, hf * 512:(hf + 1) * 512]))
            S.dma("pool", wu[:, :, hf * 512:(hf + 1) * 512], g.w_eu.v(uv[:, :, hf * 512:(hf + 1) * 512]))

    def load_d(e):
        dv = g.w_ed.t[l, e].rearrange("(fc p) d -> p fc d", p=128)
        for hf in range(2):
            S.dma("pool", wd[:, hf * 4:(hf + 1) * 4, :], g.w_ed.v(dv[:, hf * 4:(hf + 1) * 4, :]))

    load_gu(0)
    load_d(0)
    ucnt = 0
    pcnt = 0
    ycnt = 0
    tgs = TG[0:4] if not last else [(NCTX + i * 512, 512) for i in range(4)]
    tiles = list(range(NT)) if not last else list(range(2, NT))
    if not last:
        tgs = TG
    for e in range(32):
        for (t0, tn) in tgs:
            u = ut[ucnt % 2]
            ucnt += 1
            S.dma("sp", u[:, :, 0:tn], g.U2T.v(u2v[:, :, t0:t0 + tn]))
            for fc in range(8):
                k = pcnt % 2
                pcnt += 1
                pg = S.bank(2 * k)
                pu = S.bank(2 * k + 1)
                pgv = V(pg.ap[:, 0:tn], pg.bufs)
                puv = V(pu.ap[:, 0:tn], pu.bufs)
                for kc in range(KC):
                    S.mm(pgv, wg[:, kc, fc * 128:(fc + 1) * 128], u[:, kc, 0:tn], start=(kc == 0), stop=(kc == KC - 1))
                for kc in range(KC):
                    S.mm(puv, wu[:, kc, fc * 128:(fc + 1) * 128], u[:, kc, 0:tn], start=(kc == 0), stop=(kc == KC - 1))
                S.act(sg[k][:, 0:tn], pgv, AF.Silu)
                S.tt("dve", hT[:, fc, t0:t0 + tn], puv, sg[k][:, 0:tn], ALU.mult)
        if e + 1 < 32:
            load_gu(e + 1)
        for t in tiles:
            py = S.bank(4, 4)
            for n in range(4):
                for fc in range(8):
                    S.mm(V(py.ap[:, n * 512:(n + 1) * 512], py.bufs[n:n + 1]), hT[:, fc, t * 128:(t + 1) * 128],
                         wd[:, fc, n * 512:(n + 1) * 512], start=(fc == 0), stop=(fc == 7))
            y = ysb[ycnt % 2]
            ycnt += 1
            wsc = WR[:, t, e:e + 1]
            S.act(y[:, 0:1024], V(py.ap[:, 0:1024], py.bufs[0:2]), AF.Copy, scale=wsc)
            S.ts("dve", y[:, 1024:2048], V(py.ap[:, 1024:2048], py.bufs[2:4]), wsc, None, ALU.mult)
            if e == 0:
                S.dma("pool", g.YM.v(g.YM.t[t * 128:(t + 1) * 128, :]), y[:])
            else:
                S.dma("pool", g.YM.v(g.YM.t[t * 128:(t + 1) * 128, :]), y[:], accum_op=ALU.add)
        if e + 1 < 32:
            load_d(e + 1)
    S.barrier()
    S.arena_reset(base)


def phase_final(S, g, c, l, last):
    base = S.arena_off
    g2 = load_mod_tiles(S, g, l, 5, False)
    lg = bcast_tile(S, "ln2g", g.ln2g, g.ln2g.t[l])
    lb = bcast_tile(S, "ln2b", g.ln2b, g.ln2b.t[l])
    hs = [S.sb("fh%d" % i, [128, D], F32) for i in range(2)]
    ys = [S.sb("fy%d" % i, [128, D], F32) for i in range(2)]
    xn = [S.sb("fxn%d" % i, [128, D], F32) for i in range(2)]
    stats = S.sb("fstats", [128, 4, 6], F32)
    mv = S.sb("fmv", [128, 2], F32)
    rstd = S.sb("frstd", [128, 1], F32)
    nmr = S.sb("fnmr", [128, 1], F32)
    toks = []
    tiles = list(range(2, NT)) if last else list(range(NT))
    for t in tiles:
        row = 1 if t < 2 else 0
        h = hs[t % 2]
        y = ys[t % 2]
        x = xn[t % 2]
        S.dma("sp", h[:], g.H.v(g.H.t[t * 128:(t + 1) * 128, :]))
        S.dma("sp", y[:], g.YM.v(g.YM.t[t * 128:(t + 1) * 128, :]))
        S.tt("dve", y[:], y[:], g2[row][:], ALU.mult)
        S.stt("dve", y[:], h[:], float(ALPHA), y[:], ALU.mult, ALU.add)
        layer_norm_tile(S, c, y, x, stats, mv, rstd, nmr)
        S.tt("pool", x[:], x[:], lg[:], ALU.mult)
        S.tt("pool", x[:], x[:], lb[:], ALU.add)
        if last:
            toks.append(S.dma("sp", g.out.v(g.out.t[(t - 2) * 128:(t - 1) * 128, :]), x[:]))
        else:
            S.dma("sp", g.H.v(g.H.t[t * 128:(t + 1) * 128, :]), x[:])
    S.barrier()
    S.arena_reset(base)
    return toks


DBG_EXT = set()


def build_program(stop_after="all", dbg=()):
    global DBG_EXT
    DBG_EXT = set(dbg)
    nc = bass.Bass("TRN2", target_bir_lowering=False)
    S = Sched(nc)
    _orig_dram = S.dram

    def dram(name, shape, dt, kind="Internal"):
        if name in DBG_EXT and kind == "Internal":
            kind = "ExternalOutput"
        return _orig_dram(name, shape, dt, kind=kind)

    S.dram = dram
    stages = ["mod", "lnmod", "inproj", "ssdprep", "ssd", "attn", "merge", "moe", "all"]
    lim = stages.index(stop_after)
    g = declare_io(S, lim)
    declare_ssd_scratch(S, g)
    declare_merge_scratch(S, g)
    g.WRd = S.dram("WRd", [128, NT * 32], F32)
    c = load_consts(S, g)
    final = []
    for l in range(NL):
        phase_mod(S, g, c, l)
        if lim == 0:
            break
        base = S.arena_off
        UT = S.sb("UT", [128, KC, T], BF16)
        phase_ln_mod(S, g, c, l, UT, 0, 1, g.xin if l == 0 else g.H)
        if lim >= 2:
            phase_inproj(S, g, c, l, UT)
        S.arena_reset(base)
        if lim <= 2:
            break
        DTV = S.sb("DTV", [128, NT, 64], F32)
        ADT = S.sb("ADT", [128, NT, 64], F32)
        phase_ssdprep(S, g, c, l, DTV, ADT)
        if lim >= 4:
            phase_ssd_states(S, g, c, l, DTV, ADT)
            phase_ssd_y(S, g, c, l, DTV, ADT)
        S.arena_reset(base)
        if lim <= 4:
            break
        phase_attn(S, g, c, l, last=(l == NL - 1))
        if lim <= 5:
            break
        phase_merge(S, g, c, l)
        phase_outproj(S, g, c, l, g.xin if l == 0 else g.H)
        if lim <= 6:
            break
        WR = S.sb("WR", [128, NT, 32], F32)
        base2 = S.arena_off
        UT2 = S.sb("UT2", [128, KC, T], BF16)
        rt = router_setup(S, g, c, l, WR)
        phase_ln_mod(S, g, c, l, UT2, 3, 4, g.H, router=rt, flush=g.U2T)
        if "WRd" in DBG_EXT:
            S.dma("sp", g.WRd[:], V(WR.t[:, :, :].rearrange("p a b -> p (a b)"), [WR.buf]))
        S.arena_reset(base2)
        phase_moe(S, g, c, l, WR, last=(l == NL - 1))
        toks = phase_final(S, g, c, l, l == NL - 1)
        final.extend(toks)
        S.arena_reset(base)
        if lim <= 7:
            break
    if not final:
        z = S.sb("zout", [128, 64], F32)
        S.memset("dve", z[:], 0.0)
        final.append(S.dma("sp", g.out.v(g.out.t[0:128, 0:64]), z[:]))
    S.barrier()
    S.finish(final)
    return nc


def host_consts():
    nf = 16
    inv = (10000.0 ** (-(np.arange(nf, dtype=np.float32) / nf))).astype(np.float32)
    s = np.arange(2048)
    row = (s // 64).astype(np.float32)
    col = (s % 64).astype(np.float32)
    ang = np.concatenate([row[:, None] * inv[None, :], col[:, None] * inv[None, :]], axis=1).astype(np.float32)
    cos = np.ones((T, 32), np.float32)
    sin = np.zeros((T, 32), np.float32)
    cos[NCTX:] = np.cos(ang)
    sin[NCTX:] = np.sin(ang)
    k = np.arange(128)
    tri = np.stack([(k[:, None] <= k[None, :]), (k[:, None] >= k[None, :])]).astype(np.float32)
    negm = np.stack([np.where(k[None, :] >= k[:, None], 0.0, -30000.0),
                     np.where(k[None, :] <= k[:, None], 0.0, -30000.0)]).astype(np.float32)
    return dict(costab=cos, sintab=sin, identf=np.eye(128, dtype=np.float32), tri=tri, negm=negm)


def prep_core_inputs(inp, b, consts):
    f = np.ascontiguousarray
    m = {}
    m["xin"] = f(np.concatenate([inp["ctx"][b], inp["x"][b]], axis=0))
    cc = np.stack([inp["c"][b], inp["c_ctx"]], axis=1)
    m["cT"] = f(cc.reshape(KC, 128, 2).transpose(1, 0, 2).reshape(128, 32))
    m["w_ada"] = inp["w_ada"]
    m["b_ada"] = inp["b_ada"]
    m["w_in"] = inp["w_in"]
    m["convw"] = f(inp["conv_w"].reshape(NL, 5, 24, 128).transpose(0, 3, 2, 1).reshape(NL, 128, 120))
    m["convb"] = f(inp["conv_b"].reshape(NL, 24, 128).transpose(0, 2, 1))
    m["dtb"] = f(inp["ssm_dt_bias"].reshape(NL, 64))
    m["alog"] = f(inp["ssm_a_log"].reshape(NL, 64))
    m["ssmd"] = inp["ssm_d"]
    m["normg"] = inp["ssm_norm_g"]
    m["lamv"] = f(np.concatenate([inp["lam_q1"], inp["lam_k1"], inp["lam_q2"], inp["lam_k2"]], axis=1))
    m["subg"] = f(inp["attn_subln_g"].reshape(NL, 128, 1))
    for k_ in ("w_br_ssm", "w_br_att", "w_out"):
        m[k_] = inp[k_]
    m["ln1g"], m["ln1b"], m["ln2g"], m["ln2b"] = inp["ln1_g"], inp["ln1_b"], inp["ln2_g"], inp["ln2_b"]
    m["wr"] = f(np.concatenate([inp["w_router_group"], inp["w_router_expert"]], axis=2))
    m["br"] = f(np.concatenate([inp["b_router_group"], inp["b_router_expert"]], axis=1))
    m["w_eg"], m["w_eu"], m["w_ed"] = inp["w_exp_gate"], inp["w_exp_up"], inp["w_exp_down"]
    m.update(consts)
    return m


def kernel(**inputs):
    inp = {k: np.asarray(v) for k, v in inputs.items()}
    nc = build_program(stop_after="all")
    consts = host_consts()
    names = set()
    for a in nc.allocations:
        try:
            if a.kind == "ExternalInput":
                names.add(a.memorylocations[0].name)
        except Exception:
            pass
    in_maps = []
    for b in range(8):
        m = prep_core_inputs(inp, b, consts)
        in_maps.append({k: v for k, v in m.items() if k in names})
    res = run_bass_kernel_spmd(nc, in_maps, core_ids=list(range(8)))
    out = np.stack([np.asarray(res.results[b]["out"]) for b in range(8)], axis=0)
    return out.astype(np.float32)
```

```python
import numpy as np
import concourse.bass as bass
import concourse.mybir as mybir
from concourse.bass_utils import run_bass_kernel_spmd

F32 = mybir.dt.float32
BF16 = mybir.dt.bfloat16
I32 = mybir.dt.int32
AF = mybir.ActivationFunctionType
ALU = mybir.AluOpType
AX = mybir.AxisListType

NDS = 8


class Buf:
    __slots__ = ("name", "w", "r")

    def __init__(self, name=""):
        self.name = name
        self.w = None
        self.r = []


class V:
    __slots__ = ("ap", "bufs")

    def __init__(self, ap, bufs):
        self.ap = ap
        self.bufs = bufs


class Tl:
    def __init__(self, t, name=""):
        self.t = t
        self.buf = Buf(name)

    def __getitem__(self, idx):
        return V(self.t[idx], [self.buf])

    def v(self, ap):
        return V(ap, [self.buf])


class Sched:
    def __init__(self, nc):
        self.nc = nc
        self.prog = {e: [] for e in ("pe", "dve", "act", "pool", "sp")}
        self.sem = {}
        self.cnt = {}
        for e in ("pe", "dve", "act", "pool"):
            self.sem[e] = nc.alloc_semaphore("s_" + e)
            self.cnt[e] = 0
        self.dq = {}
        for q in ("sp", "act", "pool"):
            ks = []
            for i in range(NDS):
                k = "d_%s%d" % (q, i)
                self.sem[k] = nc.alloc_semaphore(k)
                self.cnt[k] = 0
                ks.append(k)
            self.dq[q] = [ks, 0]
        self.seen = {e: {} for e in self.prog}
        self.ninst = 0
        self.arena_off = 16512
        self.nalloc = 0
        self.psum_t = nc.alloc_psum_tensor("psum_all", [128, 8 * 512], F32)
        self.psum_bufs = [Buf("bank%d" % i) for i in range(8)]

    def arena_reset(self, off=16512):
        self.arena_off = off

    def sb(self, name, shape, dt):
        esz = 2 if dt == BF16 else 4
        n = 1
        for s in shape[1:]:
            n *= s
        nbytes = (n * esz + 63) // 64 * 64
        off = self.arena_off
        assert off + nbytes <= 229344, ("SBUF arena overflow", name, off, nbytes)
        self.arena_off = off + nbytes
        self.nalloc += 1
        t = self.nc.alloc_sbuf_tensor_at("%s_%d" % (name, self.nalloc), list(shape), dt, offset=off)
        return Tl(t, name)

    def bank(self, i, n=1, dt=F32):
        ap = self.psum_t[:, i * 512:(i + n) * 512]
        if dt == BF16:
            ap = ap.bitcast(BF16)
        return V(ap, self.psum_bufs[i:i + n])

    def barrier(self):
        toks = [(k, v) for k, v in self.cnt.items() if v > 0]
        for e in self.prog:
            need = [(k, v) for (k, v) in toks if self.seen[e].get(k, 0) < v and not (k == e)]
            for k, v in need:
                self.seen[e][k] = v
            sem = self.sem

            def emit(eng, need=need):
                for k, v in need:
                    eng.wait_ge(sem[k], v)

            self.prog[e].append(emit)


    def dram(self, name, shape, dt, kind="Internal"):
        return Tl(self.nc.dram_tensor(name, list(shape), dt, kind=kind).ap(), name)

    def _deps(self, e, reads, writes):
        need = {}

        def add(tok):
            if tok is None:
                return
            k, v = tok
            if k == "pe" and e == "pe":
                return
            if self.seen[e].get(k, 0) >= v:
                return
            if need.get(k, 0) < v:
                need[k] = v

        for b in reads:
            add(b.w)
        for b in writes:
            add(b.w)
            for t in b.r:
                add(t)
        for k, v in need.items():
            self.seen[e][k] = v
        return list(need.items())

    def _record(self, tok, reads, writes):
        for b in writes:
            b.w = tok
            b.r = []
        for b in reads:
            if b in writes:
                continue
            b.r.append(tok)
            if len(b.r) > 24:
                mx = {}
                for k, v in b.r:
                    if mx.get(k, 0) < v:
                        mx[k] = v
                b.r = list(mx.items())

    def op(self, e, fn, reads, writes):
        rb = [b for x in reads for b in x.bufs]
        wb = [b for x in writes for b in x.bufs]
        waits = self._deps(e, rb, wb)
        self.cnt[e] += 1
        tok = (e, self.cnt[e])
        sem = self.sem
        se = sem[e]

        def emit(eng, waits=waits, fn=fn, se=se):
            for k, v in waits:
                eng.wait_ge(sem[k], v)
            fn(eng).then_inc(se, 1)

        self.prog[e].append(emit)
        self._record(tok, rb, wb)
        self.ninst += 1
        return tok

    def dma(self, q, out, in_, **kw):
        rb = list(in_.bufs)
        wb = list(out.bufs)
        waits = self._deps(q, rb, wb)
        ks, rr = self.dq[q]
        k = ks[rr % NDS]
        self.dq[q][1] = rr + 1
        if self.cnt[k] > 0 and self.seen[q].get(k, 0) < self.cnt[k]:
            waits = [w for w in waits if w[0] != k] + [(k, self.cnt[k])]
            self.seen[q][k] = self.cnt[k]
        self.cnt[k] += 16
        tok = (k, self.cnt[k])
        sem = self.sem
        oap, iap = out.ap, in_.ap

        def emit(eng, waits=waits, sk=sem[k]):
            for kk, v in waits:
                eng.wait_ge(sem[kk], v)
            eng.dma_start(out=oap, in_=iap, **kw).then_inc(sk, 16)

        self.prog[q].append(emit)
        self._record(tok, rb, wb)
        self.ninst += 1
        return tok

    def wait_all(self, e, toks):
        sem = self.sem
        toks = [t for t in toks if t is not None]

        def emit(eng):
            for k, v in toks:
                eng.wait_ge(sem[k], v)

        self.prog[e].append(emit)

    def mm(self, out, lhsT, rhs, start=True, stop=True, extra_reads=()):
        return self.op("pe", lambda eng: eng.matmul(out.ap, lhsT.ap, rhs.ap, start=start, stop=stop),
                       [lhsT, rhs] + list(extra_reads), [out])

    def tr(self, out, in_, ident):
        return self.op("pe", lambda eng: eng.transpose(out.ap, in_.ap, ident.ap), [in_, ident], [out])

    def act(self, out, in_, func, bias=None, scale=1.0, accum_out=None, e="act"):
        reads = [in_]
        kw = {}
        if bias is not None:
            if isinstance(bias, V):
                reads.append(bias)
                kw["bias"] = bias.ap
            else:
                kw["bias"] = bias
        if isinstance(scale, V):
            reads.append(scale)
            kw["scale"] = scale.ap
        else:
            kw["scale"] = scale
        writes = [out]
        if accum_out is not None:
            writes.append(accum_out)
            kw["accum_out"] = accum_out.ap
        return self.op("act", lambda eng: eng.activation(out.ap, in_.ap, func, **kw), reads, writes)

    def tt(self, e, out, a, b, op):
        return self.op(e, lambda eng: eng.tensor_tensor(out.ap, a.ap, b.ap, op), [a, b], [out])

    def ts(self, e, out, a, s1, s2, op0, op1=None, accum_out=None):
        reads = [a]
        s1a = s1.ap if isinstance(s1, V) else s1
        s2a = s2.ap if isinstance(s2, V) else s2
        if isinstance(s1, V):
            reads.append(s1)
        if isinstance(s2, V):
            reads.append(s2)
        writes = [out]
        kw = {}
        if accum_out is not None:
            writes.append(accum_out)
            kw["accum_out"] = accum_out.ap
        if op1 is None:
            return self.op(e, lambda eng: eng.tensor_scalar(out.ap, a.ap, s1a, None, op0, **kw), reads, writes)
        return self.op(e, lambda eng: eng.tensor_scalar(out.ap, a.ap, s1a, s2a, op0, op1, **kw), reads, writes)

    def stt(self, e, out, a, s, b, op0, op1):
        reads = [a, b]
        sa = s.ap if isinstance(s, V) else s
        if isinstance(s, V):
            reads.append(s)
        return self.op(e, lambda eng: eng.scalar_tensor_tensor(out.ap, a.ap, sa, b.ap, op0, op1), reads, [out])

    def copy(self, e, out, in_):
        if e == "act":
            return self.op("act", lambda eng: eng.copy(out.ap, in_.ap), [in_], [out])
        return self.op(e, lambda eng: eng.tensor_copy(out.ap, in_.ap), [in_], [out])

    def memset(self, e, out, val):
        return self.op(e, lambda eng: eng.memset(out.ap, val), [], [out])

    def finish(self, final_tokens):
        nc = self.nc
        self.wait_all("sp", final_tokens)
        prog = self.prog
        with nc.Block() as blk:
            @blk.sync
            def _(eng):
                for f in prog["sp"]:
                    f(eng)

            @blk.tensor
            def _(eng):
                for f in prog["pe"]:
                    f(eng)

            @blk.vector
            def _(eng):
                for f in prog["dve"]:
                    f(eng)

            @blk.scalar
            def _(eng):
                for f in prog["act"]:
                    f(eng)

            @blk.gpsimd
            def _(eng):
                for f in prog["pool"]:
                    f(eng)


NL = 2
T = 2304
D = 2048
NT = 18
KC = 16
NCTX = 256
IN_COLS = 15424
OFF_Z, OFF_XBC, OFF_DT, OFF_Q, OFF_K, OFF_V, OFF_G = 0, 2048, 5120, 5184, 7232, 9280, 11328
LN_EPS = 1e-6
RMS_EPS = 1e-5
ALPHA = (2.0 * NL) ** 0.25
TG = [(0, 512), (512, 512), (1024, 512), (1536, 512), (2048, 256)]


def bcast_rows(ap1d, n=128):
    return ap1d.partition_broadcast(n)


class Ctx:
    pass


def declare_io(S, lim=99):
    nc = S.nc
    g = Ctx()
    need = {"w_br_ssm": 6, "w_br_att": 6, "w_out": 6, "w_eg": 7, "w_eu": 7, "w_ed": 7, "w_in": 2}

    def inp(name, shape, dt=F32):
        if need.get(name, 0) > lim:
            return None
        return S.dram(name, shape, dt, kind="ExternalInput")

    g.xin = inp("xin", [T, D])
    g.cT = inp("cT", [128, 32])
    g.w_ada = inp("w_ada", [NL, D, 6 * D])
    g.b_ada = inp("b_ada", [NL, 6 * D])
    g.w_in = inp("w_in", [NL, D, IN_COLS])
    g.convw = inp("convw", [NL, 128, 24 * 5])
    g.convb = inp("convb", [NL, 128, 24])
    g.dtb = inp("dtb", [NL, 64])
    g.alog = inp("alog", [NL, 64])
    g.ssmd = inp("ssmd", [NL, 32])
    g.normg = inp("normg", [NL, D])
    g.lamv = inp("lamv", [NL, 4 * 64])
    g.subg = inp("subg", [NL, 128, 1])
    g.w_br_ssm = inp("w_br_ssm", [NL, D, D])
    g.w_br_att = inp("w_br_att", [NL, D, D])
    g.w_out = inp("w_out", [NL, D, D])
    g.ln1g = inp("ln1g", [NL, D])
    g.ln1b = inp("ln1b", [NL, D])
    g.ln2g = inp("ln2g", [NL, D])
    g.ln2b = inp("ln2b", [NL, D])
    g.wr = inp("wr", [NL, D, 36])
    g.br = inp("br", [NL, 36])
    g.w_eg = inp("w_eg", [NL, 32, D, 1024])
    g.w_eu = inp("w_eu", [NL, 32, D, 1024])
    g.w_ed = inp("w_ed", [NL, 32, 1024, D])
    g.costab = inp("costab", [T, 32])
    g.sintab = inp("sintab", [T, 32])
    g.identf = inp("identf", [128, 128])
    g.tri = inp("tri", [2, 128, 128])
    g.negm = inp("negm", [2, 128, 128])
    g.out = S.dram("out", [2048, D], F32, kind="ExternalOutput")
    g.MOD = S.dram("MOD", [NL, 2, 6 * D], F32)
    g.H = S.dram("H", [T, D], F32)
    g.Z = S.dram("Z", [T, D], F32)
    g.XBCT = S.dram("XBCT", [3072, T], F32)
    g.DTR = S.dram("DTR", [T, 64], F32)
    g.QT = S.dram("QT", [16, 128, T], BF16)
    g.QN = S.dram("QN", [16, 128, T], BF16)
    g.KT = S.dram("KT", [16, 128, T], BF16)
    g.Vd = S.dram("Vd", [T, D], BF16)
    g.GT = S.dram("GT", [4096, T], F32)
    g.dbg = {}
    return g


def load_consts(S, g):
    c = Ctx()
    c.identf = S.sb("identf", [128, 128], F32)
    c.identb = S.sb("identb", [128, 128], BF16)
    c.ones_b = S.sb("ones_b", [128, 128], BF16)
    c.ones_f = S.sb("ones_f", [128, 128], F32)
    S.dma("sp", c.identf[:], g.identf[:])
    S.copy("dve", c.identb[:], c.identf[:])
    S.memset("dve", c.ones_b[:], 1.0)
    S.memset("dve", c.ones_f[:], 1.0)
    c.eps_ln = S.sb("eps_ln", [128, 1], F32)
    c.eps_rms = S.sb("eps_rms", [128, 1], F32)
    S.memset("dve", c.eps_ln[:], LN_EPS)
    S.memset("dve", c.eps_rms[:], RMS_EPS)
    return c


def phase_mod(S, g, c, l):
    base = S.arena_off
    cT = S.sb("cT", [128, 32], F32)
    scT = S.sb("scT", [128, 32], BF16)
    S.dma("sp", cT[:], g.cT[:])
    S.act(scT[:], cT[:], AF.Silu)
    wb = [S.sb("wada%d" % i, [128, KC, 512], BF16) for i in range(2)]
    bb = [S.sb("bada%d" % i, [2, 512], F32) for i in range(2)]
    ob = [S.sb("oada%d" % i, [2, 512], F32) for i in range(2)]
    wv = g.w_ada.t[l].rearrange("(kc p) n -> p kc n", p=128)
    for nb in range(24):
        w = wb[nb % 2]
        S.dma("pool", w[:], g.w_ada.v(wv[:, :, nb * 512:(nb + 1) * 512]))
        S.dma("sp", bb[nb % 2][:], g.b_ada.v(g.b_ada.t[l, nb * 512:(nb + 1) * 512].partition_broadcast(2)))
        ps = S.bank(nb % 2)
        pv = V(ps.ap[0:2, :], ps.bufs)
        for kc in range(KC):
            S.mm(pv, scT[:, kc * 2:(kc + 1) * 2], w[:, kc, :], start=(kc == 0), stop=(kc == KC - 1))
        S.tt("dve", ob[nb % 2][:], pv, bb[nb % 2][:], ALU.add)
        S.dma("sp", g.MOD.v(g.MOD.t[l, :, nb * 512:(nb + 1) * 512]), ob[nb % 2][:])
    S.barrier()
    S.arena_reset(base)


def load_mod_tiles(S, g, l, seg, plus_one):
    res = []
    for row in range(2):
        t = S.sb("mod%d_%d" % (seg, row), [128, D], F32)
        S.dma("sp", t[:], g.MOD.v(g.MOD.t[l, row, seg * D:(seg + 1) * D].partition_broadcast(128)))
        if plus_one:
            S.ts("pool", t[:], t[:], 1.0, None, ALU.add)
        res.append(t)
    return res


def layer_norm_tile(S, c, xt, xn, stats, mv, rstd, nmr):
    for j in range(4):
        S.op("dve", lambda e, j=j: e.bn_stats(stats.t[:, j, :], xt.t[:, j * 512:(j + 1) * 512]),
             [xt[:]], [stats[:]])
    S.op("dve", lambda e: e.bn_aggr(mv.t[:], stats.t[:]), [stats[:]], [mv[:]])
    S.act(rstd[:], mv[:, 1:2], AF.Sqrt, bias=c.eps_ln[:])
    S.op("dve", lambda e: e.reciprocal(rstd.t[:], rstd.t[:]), [rstd[:]], [rstd[:]])
    S.ts("dve", nmr[:], mv[:, 0:1], -1.0, None, ALU.mult)
    S.tt("dve", nmr[:], nmr[:], rstd[:], ALU.mult)
    S.act(xn[:], xt[:], AF.Identity, bias=nmr[:], scale=rstd[:])


def phase_ln_mod(S, g, c, l, UT, seg_shift, seg_scale, src, router=None, flush=None):
    base = S.arena_off
    sc = load_mod_tiles(S, g, l, seg_scale, True)
    sh = load_mod_tiles(S, g, l, seg_shift, False)
    xts = [S.sb("xt%d" % i, [128, D], F32) for i in range(2)]
    xn = S.sb("xn", [128, D], F32)
    ub = S.sb("ub", [128, D], BF16)
    stats = S.sb("stats", [128, 4, 6], F32)
    mv = S.sb("mv", [128, 2], F32)
    rstd = S.sb("rstd", [128, 1], F32)
    nmr = S.sb("nmr", [128, 1], F32)
    S.dma("sp", xts[0][:], src.v(src.t[0:128, :]))
    for t in range(NT):
        xt = xts[t % 2]
        if t + 1 < NT:
            S.dma("sp", xts[(t + 1) % 2][:], src.v(src.t[(t + 1) * 128:(t + 2) * 128, :]))
        row = 1 if t < 2 else 0
        layer_norm_tile(S, c, xt, xn, stats, mv, rstd, nmr)
        S.tt("dve", xn[:], xn[:], sc[row][:], ALU.mult)
        S.tt("pool", xn[:], xn[:], sh[row][:], ALU.add)
        S.copy("act", ub[:], xn[:])
        if router is not None:
            router_tile(S, g, c, router, xn, t)
        for j in range(4):
            pb = S.bank(6 + (j % 2), dt=BF16)
            for i in range(4):
                kc = j * 4 + i
                S.tr(V(pb.ap[:, i * 128:(i + 1) * 128], pb.bufs), ub[:, kc * 128:(kc + 1) * 128], c.identb[:])
            src_v = V(pb.ap[:, 0:512].rearrange("p (a b) -> p a b", a=4), pb.bufs)
            dst_v = UT[:, j * 4:(j + 1) * 4, t * 128:(t + 1) * 128]
            S.copy("act" if j % 2 == 0 else "dve", dst_v, src_v)
    if flush is not None:
        for kc in range(KC):
            S.dma("sp", flush.v(flush.t[kc * 128:(kc + 1) * 128, :]), UT[:, kc, :])
    S.barrier()
    S.arena_reset(base)


def phase_inproj(S, g, c, l, UT):
    base = S.arena_off
    wbuf = [S.sb("win%d" % i, [128, KC, 512], BF16) for i in range(2)]
    stg = [S.sb("stg%d" % i, [128, 512], F32) for i in range(3)]
    stgb = [S.sb("stgb%d" % i, [128, 512], BF16) for i in range(2)]
    fstg = [S.sb("fstg%d" % i, [128, T], F32) for i in range(2)]
    rot = S.sb("rot", [128, 512], BF16)
    unr = S.sb("unr", [128, 512], BF16)
    tmp = [S.sb("rt%d" % i, [128, 256], F32) for i in range(4)]
    hst = S.sb("hst", [128, 4, T], BF16)
    hsn = S.sb("hsn", [128, 4, T], BF16)
    cosb = S.sb("cosb", [128, NT, 32], F32)
    sinb = S.sb("sinb", [128, NT, 32], F32)
    S.dma("sp", cosb[:], g.costab.v(g.costab.t.rearrange("(t p) f -> p t f", p=128)))
    S.dma("sp", sinb[:], g.sintab.v(g.sintab.t.rearrange("(t p) f -> p t f", p=128)))
    wv = g.w_in.t[l].rearrange("(kc p) n -> p kc n", p=128)

    blocks = []
    for i in range(4):
        blocks.append(("z", OFF_Z + i * 512, 512, i))
    for i in range(6):
        blocks.append(("xbc", OFF_XBC + i * 512, 512, i))
    blocks.append(("dt", OFF_DT, 64, 0))
    for i in range(4):
        blocks.append(("q", OFF_Q + i * 512, 512, i))
    for i in range(4):
        blocks.append(("qn", OFF_Q + i * 512, 512, i))
    for i in range(4):
        blocks.append(("k", OFF_K + i * 512, 512, i))
    for i in range(4):
        blocks.append(("v", OFF_V + i * 512, 512, i))
    for i in range(8):
        blocks.append(("g", OFF_G + i * 512, 512, i))

    def load_w(bi):
        kind, c0, ncol, i = blocks[bi]
        w = wbuf[bi % 2]
        S.dma("pool", w[:, :, 0:ncol], g.w_in.v(wv[:, :, c0:c0 + ncol]))

    load_w(0)
    pcnt = 0
    scnt = 0
    fcnt = 0
    for bi, (kind, c0, ncol, i) in enumerate(blocks):
        if bi + 1 < len(blocks):
            load_w(bi + 1)
        w = wbuf[bi % 2]
        if kind in ("z", "dt", "q", "qn", "k", "v"):
            for t in range(NT):
                ps = S.bank(pcnt % 6)
                pcnt += 1
                pv = V(ps.ap[:, 0:ncol], ps.bufs)
                for kc in range(KC):
                    S.mm(pv, UT[:, kc, t * 128:(t + 1) * 128], w[:, kc, 0:ncol], start=(kc == 0), stop=(kc == KC - 1))
                if kind == "z":
                    s = stg[scnt % 3]
                    scnt += 1
                    S.copy("act" if t % 2 == 0 else "dve", s[:], pv)
                    S.dma("sp", g.Z.v(g.Z.t[t * 128:(t + 1) * 128, i * 512:(i + 1) * 512]), s[:])
                elif kind == "dt":
                    s = stg[scnt % 3]
                    scnt += 1
                    S.copy("act", s[:, 0:64], pv)
                    S.dma("sp", g.DTR.v(g.DTR.t[t * 128:(t + 1) * 128, :]), s[:, 0:64])
                elif kind == "v":
                    s = stgb[t % 2]
                    S.copy("act" if t % 2 == 0 else "dve", s[:], pv)
                    S.dma("sp", g.Vd.v(g.Vd.t[t * 128:(t + 1) * 128, i * 512:(i + 1) * 512]), s[:])
                else:
                    pr = ps.ap[:, 0:512].rearrange("p (a h u f) -> p a h u f", a=8, h=2, u=2)
                    u1 = V(pr[:, :, :, 0, :], ps.bufs)
                    u2 = V(pr[:, :, :, 1, :], ps.bufs)
                    tt_ = 0 if kind == "qn" else t
                    cosv = V(cosb.t[:, tt_, :].rearrange("p (h f) -> p h f", h=2).unsqueeze(1).to_broadcast([128, 8, 2, 16]), [cosb.buf])
                    sinv = V(sinb.t[:, tt_, :].rearrange("p (h f) -> p h f", h=2).unsqueeze(1).to_broadcast([128, 8, 2, 16]), [sinb.buf])
                    rr = rot.t[:, :].rearrange("p (a h u f) -> p a h u f", a=8, h=2, u=2)
                    o1 = V(rr[:, :, :, 0, :], [rot.buf])
                    o2 = V(rr[:, :, :, 1, :], [rot.buf])
                    tv = [V(x.t[:, :].rearrange("p (a h f) -> p a h f", a=8, h=2), [x.buf]) for x in tmp]
                    S.tt("dve", tv[0], u1, cosv, ALU.mult)
                    S.tt("dve", tv[1], u2, sinv, ALU.mult)
                    S.tt("pool", o1, tv[0], tv[1], ALU.subtract)
                    S.tt("dve", tv[2], u1, sinv, ALU.mult)
                    S.tt("dve", tv[3], u2, cosv, ALU.mult)
                    S.tt("pool", o2, tv[2], tv[3], ALU.add)
                    srcs = [(rot, hst)]
                    for si, (srct, dstt) in enumerate(srcs):
                        pb = S.bank(6 + si, dt=BF16)
                        for hh in range(4):
                            S.tr(V(pb.ap[:, hh * 128:(hh + 1) * 128], pb.bufs), srct[:, hh * 128:(hh + 1) * 128], c.identb[:])
                        S.copy("act", dstt[:, :, t * 128:(t + 1) * 128],
                               V(pb.ap[:, 0:512].rearrange("p (a b) -> p a b", a=4), pb.bufs))
            if kind in ("q", "k", "qn"):
                dst = {"q": g.QT, "k": g.KT, "qn": g.QN}[kind]
                for hh in range(4):
                    S.dma("sp", dst.v(dst.t[i * 4 + hh]), hst[:, hh, :])
        else:
            for m in range(4):
                fs = fstg[fcnt % 2]
                fcnt += 1
                for (t0, tn) in TG:
                    ps = S.bank(pcnt % 6)
                    pcnt += 1
                    pv = V(ps.ap[:, 0:tn], ps.bufs)
                    for kc in range(KC):
                        S.mm(pv, w[:, kc, m * 128:(m + 1) * 128], UT[:, kc, t0:t0 + tn], start=(kc == 0), stop=(kc == KC - 1))
                    if kind == "g":
                        S.act(fs[:, t0:t0 + tn], pv, AF.Sigmoid)
                    else:
                        S.copy("dve" if (pcnt % 2) else "act", fs[:, t0:t0 + tn], pv)
                dst = g.GT if kind == "g" else g.XBCT
                r0 = i * 512 + m * 128
                S.dma("sp", dst.v(dst.t[r0:r0 + 128, :]), fs[:])
    S.barrier()
    S.arena_reset(base)


def declare_ssd_scratch(S, g):
    g.Xtok = S.dram("Xtok", [T, D], BF16)
    g.Btok = S.dram("Btok", [T, 512], BF16)
    g.BT = S.dram("BT", [512, T], BF16)
    g.CT = S.dram("CT", [512, T], BF16)
    g.ST = S.dram("ST", [2 * NT * 128, D], BF16)
    g.YST = S.dram("YST", [D, T], BF16)
    g.YAT = S.dram("YAT", [D, T], BF16)
    g.YDBG = S.dram("YDBG", [T, D], F32)
    g.DTVd = S.dram("DTVd", [128, NT * 64], F32)
    g.ADTd = S.dram("ADTd", [128, NT * 64], F32)


def phase_ssdprep(S, g, c, l, DTV, ADT):
    base = S.arena_off
    XC = S.sb("XC", [128, 20, T], BF16)
    cw = S.sb("convw", [128, 120], F32)
    cb = S.sb("convb", [128, 24], F32)
    S.dma("sp", cw[:], g.convw.v(g.convw.t[l]))
    S.dma("sp", cb[:], g.convb.v(g.convb.t[l]))
    xin = [S.sb("cin%d" % i, [128, T], F32) for i in range(2)]
    acc = S.sb("cacc", [128, T], F32)
    cstg = [S.sb("cstg%d" % i, [128, T], BF16) for i in range(2)]
    S.dma("sp", xin[0][:], g.XBCT.v(g.XBCT.t[0:128, :]))
    segs = [(0, NCTX), (NCTX, T)]
    for cc in range(24):
        xi = xin[cc % 2]
        if cc + 1 < 24:
            S.dma("sp", xin[(cc + 1) % 2][:], g.XBCT.v(g.XBCT.t[(cc + 1) * 128:(cc + 2) * 128, :]))
        S.act(acc[:], xi[:], AF.Identity, bias=cb[:, cc:cc + 1], scale=cw[:, cc * 5 + 2:cc * 5 + 3])
        for j in (0, 1, 3, 4):
            s = j - 2
            for (a, b) in segs:
                lo = max(a, a - s)
                hi = min(b, b - s)
                S.stt("dve", acc[:, lo:hi], xi[:, lo + s:hi + s], cw[:, cc * 5 + j:cc * 5 + j + 1],
                      acc[:, lo:hi], ALU.mult, ALU.add)
        if cc < 20:
            S.act(XC[:, cc, :], acc[:], AF.Silu)
            if cc >= 16:
                S.dma("sp", g.BT.v(g.BT.t[(cc - 16) * 128:(cc - 15) * 128, :]), XC[:, cc, :])
        else:
            st = cstg[cc % 2]
            S.act(st[:], acc[:], AF.Silu)
            S.dma("sp", g.CT.v(g.CT.t[(cc - 20) * 128:(cc - 19) * 128, :]), st[:])
    xt = [S.sb("xtok%d" % i, [128, 2560], BF16) for i in range(2)]
    for t in range(NT):
        o = xt[t % 2]
        for j in range(5):
            pb = S.bank(6 + (j % 2), dt=BF16)
            for i in range(4):
                S.tr(V(pb.ap[:, i * 128:(i + 1) * 128], pb.bufs), XC[:, j * 4 + i, t * 128:(t + 1) * 128], c.identb[:])
            S.copy("act" if j % 2 == 0 else "dve", o[:, j * 512:(j + 1) * 512], V(pb.ap[:, 0:512], pb.bufs))
        S.dma("sp", g.Xtok.v(g.Xtok.t[t * 128:(t + 1) * 128, :]), o[:, 0:2048])
        S.dma("sp", g.Btok.v(g.Btok.t[t * 128:(t + 1) * 128, :]), o[:, 2048:2560])
    dtr = S.sb("dtr", [128, NT, 64], F32)
    tb = S.sb("dtbb", [128, 64], F32)
    al = S.sb("alb", [128, 64], F32)
    t1 = S.sb("dt1", [128, NT, 64], F32)
    S.dma("sp", dtr[:], g.DTR.v(g.DTR.t.rearrange("(t p) f -> p t f", p=128)))
    S.dma("sp", tb[:], g.dtb.v(g.dtb.t[l].partition_broadcast(128)))
    S.dma("sp", al[:], g.alog.v(g.alog.t[l].partition_broadcast(128)))
    tbb = V(tb.t[:, :].unsqueeze(1).to_broadcast([128, NT, 64]), [tb.buf])
    S.tt("dve", dtr[:], dtr[:], tbb, ALU.add)
    S.act(t1[:], dtr[:], AF.Abs)
    S.act(t1[:], t1[:], AF.Exp, scale=-1.0)
    S.act(t1[:], t1[:], AF.Ln, bias=c.ones_f[:, 0:1])
    S.stt("dve", DTV[:], dtr[:], 0.0, t1[:], ALU.max, ALU.add)
    S.act(al[:], al[:], AF.Exp)
    S.ts("dve", al[:], al[:], -1.0, None, ALU.mult)
    alb = V(al.t[:, :].unsqueeze(1).to_broadcast([128, NT, 64]), [al.buf])
    S.tt("dve", ADT[:], DTV[:], alb, ALU.mult)
    if "DTVd" in DBG_EXT:
        S.dma("sp", g.DTVd[:], V(DTV.t[:, :, :].rearrange("p a b -> p (a b)"), [DTV.buf]))
        S.dma("sp", g.ADTd[:], V(ADT.t[:, :, :].rearrange("p a b -> p (a b)"), [ADT.buf]))
    S.barrier()
    S.arena_reset(base)


def phase_ssd_states(S, g, c, l, DTV, ADT):
    base = S.arena_off
    tri = [S.sb("tri%d" % d, [128, 128], F32) for d in range(2)]
    for d in range(2):
        S.dma("sp", tri[d][:], g.tri.v(g.tri.t[d]))
    St = S.sb("Sstate", [128, D], F32)
    Sb = [S.sb("Sbf%d" % i, [128, D], BF16) for i in range(2)]
    xs = [S.sb("xs%d" % i, [128, D], BF16) for i in range(2)]
    bs = [S.sb("bs%d" % i, [128, 512], BF16) for i in range(2)]
    xdd = S.sb("xdd", [128, D], BF16)
    cs = S.sb("cs", [128, 32], F32)
    df = S.sb("df", [128, 32], F32)
    dte = S.sb("dte", [128, 32], F32)
    dch = S.sb("dch", [128, 32], F32)
    it = 0
    for d in range(2):
        order = list(range(NT)) if d == 0 else [1, 0] + list(range(NT - 1, 1, -1))
        S.memset("pool", St[:], 0.0)
        for ch in order:
            x = xs[it % 2]
            b = bs[it % 2]
            sb_ = Sb[it % 2]
            it += 1
            S.dma("sp", x[:], g.Xtok.v(g.Xtok.t[ch * 128:(ch + 1) * 128, :]))
            S.dma("sp", b[:], g.Btok.v(g.Btok.t[ch * 128:(ch + 1) * 128, :]))
            adt = ADT[:, ch, d * 32:(d + 1) * 32]
            pm = S.bank(4)
            pcs = V(pm.ap[:, 0:32], pm.bufs)
            pend = V(pm.ap[:, 32:64], pm.bufs)
            S.mm(pcs, tri[d][:], adt)
            S.mm(pend, c.ones_f[:], adt)
            S.copy("act", cs[:], pcs)
            S.tt("dve", df[:], pend, cs[:], ALU.subtract)
            S.act(df[:], df[:], AF.Exp)
            S.tt("dve", dte[:], df[:], DTV[:, ch, d * 32:(d + 1) * 32], ALU.mult)
            S.act(dch[:], pend, AF.Exp)
            xv = V(x.t[:, :].rearrange("p (h q) -> p h q", h=32), [x.buf])
            xo = V(xdd.t[:, :].rearrange("p (h q) -> p h q", h=32), [xdd.buf])
            S.tt("dve", xo, xv, V(dte.t[:, :].unsqueeze(2).to_broadcast([128, 32, 64]), [dte.buf]), ALU.mult)
            S.copy("act", sb_[:], St[:])
            r0 = (d * NT + ch) * 128
            S.dma("sp", g.ST.v(g.ST.t[r0:r0 + 128, :]), sb_[:])
            ps = S.bank(0, 4)
            for gi in range(4):
                S.mm(V(ps.ap[:, gi * 512:(gi + 1) * 512], ps.bufs[gi:gi + 1]), b[:, gi * 128:(gi + 1) * 128], xdd[:, gi * 512:(gi + 1) * 512])
            sv = V(St.t[:, :].rearrange("p (h q) -> p h q", h=32), [St.buf])
            S.tt("pool", sv, sv, V(dch.t[:, :].unsqueeze(2).to_broadcast([128, 32, 64]), [dch.buf]), ALU.mult)
            S.tt("dve", St[:], St[:], ps, ALU.add)
    S.barrier()
    S.arena_reset(base)


def phase_ssd_y(S, g, c, l, DTV, ADT):
    base = S.arena_off
    YST = S.sb("YSTres", [128, KC, T], BF16)
    tri = [S.sb("tri%d" % d, [128, 128], F32) for d in range(2)]
    negm = [S.sb("negm%d" % d, [128, 128], F32) for d in range(2)]
    for d in range(2):
        S.dma("sp", tri[d][:], g.tri.v(g.tri.t[d]))
        S.dma("sp", negm[d][:], g.negm.v(g.negm.t[d]))
    BT = S.sb("BTres", [128, 4, T], BF16)
    CT = S.sb("CTres", [128, 4, T], BF16)
    for gi in range(4):
        S.dma("sp", BT[:, gi, :], g.BT.v(g.BT.t[gi * 128:(gi + 1) * 128, :]))
        S.dma("sp", CT[:, gi, :], g.CT.v(g.CT.t[gi * 128:(gi + 1) * 128, :]))
    dsk = S.sb("dsk", [128, 32], F32)
    S.dma("sp", dsk[:], g.ssmd.v(g.ssmd.t[l].partition_broadcast(128)))
    ng = S.sb("normg", [128, D], F32)
    S.dma("sp", ng[:], g.normg.v(g.normg.t[l].partition_broadcast(128)))
    xs = [S.sb("yx%d" % i, [128, D], BF16) for i in range(2)]
    sts = [[S.sb("yst%d_%d" % (d, i), [128, D], BF16) for i in range(2)] for d in range(2)]
    zs = [S.sb("yz%d" % i, [128, D], F32) for i in range(2)]
    xdt = [S.sb("xdt%d" % d, [128, D], BF16) for d in range(2)]
    cs = S.sb("ycs", [128, 64], F32)
    ecs = S.sb("yecs", [128, 64], F32)
    CBT = S.sb("CBT", [128, 4, 128], F32)
    R = [S.sb("R%d" % i, [128, 4, 128], F32) for i in range(2)]
    Dm = [S.sb("Dm%d" % i, [128, 4, 128], F32) for i in range(2)]
    MT = [S.sb("MT%d" % i, [128, 4, 128], BF16) for i in range(2)]
    yacc = S.sb("yacc", [128, D], F32)
    t2 = S.sb("yt2", [128, 1024], F32)
    ybf = S.sb("ybf", [128, D], BF16)
    ss = S.sb("yss", [128, 4], F32)
    junk = S.sb("yjunk", [128, 512], F32)

    def loads(ch, i):
        S.dma("sp", xs[i][:], g.Xtok.v(g.Xtok.t[ch * 128:(ch + 1) * 128, :]))
        for d in range(2):
            r0 = (d * NT + ch) * 128
            S.dma("sp", sts[d][i][:], g.ST.v(g.ST.t[r0:r0 + 128, :]))
        S.dma("sp", zs[i][:], g.Z.v(g.Z.t[ch * 128:(ch + 1) * 128, :]))

    loads(0, 0)
    gcnt = 0
    for ch in range(NT):
        i = ch % 2
        if ch + 1 < NT:
            loads(ch + 1, (ch + 1) % 2)
        x = xs[i]
        z = zs[i]
        tsl = slice(ch * 128, (ch + 1) * 128)
        pm = S.bank(6)
        for d in range(2):
            S.mm(V(pm.ap[:, d * 32:(d + 1) * 32], pm.bufs), tri[d][:], ADT[:, ch, d * 32:(d + 1) * 32])
        S.copy("act", cs[:], V(pm.ap[:, 0:64], pm.bufs))
        S.act(ecs[:], V(pm.ap[:, 0:64], pm.bufs), AF.Exp)
        pc = S.bank(7)
        for gi in range(4):
            S.mm(V(pc.ap[:, gi * 128:(gi + 1) * 128], pc.bufs), BT[:, gi, tsl], CT[:, gi, tsl])
        S.copy("dve", V(CBT.t[:, :, :].rearrange("p a b -> p (a b)"), [CBT.buf]), pc)
        xv = V(x.t[:, :].rearrange("p (h q) -> p h q", h=32), [x.buf])
        for d in range(2):
            xo = V(xdt[d].t[:, :].rearrange("p (h q) -> p h q", h=32), [xdt[d].buf])
            dv = V(DTV.t[:, ch, d * 32:(d + 1) * 32].unsqueeze(2).to_broadcast([128, 32, 64]), [DTV.buf])
            S.tt("pool", xo, xv, dv, ALU.mult)
        for half in range(2):
            yp = S.bank(0, 2)
            for jj in range(4):
                j = half * 4 + jj
                gi = j // 2
                for d in range(2):
                    k = gcnt % 2
                    gcnt += 1
                    a4 = V(ADT.t[:, ch, d * 32 + 4 * j:d * 32 + 4 * j + 4].unsqueeze(2).to_broadcast([128, 4, 128]), [ADT.buf])
                    tb = V(tri[d].t[:, :].unsqueeze(1).to_broadcast([128, 4, 128]), [tri[d].buf])
                    S.tt("dve", R[k][:], tb, a4, ALU.mult)
                    dp = S.bank(4 + k)
                    S.mm(dp, c.ones_f[:], V(R[k].t[:, :, :].rearrange("p a b -> p (a b)"), [R[k].buf]))
                    for ii in range(4):
                        h = 4 * j + ii
                        S.stt("dve", Dm[k][:, ii, :], V(dp.ap[:, ii * 128:(ii + 1) * 128], dp.bufs),
                              cs[:, d * 32 + h:d * 32 + h + 1], negm[d][:], ALU.subtract, ALU.add)
                    S.act(Dm[k][:], Dm[k][:], AF.Exp)
                    cb = V(CBT.t[:, gi, :].unsqueeze(1).to_broadcast([128, 4, 128]), [CBT.buf])
                    S.tt("pool", MT[d][:], Dm[k][:], cb, ALU.mult)
                for ii in range(4):
                    h = 4 * j + ii
                    hl = h - half * 16
                    for d in range(2):
                        S.mm(V(yp.ap[:, hl * 64:(hl + 1) * 64], yp.bufs[hl // 8:hl // 8 + 1]), MT[d][:, ii, :],
                             xdt[d][:, h * 64:(h + 1) * 64], start=(d == 0), stop=(d == 1))
            ya = yacc[:, half * 1024:(half + 1) * 1024]
            ya3 = V(yacc.t[:, half * 1024:(half + 1) * 1024].rearrange("p (h q) -> p h q", h=16), [yacc.buf])
            for d in range(2):
                yo = S.bank(2, 2)
                for gg in range(2):
                    gi = half * 2 + gg
                    S.mm(V(yo.ap[:, gg * 512:(gg + 1) * 512], yo.bufs[gg:gg + 1]), CT[:, gi, tsl],
                         sts[d][i][:, gi * 512:(gi + 1) * 512])
                ev = V(ecs.t[:, d * 32 + half * 16:d * 32 + half * 16 + 16].unsqueeze(2).to_broadcast([128, 16, 64]), [ecs.buf])
                yo3 = V(yo.ap.rearrange("p (h q) -> p h q", h=16), yo.bufs)
                if d == 0:
                    S.tt("dve", ya3, yo3, ev, ALU.mult)
                else:
                    t23 = V(t2.t[:, :].rearrange("p (h q) -> p h q", h=16), [t2.buf])
                    S.tt("dve", t23, yo3, ev, ALU.mult)
                    S.tt("pool", ya, ya, t2[:], ALU.add)
            S.tt("dve", ya, ya, yp, ALU.add)
        if "YDBG" in DBG_EXT:
            S.dma("sp", g.YDBG.v(g.YDBG.t[ch * 128:(ch + 1) * 128, :]), yacc[:])
        y3 = V(yacc.t[:, :].rearrange("p (h q) -> p h q", h=32), [yacc.buf])
        dk = V(dsk.t[:, :].unsqueeze(2).to_broadcast([128, 32, 64]), [dsk.buf])
        xf = V(z.t[:, :].rearrange("p (h q) -> p h q", h=32), [z.buf])
        S.act(z[:], z[:], AF.Silu)
        S.tt("dve", yacc[:], yacc[:], z[:], ALU.mult)
        t3 = V(junk.t[:, :], [junk.buf])
        for q4 in range(4):
            xq = V(x.t[:, q4 * 512:(q4 + 1) * 512].rearrange("p (h q) -> p h q", h=8), [x.buf])
            dq = V(dsk.t[:, q4 * 8:(q4 + 1) * 8].unsqueeze(2).to_broadcast([128, 8, 64]), [dsk.buf])
            j3 = V(junk.t[:, :].rearrange("p (h q) -> p h q", h=8), [junk.buf])
            S.tt("pool", j3, xq, dq, ALU.mult)
            S.tt("pool", junk[:], junk[:], z[:, q4 * 512:(q4 + 1) * 512], ALU.mult)
            S.tt("dve", yacc[:, q4 * 512:(q4 + 1) * 512], yacc[:, q4 * 512:(q4 + 1) * 512], junk[:], ALU.add)
        S.memset("dve", ss[:], 0.0)
        for gi in range(4):
            S.act(junk[:], yacc[:, gi * 512:(gi + 1) * 512], AF.Square, accum_out=ss[:, gi:gi + 1])
        S.act(ss[:], ss[:], AF.Sqrt, bias=c.eps_rms[:], scale=1.0 / 512)
        S.op("dve", lambda e: e.reciprocal(ss.t[:], ss.t[:]), [ss[:]], [ss[:]])
        for gi in range(4):
            S.act(yacc[:, gi * 512:(gi + 1) * 512], yacc[:, gi * 512:(gi + 1) * 512], AF.Copy, scale=ss[:, gi:gi + 1])
        S.tt("dve", ybf[:], yacc[:], ng[:], ALU.mult)
        for j in range(4):
            pb = S.bank(6 + (j % 2), dt=BF16)
            for ii in range(4):
                kc = j * 4 + ii
                S.tr(V(pb.ap[:, ii * 128:(ii + 1) * 128], pb.bufs), ybf[:, kc * 128:(kc + 1) * 128], c.identb[:])
            S.copy("act", YST[:, j * 4:(j + 1) * 4, tsl], V(pb.ap[:, 0:512].rearrange("p (a b) -> p a b", a=4), pb.bufs))
    for kc in range(KC):
        S.dma("sp", g.YST.v(g.YST.t[kc * 128:(kc + 1) * 128, :]), YST[:, kc, :])
    S.barrier()
    S.arena_reset(base)


def phase_attn(S, g, c, l, last=False):
    import math as _m
    base = S.arena_off
    lam_init = 0.8 - 0.6 * _m.exp(-0.3 * l)
    lv = S.sb("lamv", [128, 256], F32)
    S.dma("sp", lv[:], g.lamv.v(g.lamv.t[l].partition_broadcast(128)))
    pr = S.sb("lampr", [128, 128], F32)
    sm = S.sb("lamsm", [128, 2], F32)
    neglam = S.sb("neglam", [128, 1], F32)
    gsc = S.sb("gsc", [128, 1], F32)
    S.tt("dve", pr[:, 0:64], lv[:, 0:64], lv[:, 64:128], ALU.mult)
    S.tt("dve", pr[:, 64:128], lv[:, 128:192], lv[:, 192:256], ALU.mult)
    for i in range(2):
        S.op("dve", lambda e, i=i: e.reduce_sum(sm.t[:, i:i + 1], pr.t[:, i * 64:(i + 1) * 64], AX.X), [pr[:]], [sm[:]])
    S.act(sm[:], sm[:], AF.Exp)
    S.tt("dve", neglam[:], sm[:, 1:2], sm[:, 0:1], ALU.subtract)
    S.ts("dve", neglam[:], neglam[:], -lam_init, None, ALU.add)
    S.dma("sp", gsc[:], g.subg.v(g.subg.t[l]))
    S.ts("dve", gsc[:], gsc[:], 1.0 - lam_init, None, ALU.mult)

    KTh = [S.sb("KTh%d" % i, [128, T], BF16) for i in range(2)]
    QTh = [S.sb("QTh%d" % i, [128, T], BF16) for i in range(2)]
    QNh = [S.sb("QNh%d" % i, [128, T], BF16) for i in range(2)]
    Vh = [S.sb("Vh%d" % i, [128, NT, 128], BF16) for i in range(2)]
    YA = [S.sb("YAh%d" % i, [128, T], BF16) for i in range(2)]
    psum_acc = [S.sb("pacc%d" % m, [128, 512], F32) for m in range(2)]
    psum_acc2 = [S.sb("paccp%d" % m, [128, 512], F32) for m in range(2)]
    PT = [S.sb("PT%d" % i, [128, 512], BF16) for i in range(9)]
    r = [S.sb("ar%d" % i, [128, 512], F32) for i in range(2)]
    tt_ = [S.sb("at%d" % i, [128, 512], F32) for i in range(2)]
    o2 = [S.sb("ao%d" % i, [128, 512], F32) for i in range(2)]
    tt2 = [[S.sb("at%d_%d" % (i, j), [128, 512], F32) for j in range(2)] for i in range(2)]
    pending = []
    qcnt = 0
    sq = S.sb("asq", [128, 512], BF16)
    rs = S.sb("ars", [128, 512], F32)

    def loads(h, i):
        S.dma("sp", KTh[i][:], g.KT.v(g.KT.t[h]))
        S.dma("sp", QTh[i][:], g.QT.v(g.QT.t[h]))
        S.dma("sp", QNh[i][:], g.QN.v(g.QN.t[h]))
        S.dma("sp", Vh[i][:], g.Vd.v(g.Vd.t[:, h * 128:(h + 1) * 128].rearrange("(kc p) e -> p kc e", p=128)))

    loads(0, 0)
    pcnt = 0
    bcnt = 0
    qgroups = [(NCTX + i * 512, 512, list(range(NT))) for i in range(4)] + ([] if last else [(0, NCTX, [0, 1])])
    for h in range(16):
        i = h % 2
        if h + 1 < 16:
            loads(h + 1, (h + 1) % 2)
        for (q0, nq, kcs) in qgroups:
            o = o2[qcnt % 2]
            tt_ = tt2[qcnt % 2]
            qcnt += 1
            BS = 3
            batches = []
            for m in range(2):
                for b0 in range(0, len(kcs), BS):
                    batches.append([(m, ki, kcs[ki]) for ki in range(b0, min(b0 + BS, len(kcs)))])
            obv = []
            for m in range(2):
                ob = S.bank(6 + m)
                obv.append(V(ob.ap[:, 0:nq], ob.bufs))
            pv_order = {0: [], 1: []}
            for bt in batches:
                for (m, ki, kc) in reversed(bt):
                    pv_order[m].append(ki)
            pts = {}

            def norm_map(m, nq=nq, obv=obv, tt_=tt_):
                sbk = S.bank(m)
                sv = V(sbk.ap[:, 0:nq], sbk.bufs)
                S.mm(sv, c.ones_f[:], psum_acc[m][:, 0:nq], start=True, stop=False)
                S.mm(sv, c.ones_f[:], psum_acc2[m][:, 0:nq], start=False, stop=True)
                S.op("dve", lambda e, m=m, sv=sv, nq=nq: e.reciprocal(r[m].t[:, 0:nq], sv.ap), [sv], [r[m][:]])
                S.tt("dve", tt_[m][:, 0:nq], obv[m], r[m][:, 0:nq], ALU.mult)

            def emit_pv(bt):
                for (m, ki, kc) in reversed(bt):
                    p = pts.pop((m, ki))
                    S.mm(obv[m], Vh[i][:, kc, :], p[:, 0:nq], start=(ki == pv_order[m][0]), stop=(ki == pv_order[m][-1]))

            for bi, bt in enumerate(batches):
                sset = (bcnt % 2) * 3
                bcnt += 1
                stvs = {}
                for j, (m, ki, kc) in reversed(list(enumerate(bt))):
                    ps_ = slice(m * 64, (m + 1) * 64)
                    st = S.bank(sset + j)
                    stv = V(st.ap[:, 0:nq], st.bufs)
                    stvs[j] = stv
                    qsrc = QNh[i] if kc < 2 else QTh[i]
                    S.mm(stv, KTh[i][ps_, kc * 128:(kc + 1) * 128], qsrc[ps_, q0:q0 + nq])
                for j, (m, ki, kc) in enumerate(bt):
                    p = PT[pcnt % 9]
                    pcnt += 1
                    S.act(p[:, 0:nq], stvs[j], AF.Exp, scale=0.125)
                    pts[(m, ki)] = p
                    if ki == 0:
                        S.copy("dve", psum_acc[m][:, 0:nq], p[:, 0:nq])
                    elif ki == 1:
                        S.copy("pool", psum_acc2[m][:, 0:nq], p[:, 0:nq])
                    elif ki % 3 == 1:
                        S.tt("pool", psum_acc2[m][:, 0:nq], psum_acc2[m][:, 0:nq], p[:, 0:nq], ALU.add)
                    else:
                        S.tt("dve", psum_acc[m][:, 0:nq], psum_acc[m][:, 0:nq], p[:, 0:nq], ALU.add)
                if bi >= 1:
                    emit_pv(batches[bi - 1])
                    if batches[bi - 1][-1][0] == 0 and bt[0][0] == 1:
                        norm_map(0)
                if bi == 1 and pending:
                    pending.pop(0)()
            emit_pv(batches[-1])
            norm_map(1)
            if pending:
                pending.pop(0)()
            S.stt("dve", o[:, 0:nq], tt_[1][:, 0:nq], neglam[:], tt_[0][:, 0:nq], ALU.mult, ALU.add)

            def tail(o=o, nq=nq, q0=q0, i=i):
                S.act(sq[:, 0:nq], o[:, 0:nq], AF.Square)
                mb = S.bank(2)
                mbv = V(mb.ap[:, 0:nq], mb.bufs)
                S.mm(mbv, c.ones_b[:], sq[:, 0:nq])
                S.act(rs[:, 0:nq], mbv, AF.Sqrt, bias=c.eps_rms[:], scale=1.0 / 128)
                S.op("dve", lambda e, nq=nq: e.reciprocal(rs.t[:, 0:nq], rs.t[:, 0:nq]), [rs[:]], [rs[:]])
                S.tt("pool", o[:, 0:nq], o[:, 0:nq], rs[:, 0:nq], ALU.mult)
                S.act(YA[i][:, q0:q0 + nq], o[:, 0:nq], AF.Copy, scale=gsc[:])

            pending.append(tail)
        while pending:
            pending.pop(0)()
        S.dma("sp", g.YAT.v(g.YAT.t[h * 128:(h + 1) * 128, :]), YA[i][:])
    S.barrier()
    S.arena_reset(base)


def declare_merge_scratch(S, g):
    g.MTd = S.dram("MTd", [D, T], BF16)
    g.U2T = S.dram("U2T", [D, T], BF16)
    g.YM = S.dram("YM", [T, D], F32)


def phase_merge(S, g, c, l):
    base = S.arena_off
    YS = S.sb("YSr", [128, KC, T], BF16)
    YA = S.sb("YAr", [128, KC, T], BF16)
    for kc in range(KC):
        S.dma("sp", YS[:, kc, :], g.YST.v(g.YST.t[kc * 128:(kc + 1) * 128, :]))
        S.dma("sp", YA[:, kc, :], g.YAT.v(g.YAT.t[kc * 128:(kc + 1) * 128, :]))
    ws = [S.sb("wbs%d" % i, [128, KC, 128], BF16) for i in range(2)]
    wa = [S.sb("wba%d" % i, [128, KC, 128], BF16) for i in range(2)]
    gs = S.sb("ggs", [128, T], F32)
    ga = S.sb("gga", [128, T], F32)
    m1 = [S.sb("mm1_%d" % i, [128, 512], F32) for i in range(2)]
    m2 = [S.sb("mm2_%d" % i, [128, 512], F32) for i in range(2)]
    mrow = [S.sb("mrow%d" % i, [128, T], BF16) for i in range(2)]
    wsv = g.w_br_ssm.t[l].rearrange("(kc p) n -> p kc n", p=128)
    wav = g.w_br_att.t[l].rearrange("(kc p) n -> p kc n", p=128)

    def loadw(cc):
        S.dma("pool", ws[cc % 2][:], g.w_br_ssm.v(wsv[:, :, cc * 128:(cc + 1) * 128]))
        S.dma("pool", wa[cc % 2][:], g.w_br_att.v(wav[:, :, cc * 128:(cc + 1) * 128]))

    loadw(0)
    cnt = 0
    for cc in range(16):
        if cc + 1 < 16:
            loadw(cc + 1)
        S.dma("sp", gs[:], g.GT.v(g.GT.t[cc * 128:(cc + 1) * 128, :]))
        S.dma("sp", ga[:], g.GT.v(g.GT.t[D + cc * 128:D + (cc + 1) * 128, :]))
        mr = mrow[cc % 2]
        for (t0, tn) in TG:
            k = cnt % 2
            cnt += 1
            p1 = S.bank(2 * k)
            p2 = S.bank(2 * k + 1)
            p1v = V(p1.ap[:, 0:tn], p1.bufs)
            p2v = V(p2.ap[:, 0:tn], p2.bufs)
            for kc in range(KC):
                S.mm(p1v, ws[cc % 2][:, kc, :], YS[:, kc, t0:t0 + tn], start=(kc == 0), stop=(kc == KC - 1))
            for kc in range(KC):
                S.mm(p2v, wa[cc % 2][:, kc, :], YA[:, kc, t0:t0 + tn], start=(kc == 0), stop=(kc == KC - 1))
            S.tt("dve", m1[k][:, 0:tn], p1v, gs[:, t0:t0 + tn], ALU.mult)
            S.tt("dve", m2[k][:, 0:tn], p2v, ga[:, t0:t0 + tn], ALU.mult)
            S.tt("pool", mr[:, t0:t0 + tn], m1[k][:, 0:tn], m2[k][:, 0:tn], ALU.add)
        S.dma("sp", g.MTd.v(g.MTd.t[cc * 128:(cc + 1) * 128, :]), mr[:])
    S.barrier()
    S.arena_reset(base)


def bcast_tile(S, name, dram_tl, ap1d):
    t = S.sb(name, [128, D], F32)
    S.dma("sp", t[:], dram_tl.v(ap1d.partition_broadcast(128)))
    return t


def phase_outproj(S, g, c, l, src):
    base = S.arena_off
    wo = S.sb("wout", [128, KC, D], BF16)
    wov = g.w_out.t[l].rearrange("(kc p) n -> p kc n", p=128)
    for n in range(4):
        S.dma("pool", wo[:, :, n * 512:(n + 1) * 512], g.w_out.v(wov[:, :, n * 512:(n + 1) * 512]))
    g1 = load_mod_tiles(S, g, l, 2, False)
    lg = bcast_tile(S, "ln1g", g.ln1g, g.ln1g.t[l])
    lb = bcast_tile(S, "ln1b", g.ln1b, g.ln1b.t[l])
    mts = [S.sb("mt%d" % i, [128, KC, 128], BF16) for i in range(2)]
    hs = [S.sb("hh%d" % i, [128, D], F32) for i in range(2)]
    t1 = S.sb("ot1", [128, D], F32)
    xn = S.sb("oxn", [128, D], F32)
    stats = S.sb("ostats", [128, 4, 6], F32)
    mv = S.sb("omv", [128, 2], F32)
    rstd = S.sb("orstd", [128, 1], F32)
    nmr = S.sb("onmr", [128, 1], F32)
    mtv = g.MTd.t.rearrange("(cc p) t -> p cc t", p=128)

    def loads(t):
        S.dma("sp", mts[t % 2][:], g.MTd.v(mtv[:, :, t * 128:(t + 1) * 128]))
        S.dma("sp", hs[t % 2][:], src.v(src.t[t * 128:(t + 1) * 128, :]))

    loads(0)
    for t in range(NT):
        if t + 1 < NT:
            loads(t + 1)
        row = 1 if t < 2 else 0
        mt = mts[t % 2]
        h = hs[t % 2]
        po = S.bank(0, 4)
        for n in range(4):
            for cc in range(KC):
                S.mm(V(po.ap[:, n * 512:(n + 1) * 512], po.bufs[n:n + 1]), mt[:, cc, :], wo[:, cc, n * 512:(n + 1) * 512],
                     start=(cc == 0), stop=(cc == KC - 1))
        S.tt("dve", t1[:], po, g1[row][:], ALU.mult)
        S.stt("dve", t1[:], h[:], float(ALPHA), t1[:], ALU.mult, ALU.add)
        layer_norm_tile(S, c, t1, xn, stats, mv, rstd, nmr)
        S.tt("pool", xn[:], xn[:], lg[:], ALU.mult)
        S.tt("pool", xn[:], xn[:], lb[:], ALU.add)
        S.dma("sp", g.H.v(g.H.t[t * 128:(t + 1) * 128, :]), xn[:])
    S.barrier()
    S.arena_reset(base)


def router_tile(S, g, c, rt, xn, t):
    u2Tf = rt.u2Tf
    for j in range(4):
        pb = S.bank(4 + (j % 2))
        for i in range(4):
            kc = j * 4 + i
            S.tr(V(pb.ap[:, i * 128:(i + 1) * 128], pb.bufs), xn[:, kc * 128:(kc + 1) * 128], c.identf[:])
        S.copy("dve", u2Tf[:, j * 4:(j + 1) * 4, :], V(pb.ap[:, 0:512].rearrange("p (a b) -> p a b", a=4), pb.bufs))
    pl = S.bank(3)
    plv = V(pl.ap[:, 0:36], pl.bufs)
    for kc in range(KC):
        S.mm(plv, u2Tf[:, kc, :], rt.wr[:, kc, :], start=(kc == 0), stop=(kc == KC - 1))
    lg = rt.lg
    S.tt("dve", lg[:], plv, rt.brb[:], ALU.add)
    mx = rt.s[:, 0:1]
    s4 = rt.s[:, 1:2]
    pg = rt.s[:, 2:3]
    m1 = rt.s[:, 3:4]
    m2 = rt.s[:, 4:5]
    e2 = rt.s[:, 5:6]
    w1 = rt.s[:, 6:7]
    w2 = rt.s[:, 7:8]
    nmx = rt.s[:, 8:9]
    S.op("dve", lambda e: e.reduce_max(rt.s.t[:, 0:1], lg.t[:, 0:4], AX.X), [lg[:]], [rt.s[:]])
    S.ts("dve", nmx, mx, -1.0, None, ALU.mult)
    S.memset("dve", s4, 0.0)
    S.act(rt.e4[:], lg[:, 0:4], AF.Exp, bias=nmx, accum_out=s4)
    S.op("dve", lambda e: e.reciprocal(rt.s.t[:, 2:3], rt.s.t[:, 1:2]), [rt.s[:]], [rt.s[:]])
    S.ts("dve", rt.oh4[:], lg[:, 0:4], mx, None, ALU.is_equal)
    le = V(lg.t[:, 4:36].rearrange("p (g e) -> p g e", g=4), [lg.buf])
    ohb = V(rt.oh4.t[:, :].unsqueeze(2).to_broadcast([128, 4, 8]), [rt.oh4.buf])
    S.tt("dve", V(rt.t32.t[:, :].rearrange("p (g e) -> p g e", g=4), [rt.t32.buf]), le, ohb, ALU.mult)
    S.op("dve", lambda e: e.reduce_sum(rt.l8.t[:, :], rt.t32.t[:, :].rearrange("p (g e) -> p e g", g=4), AX.X), [rt.t32[:]], [rt.l8[:]])
    S.op("dve", lambda e: e.reduce_max(rt.s.t[:, 3:4], rt.l8.t[:, :], AX.X), [rt.l8[:]], [rt.s[:]])
    S.ts("dve", rt.oh1[:], rt.l8[:], m1, None, ALU.is_equal)
    S.stt("dve", rt.l8b[:], rt.oh1[:], -1.0e30, rt.l8[:], ALU.mult, ALU.add)
    S.op("dve", lambda e: e.reduce_max(rt.s.t[:, 4:5], rt.l8b.t[:, :], AX.X), [rt.l8b[:]], [rt.s[:]])
    S.ts("dve", rt.oh2[:], rt.l8b[:], m2, None, ALU.is_equal)
    S.tt("dve", e2, m2, m1, ALU.subtract)
    S.act(e2, e2, AF.Exp)
    S.ts("dve", w1, e2, 1.0, None, ALU.add)
    S.op("dve", lambda e: e.reciprocal(rt.s.t[:, 6:7], rt.s.t[:, 6:7]), [rt.s[:]], [rt.s[:]])
    S.tt("dve", w1, w1, pg, ALU.mult)
    S.tt("dve", w2, w1, e2, ALU.mult)
    S.ts("dve", rt.oh1[:], rt.oh1[:], w1, None, ALU.mult)
    S.stt("dve", rt.w8[:], rt.oh2[:], w2, rt.oh1[:], ALU.mult, ALU.add)
    w8b = V(rt.w8.t[:, :].unsqueeze(1).to_broadcast([128, 4, 8]), [rt.w8.buf])
    S.tt("dve", V(rt.WR.t[:, t, :].rearrange("p (g e) -> p g e", g=4), [rt.WR.buf]), ohb, w8b, ALU.mult)


def router_setup(S, g, c, l, WR):
    rt = Ctx()
    rt.WR = WR
    rt.u2Tf = S.sb("u2Tf", [128, KC, 128], F32)
    rt.wr = S.sb("wr", [128, KC, 36], F32)
    S.dma("sp", rt.wr[:], g.wr.v(g.wr.t[l].rearrange("(kc p) n -> p kc n", p=128)))
    rt.brb = S.sb("brb", [128, 36], F32)
    S.dma("sp", rt.brb[:], g.br.v(g.br.t[l].partition_broadcast(128)))
    rt.lg = S.sb("rlg", [128, 36], F32)
    rt.s = S.sb("rs", [128, 16], F32)
    rt.e4 = S.sb("re4", [128, 4], F32)
    rt.oh4 = S.sb("roh4", [128, 4], F32)
    rt.t32 = S.sb("rt32", [128, 32], F32)
    rt.l8 = S.sb("rl8", [128, 8], F32)
    rt.l8b = S.sb("rl8b", [128, 8], F32)
    rt.oh1 = S.sb("roh1", [128, 8], F32)
    rt.oh2 = S.sb("roh2", [128, 8], F32)
    rt.w8 = S.sb("rw8", [128, 8], F32)
    return rt


def phase_moe(S, g, c, l, WR, last=False):
    base = S.arena_off
    wg = S.sb("wg", [128, KC, 1024], BF16)
    wu = S.sb("wu", [128, KC, 1024], BF16)
    wd = S.sb("wd", [128, 8, D], BF16)
    hT = S.sb("hT", [128, 8, T], BF16)
    ut = [S.sb("ut%d" % i, [128, KC, 512], BF16) for i in range(2)]
    sg = [S.sb("sg%d" % i, [128, 512], F32) for i in range(2)]
    ysb = [S.sb("ysb%d" % i, [128, D], F32) for i in range(2)]
    u2v = g.U2T.t.rearrange("(kc p) t -> p kc t", p=128)

    def load_gu(e):
        gv = g.w_eg.t[l, e].rearrange("(kc p) f -> p kc f", p=128)
        uv = g.w_eu.t[l, e].rearrange("(kc p) f -> p kc f", p=128)
        for hf in range(2):
            S.dma("pool", wg[:, :, hf * 512:(hf + 1) * 512], g.w_eg.v(gv[:, :, hf * 512:(hf + 1) * 512]))
            S.dma("pool", wu[:, :, hf * 512:(hf + 1) * 512], g.w_eu.v(uv[:, :, hf * 512:(hf + 1) * 512]))

    def load_d(e):
        dv = g.w_ed.t[l, e].rearrange("(fc p) d -> p fc d", p=128)
        for hf in range(2):
            S.dma("pool", wd[:, hf * 4:(hf + 1) * 4, :], g.w_ed.v(dv[:, hf * 4:(hf + 1) * 4, :]))

    load_gu(0)
    load_d(0)
    ucnt = 0
    pcnt = 0
    ycnt = 0
    tgs = TG[0:4] if not last else [(NCTX + i * 512, 512) for i in range(4)]
    tiles = list(range(NT)) if not last else list(range(2, NT))
    if not last:
        tgs = TG
    for e in range(32):
        for (t0, tn) in tgs:
            u = ut[ucnt % 2]
            ucnt += 1
            S.dma("sp", u[:, :, 0:tn], g.U2T.v(u2v[:, :, t0:t0 + tn]))
            for fc in range(8):
                k = pcnt % 2
                pcnt += 1
                pg = S.bank(2 * k)
                pu = S.bank(2 * k + 1)
                pgv = V(pg.ap[:, 0:tn], pg.bufs)
                puv = V(pu.ap[:, 0:tn], pu.bufs)
                for kc in range(KC):
                    S.mm(pgv, wg[:, kc, fc * 128:(fc + 1) * 128], u[:, kc, 0:tn], start=(kc == 0), stop=(kc == KC - 1))
                for kc in range(KC):
                    S.mm(puv, wu[:, kc, fc * 128:(fc + 1) * 128], u[:, kc, 0:tn], start=(kc == 0), stop=(kc == KC - 1))
                S.act(sg[k][:, 0:tn], pgv, AF.Silu)
                S.tt("dve", hT[:, fc, t0:t0 + tn], puv, sg[k][:, 0:tn], ALU.mult)
        if e + 1 < 32:
            load_gu(e + 1)
        for t in tiles:
            py = S.bank(4, 4)
            for n in range(4):
                for fc in range(8):
                    S.mm(V(py.ap[:, n * 512:(n + 1) * 512], py.bufs[n:n + 1]), hT[:, fc, t * 128:(t + 1) * 128],
                         wd[:, fc, n * 512:(n + 1) * 512], start=(fc == 0), stop=(fc == 7))
            y = ysb[ycnt % 2]
            ycnt += 1
            wsc = WR[:, t, e:e + 1]
            S.act(y[:, 0:1024], V(py.ap[:, 0:1024], py.bufs[0:2]), AF.Copy, scale=wsc)
            S.ts("dve", y[:, 1024:2048], V(py.ap[:, 1024:2048], py.bufs[2:4]), wsc, None, ALU.mult)
            if e == 0:
                S.dma("pool", g.YM.v(g.YM.t[t * 128:(t + 1) * 128, :]), y[:])
            else:
                S.dma("pool", g.YM.v(g.YM.t[t * 128:(t + 1) * 128, :]), y[:], accum_op=ALU.add)
        if e + 1 < 32:
            load_d(e + 1)
    S.barrier()
    S.arena_reset(base)


def phase_final(S, g, c, l, last):
    base = S.arena_off
    g2 = load_mod_tiles(S, g, l, 5, False)
    lg = bcast_tile(S, "ln2g", g.ln2g, g.ln2g.t[l])
    lb = bcast_tile(S, "ln2b", g.ln2b, g.ln2b.t[l])
    hs = [S.sb("fh%d" % i, [128, D], F32) for i in range(2)]
    ys = [S.sb("fy%d" % i, [128, D], F32) for i in range(2)]
    xn = [S.sb("fxn%d" % i, [128, D], F32) for i in range(2)]
    stats = S.sb("fstats", [128, 4, 6], F32)
    mv = S.sb("fmv", [128, 2], F32)
    rstd = S.sb("frstd", [128, 1], F32)
    nmr = S.sb("fnmr", [128, 1], F32)
    toks = []
    tiles = list(range(2, NT)) if last else list(range(NT))
    for t in tiles:
        row = 1 if t < 2 else 0
        h = hs[t % 2]
        y = ys[t % 2]
        x = xn[t % 2]
        S.dma("sp", h[:], g.H.v(g.H.t[t * 128:(t + 1) * 128, :]))
        S.dma("sp", y[:], g.YM.v(g.YM.t[t * 128:(t + 1) * 128, :]))
        S.tt("dve", y[:], y[:], g2[row][:], ALU.mult)
        S.stt("dve", y[:], h[:], float(ALPHA), y[:], ALU.mult, ALU.add)
        layer_norm_tile(S, c, y, x, stats, mv, rstd, nmr)
        S.tt("pool", x[:], x[:], lg[:], ALU.mult)
        S.tt("pool", x[:], x[:], lb[:], ALU.add)
        if last:
            toks.append(S.dma("sp", g.out.v(g.out.t[(t - 2) * 128:(t - 1) * 128, :]), x[:]))
        else:
            S.dma("sp", g.H.v(g.H.t[t * 128:(t + 1) * 128, :]), x[:])
    S.barrier()
    S.arena_reset(base)
    return toks


DBG_EXT = set()


def build_program(stop_after="all", dbg=()):
    global DBG_EXT
    DBG_EXT = set(dbg)
    nc = bass.Bass("TRN2", target_bir_lowering=False)
    S = Sched(nc)
    _orig_dram = S.dram

    def dram(name, shape, dt, kind="Internal"):
        if name in DBG_EXT and kind == "Internal":
            kind = "ExternalOutput"
        return _orig_dram(name, shape, dt, kind=kind)

    S.dram = dram
    stages = ["mod", "lnmod", "inproj", "ssdprep", "ssd", "attn", "merge", "moe", "all"]
    lim = stages.index(stop_after)
    g = declare_io(S, lim)
    declare_ssd_scratch(S, g)
    declare_merge_scratch(S, g)
    g.WRd = S.dram("WRd", [128, NT * 32], F32)
    c = load_consts(S, g)
    final = []
    for l in range(NL):
        phase_mod(S, g, c, l)
        if lim == 0:
            break
        base = S.arena_off
        UT = S.sb("UT", [128, KC, T], BF16)
        phase_ln_mod(S, g, c, l, UT, 0, 1, g.xin if l == 0 else g.H)
        if lim >= 2:
            phase_inproj(S, g, c, l, UT)
        S.arena_reset(base)
        if lim <= 2:
            break
        DTV = S.sb("DTV", [128, NT, 64], F32)
        ADT = S.sb("ADT", [128, NT, 64], F32)
        phase_ssdprep(S, g, c, l, DTV, ADT)
        if lim >= 4:
            phase_ssd_states(S, g, c, l, DTV, ADT)
            phase_ssd_y(S, g, c, l, DTV, ADT)
        S.arena_reset(base)
        if lim <= 4:
            break
        phase_attn(S, g, c, l, last=(l == NL - 1))
        if lim <= 5:
            break
        phase_merge(S, g, c, l)
        phase_outproj(S, g, c, l, g.xin if l == 0 else g.H)
        if lim <= 6:
            break
        WR = S.sb("WR", [128, NT, 32], F32)
        base2 = S.arena_off
        UT2 = S.sb("UT2", [128, KC, T], BF16)
        rt = router_setup(S, g, c, l, WR)
        phase_ln_mod(S, g, c, l, UT2, 3, 4, g.H, router=rt, flush=g.U2T)
        if "WRd" in DBG_EXT:
            S.dma("sp", g.WRd[:], V(WR.t[:, :, :].rearrange("p a b -> p (a b)"), [WR.buf]))
        S.arena_reset(base2)
        phase_moe(S, g, c, l, WR, last=(l == NL - 1))
        toks = phase_final(S, g, c, l, l == NL - 1)
        final.extend(toks)
        S.arena_reset(base)
        if lim <= 7:
            break
    if not final:
        z = S.sb("zout", [128, 64], F32)
        S.memset("dve", z[:], 0.0)
        final.append(S.dma("sp", g.out.v(g.out.t[0:128, 0:64]), z[:]))
    S.barrier()
    S.finish(final)
    return nc


def host_consts():
    nf = 16
    inv = (10000.0 ** (-(np.arange(nf, dtype=np.float32) / nf))).astype(np.float32)
    s = np.arange(2048)
    row = (s // 64).astype(np.float32)
    col = (s % 64).astype(np.float32)
    ang = np.concatenate([row[:, None] * inv[None, :], col[:, None] * inv[None, :]], axis=1).astype(np.float32)
    cos = np.ones((T, 32), np.float32)
    sin = np.zeros((T, 32), np.float32)
    cos[NCTX:] = np.cos(ang)
    sin[NCTX:] = np.sin(ang)
    k = np.arange(128)
    tri = np.stack([(k[:, None] <= k[None, :]), (k[:, None] >= k[None, :])]).astype(np.float32)
    negm = np.stack([np.where(k[None, :] >= k[:, None], 0.0, -30000.0),
                     np.where(k[None, :] <= k[:, None], 0.0, -30000.0)]).astype(np.float32)
    return dict(costab=cos, sintab=sin, identf=np.eye(128, dtype=np.float32), tri=tri, negm=negm)


def prep_core_inputs(inp, b, consts):
    f = np.ascontiguousarray
    m = {}
    m["xin"] = f(np.concatenate([inp["ctx"][b], inp["x"][b]], axis=0))
    cc = np.stack([inp["c"][b], inp["c_ctx"]], axis=1)
    m["cT"] = f(cc.reshape(KC, 128, 2).transpose(1, 0, 2).reshape(128, 32))
    m["w_ada"] = inp["w_ada"]
    m["b_ada"] = inp["b_ada"]
    m["w_in"] = inp["w_in"]
    m["convw"] = f(inp["conv_w"].reshape(NL, 5, 24, 128).transpose(0, 3, 2, 1).reshape(NL, 128, 120))
    m["convb"] = f(inp["conv_b"].reshape(NL, 24, 128).transpose(0, 2, 1))
    m["dtb"] = f(inp["ssm_dt_bias"].reshape(NL, 64))
    m["alog"] = f(inp["ssm_a_log"].reshape(NL, 64))
    m["ssmd"] = inp["ssm_d"]
    m["normg"] = inp["ssm_norm_g"]
    m["lamv"] = f(np.concatenate([inp["lam_q1"], inp["lam_k1"], inp["lam_q2"], inp["lam_k2"]], axis=1))
    m["subg"] = f(inp["attn_subln_g"].reshape(NL, 128, 1))
    for k_ in ("w_br_ssm", "w_br_att", "w_out"):
        m[k_] = inp[k_]
    m["ln1g"], m["ln1b"], m["ln2g"], m["ln2b"] = inp["ln1_g"], inp["ln1_b"], inp["ln2_g"], inp["ln2_b"]
    m["wr"] = f(np.concatenate([inp["w_router_group"], inp["w_router_expert"]], axis=2))
    m["br"] = f(np.concatenate([inp["b_router_group"], inp["b_router_expert"]], axis=1))
    m["w_eg"], m["w_eu"], m["w_ed"] = inp["w_exp_gate"], inp["w_exp_up"], inp["w_exp_down"]
    m.update(consts)
    return m


def kernel(**inputs):
    inp = {k: np.asarray(v) for k, v in inputs.items()}
    nc = build_program(stop_after="all")
    consts = host_consts()
    names = set()
    for a in nc.allocations:
        try:
            if a.kind == "ExternalInput":
                names.add(a.memorylocations[0].name)
        except Exception:
            pass
    in_maps = []
    for b in range(8):
        m = prep_core_inputs(inp, b, consts)
        in_maps.append({k: v for k, v in m.items() if k in names})
    res = run_bass_kernel_spmd(nc, in_maps, core_ids=list(range(8)))
    out = np.stack([np.asarray(res.results[b]["out"]) for b in range(8)], axis=0)
    return out.astype(np.float32)
```

```python
import numpy as np
import concourse.bass as bass
import concourse.mybir as mybir
from concourse.bass_utils import run_bass_kernel_spmd

F32 = mybir.dt.float32
BF16 = mybir.dt.bfloat16
I32 = mybir.dt.int32
AF = mybir.ActivationFunctionType
ALU = mybir.AluOpType
AX = mybir.AxisListType

NDS = 8


class Buf:
    __slots__ = ("name", "w", "r")

    def __init__(self, name=""):
        self.name = name
        self.w = None
        self.r = []


class V:
    __slots__ = ("ap", "bufs")

    def __init__(self, ap, bufs):
        self.ap = ap
        self.bufs = bufs


class Tl:
    def __init__(self, t, name=""):
        self.t = t
        self.buf = Buf(name)

    def __getitem__(self, idx):
        return V(self.t[idx], [self.buf])

    def v(self, ap):
        return V(ap, [self.buf])


class Sched:
    def __init__(self, nc):
        self.nc = nc
        self.prog = {e: [] for e in ("pe", "dve", "act", "pool", "sp")}
        self.sem = {}
        self.cnt = {}
        for e in ("pe", "dve", "act", "pool"):
            self.sem[e] = nc.alloc_semaphore("s_" + e)
            self.cnt[e] = 0
        self.dq = {}
        for q in ("sp", "act", "pool"):
            ks = []
            for i in range(NDS):
                k = "d_%s%d" % (q, i)
                self.sem[k] = nc.alloc_semaphore(k)
                self.cnt[k] = 0
                ks.append(k)
            self.dq[q] = [ks, 0]
        self.seen = {e: {} for e in self.prog}
        self.ninst = 0
        self.arena_off = 16512
        self.nalloc = 0
        self.psum_t = nc.alloc_psum_tensor("psum_all", [128, 8 * 512], F32)
        self.psum_bufs = [Buf("bank%d" % i) for i in range(8)]

    def arena_reset(self, off=16512):
        self.arena_off = off

    def sb(self, name, shape, dt):
        esz = 2 if dt == BF16 else 4
        n = 1
        for s in shape[1:]:
            n *= s
        nbytes = (n * esz + 63) // 64 * 64
        off = self.arena_off
        assert off + nbytes <= 229344, ("SBUF arena overflow", name, off, nbytes)
        self.arena_off = off + nbytes
        self.nalloc += 1
        t = self.nc.alloc_sbuf_tensor_at("%s_%d" % (name, self.nalloc), list(shape), dt, offset=off)
        return Tl(t, name)

    def bank(self, i, n=1, dt=F32):
        ap = self.psum_t[:, i * 512:(i + n) * 512]
        if dt == BF16:
            ap = ap.bitcast(BF16)
        return V(ap, self.psum_bufs[i:i + n])

    def barrier(self):
        toks = [(k, v) for k, v in self.cnt.items() if v > 0]
        for e in self.prog:
            need = [(k, v) for (k, v) in toks if self.seen[e].get(k, 0) < v and not (k == e)]
            for k, v in need:
                self.seen[e][k] = v
            sem = self.sem

            def emit(eng, need=need):
                for k, v in need:
                    eng.wait_ge(sem[k], v)

            self.prog[e].append(emit)


    def dram(self, name, shape, dt, kind="Internal"):
        return Tl(self.nc.dram_tensor(name, list(shape), dt, kind=kind).ap(), name)

    def _deps(self, e, reads, writes):
        need = {}

        def add(tok):
            if tok is None:
                return
            k, v = tok
            if k == "pe" and e == "pe":
                return
            if self.seen[e].get(k, 0) >= v:
                return
            if need.get(k, 0) < v:
                need[k] = v

        for b in reads:
            add(b.w)
        for b in writes:
            add(b.w)
            for t in b.r:
                add(t)
        for k, v in need.items():
            self.seen[e][k] = v
        return list(need.items())

    def _record(self, tok, reads, writes):
        for b in writes:
            b.w = tok
            b.r = []
        for b in reads:
            if b in writes:
                continue
            b.r.append(tok)
            if len(b.r) > 24:
                mx = {}
                for k, v in b.r:
                    if mx.get(k, 0) < v:
                        mx[k] = v
                b.r = list(mx.items())

    def op(self, e, fn, reads, writes):
        rb = [b for x in reads for b in x.bufs]
        wb = [b for x in writes for b in x.bufs]
        waits = self._deps(e, rb, wb)
        self.cnt[e] += 1
        tok = (e, self.cnt[e])
        sem = self.sem
        se = sem[e]

        def emit(eng, waits=waits, fn=fn, se=se):
            for k, v in waits:
                eng.wait_ge(sem[k], v)
            fn(eng).then_inc(se, 1)

        self.prog[e].append(emit)
        self._record(tok, rb, wb)
        self.ninst += 1
        return tok

    def dma(self, q, out, in_, **kw):
        rb = list(in_.bufs)
        wb = list(out.bufs)
        waits = self._deps(q, rb, wb)
        ks, rr = self.dq[q]
        k = ks[rr % NDS]
        self.dq[q][1] = rr + 1
        if self.cnt[k] > 0 and self.seen[q].get(k, 0) < self.cnt[k]:
            waits = [w for w in waits if w[0] != k] + [(k, self.cnt[k])]
            self.seen[q][k] = self.cnt[k]
        self.cnt[k] += 16
        tok = (k, self.cnt[k])
        sem = self.sem
        oap, iap = out.ap, in_.ap

        def emit(eng, waits=waits, sk=sem[k]):
            for kk, v in waits:
                eng.wait_ge(sem[kk], v)
            eng.dma_start(out=oap, in_=iap, **kw).then_inc(sk, 16)

        self.prog[q].append(emit)
        self._record(tok, rb, wb)
        self.ninst += 1
        return tok

    def wait_all(self, e, toks):
        sem = self.sem
        toks = [t for t in toks if t is not None]

        def emit(eng):
            for k, v in toks:
                eng.wait_ge(sem[k], v)

        self.prog[e].append(emit)

    def mm(self, out, lhsT, rhs, start=True, stop=True, extra_reads=()):
        return self.op("pe", lambda eng: eng.matmul(out.ap, lhsT.ap, rhs.ap, start=start, stop=stop),
                       [lhsT, rhs] + list(extra_reads), [out])

    def tr(self, out, in_, ident):
        return self.op("pe", lambda eng: eng.transpose(out.ap, in_.ap, ident.ap), [in_, ident], [out])

    def act(self, out, in_, func, bias=None, scale=1.0, accum_out=None, e="act"):
        reads = [in_]
        kw = {}
        if bias is not None:
            if isinstance(bias, V):
                reads.append(bias)
                kw["bias"] = bias.ap
            else:
                kw["bias"] = bias
        if isinstance(scale, V):
            reads.append(scale)
            kw["scale"] = scale.ap
        else:
            kw["scale"] = scale
        writes = [out]
        if accum_out is not None:
            writes.append(accum_out)
            kw["accum_out"] = accum_out.ap
        return self.op("act", lambda eng: eng.activation(out.ap, in_.ap, func, **kw), reads, writes)

    def tt(self, e, out, a, b, op):
        return self.op(e, lambda eng: eng.tensor_tensor(out.ap, a.ap, b.ap, op), [a, b], [out])

    def ts(self, e, out, a, s1, s2, op0, op1=None, accum_out=None):
        reads = [a]
        s1a = s1.ap if isinstance(s1, V) else s1
        s2a = s2.ap if isinstance(s2, V) else s2
        if isinstance(s1, V):
            reads.append(s1)
        if isinstance(s2, V):
            reads.append(s2)
        writes = [out]
        kw = {}
        if accum_out is not None:
            writes.append(accum_out)
            kw["accum_out"] = accum_out.ap
        if op1 is None:
            return self.op(e, lambda eng: eng.tensor_scalar(out.ap, a.ap, s1a, None, op0, **kw), reads, writes)
        return self.op(e, lambda eng: eng.tensor_scalar(out.ap, a.ap, s1a, s2a, op0, op1, **kw), reads, writes)

    def stt(self, e, out, a, s, b, op0, op1):
        reads = [a, b]
        sa = s.ap if isinstance(s, V) else s
        if isinstance(s, V):
            reads.append(s)
        return self.op(e, lambda eng: eng.scalar_tensor_tensor(out.ap, a.ap, sa, b.ap, op0, op1), reads, [out])

    def copy(self, e, out, in_):
        if e == "act":
            return self.op("act", lambda eng: eng.copy(out.ap, in_.ap), [in_], [out])
        return self.op(e, lambda eng: eng.tensor_copy(out.ap, in_.ap), [in_], [out])

    def memset(self, e, out, val):
        return self.op(e, lambda eng: eng.memset(out.ap, val), [], [out])

    def finish(self, final_tokens):
        nc = self.nc
        self.wait_all("sp", final_tokens)
        prog = self.prog
        with nc.Block() as blk:
            @blk.sync
            def _(eng):
                for f in prog["sp"]:
                    f(eng)

            @blk.tensor
            def _(eng):
                for f in prog["pe"]:
                    f(eng)

            @blk.vector
            def _(eng):
                for f in prog["dve"]:
                    f(eng)

            @blk.scalar
            def _(eng):
                for f in prog["act"]:
                    f(eng)

            @blk.gpsimd
            def _(eng):
                for f in prog["pool"]:
                    f(eng)


NL = 2
T = 2304
D = 2048
NT = 18
KC = 16
NCTX = 256
IN_COLS = 15424
OFF_Z, OFF_XBC, OFF_DT, OFF_Q, OFF_K, OFF_V, OFF_G = 0, 2048, 5120, 5184, 7232, 9280, 11328
LN_EPS = 1e-6
RMS_EPS = 1e-5
ALPHA = (2.0 * NL) ** 0.25
TG = [(0, 512), (512, 512), (1024, 512), (1536, 512), (2048, 256)]


def bcast_rows(ap1d, n=128):
    return ap1d.partition_broadcast(n)


class Ctx:
    pass


def declare_io(S, lim=99):
    nc = S.nc
    g = Ctx()
    need = {"w_br_ssm": 6, "w_br_att": 6, "w_out": 6, "w_eg": 7, "w_eu": 7, "w_ed": 7, "w_in": 2}

    def inp(name, shape, dt=F32):
        if need.get(name, 0) > lim:
            return None
        return S.dram(name, shape, dt, kind="ExternalInput")

    g.xin = inp("xin", [T, D])
    g.cT = inp("cT", [128, 32])
    g.w_ada = inp("w_ada", [NL, D, 6 * D])
    g.b_ada = inp("b_ada", [NL, 6 * D])
    g.w_in = inp("w_in", [NL, D, IN_COLS])
    g.convw = inp("convw", [NL, 128, 24 * 5])
    g.convb = inp("convb", [NL, 128, 24])
    g.dtb = inp("dtb", [NL, 64])
    g.alog = inp("alog", [NL, 64])
    g.ssmd = inp("ssmd", [NL, 32])
    g.normg = inp("normg", [NL, D])
    g.lamv = inp("lamv", [NL, 4 * 64])
    g.subg = inp("subg", [NL, 128, 1])
    g.w_br_ssm = inp("w_br_ssm", [NL, D, D])
    g.w_br_att = inp("w_br_att", [NL, D, D])
    g.w_out = inp("w_out", [NL, D, D])
    g.ln1g = inp("ln1g", [NL, D])
    g.ln1b = inp("ln1b", [NL, D])
    g.ln2g = inp("ln2g", [NL, D])
    g.ln2b = inp("ln2b", [NL, D])
    g.wr = inp("wr", [NL, D, 36])
    g.br = inp("br", [NL, 36])
    g.w_eg = inp("w_eg", [NL, 32, D, 1024])
    g.w_eu = inp("w_eu", [NL, 32, D, 1024])
    g.w_ed = inp("w_ed", [NL, 32, 1024, D])
    g.costab = inp("costab", [T, 32])
    g.sintab = inp("sintab", [T, 32])
    g.identf = inp("identf", [128, 128])
    g.tri = inp("tri", [2, 128, 128])
    g.negm = inp("negm", [2, 128, 128])
    g.out = S.dram("out", [2048, D], F32, kind="ExternalOutput")
    g.MOD = S.dram("MOD", [NL, 2, 6 * D], F32)
    g.H = S.dram("H", [T, D], F32)
    g.Z = S.dram("Z", [T, D], F32)
    g.XBCT = S.dram("XBCT", [3072, T], F32)
    g.DTR = S.dram("DTR", [T, 64], F32)
    g.QT = S.dram("QT", [16, 128, T], BF16)
    g.QN = S.dram("QN", [16, 128, T], BF16)
    g.KT = S.dram("KT", [16, 128, T], BF16)
    g.Vd = S.dram("Vd", [T, D], BF16)
    g.GT = S.dram("GT", [4096, T], F32)
    g.dbg = {}
    return g


def load_consts(S, g):
    c = Ctx()
    c.identf = S.sb("identf", [128, 128], F32)
    c.identb = S.sb("identb", [128, 128], BF16)
    c.ones_b = S.sb("ones_b", [128, 128], BF16)
    c.ones_f = S.sb("ones_f", [128, 128], F32)
    S.dma("sp", c.identf[:], g.identf[:])
    S.copy("dve", c.identb[:], c.identf[:])
    S.memset("dve", c.ones_b[:], 1.0)
    S.memset("dve", c.ones_f[:], 1.0)
    c.eps_ln = S.sb("eps_ln", [128, 1], F32)
    c.eps_rms = S.sb("eps_rms", [128, 1], F32)
    S.memset("dve", c.eps_ln[:], LN_EPS)
    S.memset("dve", c.eps_rms[:], RMS_EPS)
    return c


def phase_mod(S, g, c, l):
    base = S.arena_off
    cT = S.sb("cT", [128, 32], F32)
    scT = S.sb("scT", [128, 32], BF16)
    S.dma("sp", cT[:], g.cT[:])
    S.act(scT[:], cT[:], AF.Silu)
    wb = [S.sb("wada%d" % i, [128, KC, 512], BF16) for i in range(2)]
    bb = [S.sb("bada%d" % i, [2, 512], F32) for i in range(2)]
    ob = [S.sb("oada%d" % i, [2, 512], F32) for i in range(2)]
    wv = g.w_ada.t[l].rearrange("(kc p) n -> p kc n", p=128)
    for nb in range(24):
        w = wb[nb % 2]
        S.dma("pool", w[:], g.w_ada.v(wv[:, :, nb * 512:(nb + 1) * 512]))
        S.dma("sp", bb[nb % 2][:], g.b_ada.v(g.b_ada.t[l, nb * 512:(nb + 1) * 512].partition_broadcast(2)))
        ps = S.bank(nb % 2)
        pv = V(ps.ap[0:2, :], ps.bufs)
        for kc in range(KC):
            S.mm(pv, scT[:, kc * 2:(kc + 1) * 2], w[:, kc, :], start=(kc == 0), stop=(kc == KC - 1))
        S.tt("dve", ob[nb % 2][:], pv, bb[nb % 2][:], ALU.add)
        S.dma("sp", g.MOD.v(g.MOD.t[l, :, nb * 512:(nb + 1) * 512]), ob[nb % 2][:])
    S.barrier()
    S.arena_reset(base)


def load_mod_tiles(S, g, l, seg, plus_one):
    res = []
    for row in range(2):
        t = S.sb("mod%d_%d" % (seg, row), [128, D], F32)
        S.dma("sp", t[:], g.MOD.v(g.MOD.t[l, row, seg * D:(seg + 1) * D].partition_broadcast(128)))
        if plus_one:
            S.ts("pool", t[:], t[:], 1.0, None, ALU.add)
        res.append(t)
    return res


def layer_norm_tile(S, c, xt, xn, stats, mv, rstd, nmr):
    for j in range(4):
        S.op("dve", lambda e, j=j: e.bn_stats(stats.t[:, j, :], xt.t[:, j * 512:(j + 1) * 512]),
             [xt[:]], [stats[:]])
    S.op("dve", lambda e: e.bn_aggr(mv.t[:], stats.t[:]), [stats[:]], [mv[:]])
    S.act(rstd[:], mv[:, 1:2], AF.Sqrt, bias=c.eps_ln[:])
    S.op("dve", lambda e: e.reciprocal(rstd.t[:], rstd.t[:]), [rstd[:]], [rstd[:]])
    S.ts("dve", nmr[:], mv[:, 0:1], -1.0, None, ALU.mult)
    S.tt("dve", nmr[:], nmr[:], rstd[:], ALU.mult)
    S.act(xn[:], xt[:], AF.Identity, bias=nmr[:], scale=rstd[:])


def phase_ln_mod(S, g, c, l, UT, seg_shift, seg_scale, src, router=None, flush=None):
    base = S.arena_off
    sc = load_mod_tiles(S, g, l, seg_scale, True)
    sh = load_mod_tiles(S, g, l, seg_shift, False)
    xts = [S.sb("xt%d" % i, [128, D], F32) for i in range(2)]
    xns = [S.sb("xn%d" % i, [128, D], F32) for i in range(2)]
    ubs = [S.sb("ub%d" % i, [128, D], BF16) for i in range(2)]
    stats = S.sb("stats", [128, 4, 6], F32)
    mv = S.sb("mv", [128, 2], F32)
    rstd = S.sb("rstd", [128, 1], F32)
    nmr = S.sb("nmr", [128, 1], F32)
    S.dma("sp", xts[0][:], src.v(src.t[0:128, :]))
    for t in range(NT):
        xt = xts[t % 2]
        if t + 1 < NT:
            S.dma("sp", xts[(t + 1) % 2][:], src.v(src.t[(t + 1) * 128:(t + 2) * 128, :]))
        row = 1 if t < 2 else 0
        xn = xns[t % 2]
        ub = ubs[t % 2]
        layer_norm_tile(S, c, xt, xn, stats, mv, rstd, nmr)
        S.tt("dve", xn[:], xn[:], sc[row][:], ALU.mult)
        S.tt("pool", xn[:], xn[:], sh[row][:], ALU.add)
        S.copy("act", ub[:], xn[:])
        if router is not None:
            router_tile(S, g, c, router, xn, t)
        for j in range(4):
            pb = S.bank(6 + (j % 2), dt=BF16)
            for i in range(4):
                kc = j * 4 + i
                S.tr(V(pb.ap[:, i * 128:(i + 1) * 128], pb.bufs), ub[:, kc * 128:(kc + 1) * 128], c.identb[:])
            src_v = V(pb.ap[:, 0:512].rearrange("p (a b) -> p a b", a=4), pb.bufs)
            dst_v = UT[:, j * 4:(j + 1) * 4, t * 128:(t + 1) * 128]
            S.copy("act" if j % 2 == 0 else "dve", dst_v, src_v)
    if flush is not None:
        for kc in range(KC):
            S.dma("sp", flush.v(flush.t[kc * 128:(kc + 1) * 128, :]), UT[:, kc, :])
    S.barrier()
    S.arena_reset(base)


def phase_inproj(S, g, c, l, UT):
    base = S.arena_off
    wbuf = [S.sb("win%d" % i, [128, KC, 512], BF16) for i in range(2)]
    stg = [S.sb("stg%d" % i, [128, 512], F32) for i in range(3)]
    stgb = [S.sb("stgb%d" % i, [128, 512], BF16) for i in range(2)]
    fstg = [S.sb("fstg%d" % i, [128, T], F32) for i in range(2)]
    rots = [S.sb("rot%d" % i, [128, 512], BF16) for i in range(2)]
    unr = S.sb("unr", [128, 512], BF16)
    tmp = [S.sb("rt%d" % i, [128, 256], F32) for i in range(4)]
    hst = S.sb("hst", [128, 4, T], BF16)
    hsn = S.sb("hsn", [128, 4, T], BF16)
    cosb = S.sb("cosb", [128, NT, 32], F32)
    sinb = S.sb("sinb", [128, NT, 32], F32)
    S.dma("sp", cosb[:], g.costab.v(g.costab.t.rearrange("(t p) f -> p t f", p=128)))
    S.dma("sp", sinb[:], g.sintab.v(g.sintab.t.rearrange("(t p) f -> p t f", p=128)))
    wv = g.w_in.t[l].rearrange("(kc p) n -> p kc n", p=128)

    blocks = []
    for i in range(4):
        blocks.append(("z", OFF_Z + i * 512, 512, i))
    for i in range(6):
        blocks.append(("xbc", OFF_XBC + i * 512, 512, i))
    blocks.append(("dt", OFF_DT, 64, 0))
    for i in range(4):
        blocks.append(("q", OFF_Q + i * 512, 512, i))
    for i in range(4):
        blocks.append(("qn", OFF_Q + i * 512, 512, i))
    for i in range(4):
        blocks.append(("k", OFF_K + i * 512, 512, i))
    for i in range(4):
        blocks.append(("v", OFF_V + i * 512, 512, i))
    for i in range(8):
        blocks.append(("g", OFF_G + i * 512, 512, i))

    def load_w(bi):
        kind, c0, ncol, i = blocks[bi]
        w = wbuf[bi % 2]
        S.dma("pool", w[:, :, 0:ncol], g.w_in.v(wv[:, :, c0:c0 + ncol]))

    load_w(0)
    pcnt = 0
    scnt = 0
    fcnt = 0
    for bi, (kind, c0, ncol, i) in enumerate(blocks):
        if bi + 1 < len(blocks):
            load_w(bi + 1)
        w = wbuf[bi % 2]
        if kind in ("z", "dt", "q", "qn", "k", "v"):
            deferred = []
            for t in range(NT):
                ps = S.bank(pcnt % 6)
                pcnt += 1
                pv = V(ps.ap[:, 0:ncol], ps.bufs)
                for kc in range(KC):
                    S.mm(pv, UT[:, kc, t * 128:(t + 1) * 128], w[:, kc, 0:ncol], start=(kc == 0), stop=(kc == KC - 1))
                if kind == "z":
                    s = stg[scnt % 3]
                    scnt += 1
                    S.copy("act" if t % 2 == 0 else "dve", s[:], pv)
                    S.dma("sp", g.Z.v(g.Z.t[t * 128:(t + 1) * 128, i * 512:(i + 1) * 512]), s[:])
                elif kind == "dt":
                    s = stg[scnt % 3]
                    scnt += 1
                    S.copy("act", s[:, 0:64], pv)
                    S.dma("sp", g.DTR.v(g.DTR.t[t * 128:(t + 1) * 128, :]), s[:, 0:64])
                elif kind == "v":
                    s = stgb[t % 2]
                    S.copy("act" if t % 2 == 0 else "dve", s[:], pv)
                    S.dma("sp", g.Vd.v(g.Vd.t[t * 128:(t + 1) * 128, i * 512:(i + 1) * 512]), s[:])
                else:
                    pr = ps.ap[:, 0:512].rearrange("p (a h u f) -> p a h u f", a=8, h=2, u=2)
                    u1 = V(pr[:, :, :, 0, :], ps.bufs)
                    u2 = V(pr[:, :, :, 1, :], ps.bufs)
                    tt_ = 0 if kind == "qn" else t
                    cosv = V(cosb.t[:, tt_, :].rearrange("p (h f) -> p h f", h=2).unsqueeze(1).to_broadcast([128, 8, 2, 16]), [cosb.buf])
                    sinv = V(sinb.t[:, tt_, :].rearrange("p (h f) -> p h f", h=2).unsqueeze(1).to_broadcast([128, 8, 2, 16]), [sinb.buf])
                    rot = rots[t % 2]
                    rr = rot.t[:, :].rearrange("p (a h u f) -> p a h u f", a=8, h=2, u=2)
                    o1 = V(rr[:, :, :, 0, :], [rot.buf])
                    o2 = V(rr[:, :, :, 1, :], [rot.buf])
                    tv = [V(x.t[:, :].rearrange("p (a h f) -> p a h f", a=8, h=2), [x.buf]) for x in tmp]
                    S.tt("dve", tv[0], u1, cosv, ALU.mult)
                    S.tt("dve", tv[1], u2, sinv, ALU.mult)
                    S.tt("pool", o1, tv[0], tv[1], ALU.subtract)
                    S.tt("dve", tv[2], u1, sinv, ALU.mult)
                    S.tt("dve", tv[3], u2, cosv, ALU.mult)
                    S.tt("pool", o2, tv[2], tv[3], ALU.add)
                    def trs(rot=rot, t=t):
                        pb = S.bank(6 + (t % 2), dt=BF16)
                        for hh in range(4):
                            S.tr(V(pb.ap[:, hh * 128:(hh + 1) * 128], pb.bufs), rot[:, hh * 128:(hh + 1) * 128], c.identb[:])
                        S.copy("act", hst[:, :, t * 128:(t + 1) * 128],
                               V(pb.ap[:, 0:512].rearrange("p (a b) -> p a b", a=4), pb.bufs))

                    while deferred:
                        deferred.pop(0)()
                    deferred.append(trs)
            while deferred:
                deferred.pop(0)()
            if kind in ("q", "k", "qn"):
                dst = {"q": g.QT, "k": g.KT, "qn": g.QN}[kind]
                for hh in range(4):
                    S.dma("sp", dst.v(dst.t[i * 4 + hh]), hst[:, hh, :])
        else:
            for m in range(4):
                fs = fstg[fcnt % 2]
                fcnt += 1
                for (t0, tn) in TG:
                    ps = S.bank(pcnt % 6)
                    pcnt += 1
                    pv = V(ps.ap[:, 0:tn], ps.bufs)
                    for kc in range(KC):
                        S.mm(pv, w[:, kc, m * 128:(m + 1) * 128], UT[:, kc, t0:t0 + tn], start=(kc == 0), stop=(kc == KC - 1))
                    if kind == "g":
                        S.act(fs[:, t0:t0 + tn], pv, AF.Sigmoid)
                    else:
                        S.copy("dve" if (pcnt % 2) else "act", fs[:, t0:t0 + tn], pv)
                dst = g.GT if kind == "g" else g.XBCT
                r0 = i * 512 + m * 128
                S.dma("sp", dst.v(dst.t[r0:r0 + 128, :]), fs[:])
    S.barrier()
    S.arena_reset(base)


def declare_ssd_scratch(S, g):
    g.Xtok = S.dram("Xtok", [T, D], BF16)
    g.Btok = S.dram("Btok", [T, 512], BF16)
    g.BT = S.dram("BT", [512, T], BF16)
    g.CT = S.dram("CT", [512, T], BF16)
    g.ST = S.dram("ST", [2 * NT * 128, D], BF16)
    g.YST = S.dram("YST", [D, T], BF16)
    g.YAT = S.dram("YAT", [D, T], BF16)
    g.YDBG = S.dram("YDBG", [T, D], F32)
    g.DTVd = S.dram("DTVd", [128, NT * 64], F32)
    g.ADTd = S.dram("ADTd", [128, NT * 64], F32)


def phase_ssdprep(S, g, c, l, DTV, ADT):
    base = S.arena_off
    XC = S.sb("XC", [128, 20, T], BF16)
    cw = S.sb("convw", [128, 120], F32)
    cb = S.sb("convb", [128, 24], F32)
    S.dma("sp", cw[:], g.convw.v(g.convw.t[l]))
    S.dma("sp", cb[:], g.convb.v(g.convb.t[l]))
    xin = [S.sb("cin%d" % i, [128, T], F32) for i in range(2)]
    acc = S.sb("cacc", [128, T], F32)
    cstg = [S.sb("cstg%d" % i, [128, T], BF16) for i in range(2)]
    S.dma("sp", xin[0][:], g.XBCT.v(g.XBCT.t[0:128, :]))
    segs = [(0, NCTX), (NCTX, T)]
    for cc in range(24):
        xi = xin[cc % 2]
        if cc + 1 < 24:
            S.dma("sp", xin[(cc + 1) % 2][:], g.XBCT.v(g.XBCT.t[(cc + 1) * 128:(cc + 2) * 128, :]))
        S.act(acc[:], xi[:], AF.Identity, bias=cb[:, cc:cc + 1], scale=cw[:, cc * 5 + 2:cc * 5 + 3])
        for j in (0, 1, 3, 4):
            s = j - 2
            for (a, b) in segs:
                lo = max(a, a - s)
                hi = min(b, b - s)
                S.stt("dve", acc[:, lo:hi], xi[:, lo + s:hi + s], cw[:, cc * 5 + j:cc * 5 + j + 1],
                      acc[:, lo:hi], ALU.mult, ALU.add)
        if cc < 20:
            S.act(XC[:, cc, :], acc[:], AF.Silu)
            if cc >= 16:
                S.dma("sp", g.BT.v(g.BT.t[(cc - 16) * 128:(cc - 15) * 128, :]), XC[:, cc, :])
        else:
            st = cstg[cc % 2]
            S.act(st[:], acc[:], AF.Silu)
            S.dma("sp", g.CT.v(g.CT.t[(cc - 20) * 128:(cc - 19) * 128, :]), st[:])
    xt = [S.sb("xtok%d" % i, [128, 2560], BF16) for i in range(2)]
    for t in range(NT):
        o = xt[t % 2]
        for j in range(5):
            pb = S.bank(6 + (j % 2), dt=BF16)
            for i in range(4):
                S.tr(V(pb.ap[:, i * 128:(i + 1) * 128], pb.bufs), XC[:, j * 4 + i, t * 128:(t + 1) * 128], c.identb[:])
            S.copy("act" if j % 2 == 0 else "dve", o[:, j * 512:(j + 1) * 512], V(pb.ap[:, 0:512], pb.bufs))
        S.dma("sp", g.Xtok.v(g.Xtok.t[t * 128:(t + 1) * 128, :]), o[:, 0:2048])
        S.dma("sp", g.Btok.v(g.Btok.t[t * 128:(t + 1) * 128, :]), o[:, 2048:2560])
    dtr = S.sb("dtr", [128, NT, 64], F32)
    tb = S.sb("dtbb", [128, 64], F32)
    al = S.sb("alb", [128, 64], F32)
    t1 = S.sb("dt1", [128, NT, 64], F32)
    S.dma("sp", dtr[:], g.DTR.v(g.DTR.t.rearrange("(t p) f -> p t f", p=128)))
    S.dma("sp", tb[:], g.dtb.v(g.dtb.t[l].partition_broadcast(128)))
    S.dma("sp", al[:], g.alog.v(g.alog.t[l].partition_broadcast(128)))
    tbb = V(tb.t[:, :].unsqueeze(1).to_broadcast([128, NT, 64]), [tb.buf])
    S.tt("dve", dtr[:], dtr[:], tbb, ALU.add)
    S.act(t1[:], dtr[:], AF.Abs)
    S.act(t1[:], t1[:], AF.Exp, scale=-1.0)
    S.act(t1[:], t1[:], AF.Ln, bias=c.ones_f[:, 0:1])
    S.stt("dve", DTV[:], dtr[:], 0.0, t1[:], ALU.max, ALU.add)
    S.act(al[:], al[:], AF.Exp)
    S.ts("dve", al[:], al[:], -1.0, None, ALU.mult)
    alb = V(al.t[:, :].unsqueeze(1).to_broadcast([128, NT, 64]), [al.buf])
    S.tt("dve", ADT[:], DTV[:], alb, ALU.mult)
    if "DTVd" in DBG_EXT:
        S.dma("sp", g.DTVd[:], V(DTV.t[:, :, :].rearrange("p a b -> p (a b)"), [DTV.buf]))
        S.dma("sp", g.ADTd[:], V(ADT.t[:, :, :].rearrange("p a b -> p (a b)"), [ADT.buf]))
    S.barrier()
    S.arena_reset(base)


def phase_ssd_states(S, g, c, l, DTV, ADT):
    base = S.arena_off
    tri = [S.sb("tri%d" % d, [128, 128], F32) for d in range(2)]
    for d in range(2):
        S.dma("sp", tri[d][:], g.tri.v(g.tri.t[d]))
    St = S.sb("Sstate", [128, D], F32)
    Sb = [S.sb("Sbf%d" % i, [128, D], BF16) for i in range(2)]
    xs = [S.sb("xs%d" % i, [128, D], BF16) for i in range(2)]
    bs = [S.sb("bs%d" % i, [128, 512], BF16) for i in range(2)]
    xdds = [S.sb("xdd%d" % i, [128, D], BF16) for i in range(2)]
    css = [S.sb("cs%d" % i, [128, 32], F32) for i in range(2)]
    dfs = [S.sb("df%d" % i, [128, 32], F32) for i in range(2)]
    dtes = [S.sb("dte%d" % i, [128, 32], F32) for i in range(2)]
    dchs = [S.sb("dch%d" % i, [128, 32], F32) for i in range(2)]
    it = 0
    for d in range(2):
        order = list(range(NT)) if d == 0 else [1, 0] + list(range(NT - 1, 1, -1))
        S.memset("pool", St[:], 0.0)
        for ch in order:
            x = xs[it % 2]
            b = bs[it % 2]
            sb_ = Sb[it % 2]
            xdd, cs, df, dte, dch = xdds[it % 2], css[it % 2], dfs[it % 2], dtes[it % 2], dchs[it % 2]
            pmb = 4 + (it % 2)
            it += 1
            S.dma("sp", x[:], g.Xtok.v(g.Xtok.t[ch * 128:(ch + 1) * 128, :]))
            S.dma("sp", b[:], g.Btok.v(g.Btok.t[ch * 128:(ch + 1) * 128, :]))
            adt = ADT[:, ch, d * 32:(d + 1) * 32]
            pm = S.bank(pmb)
            pcs = V(pm.ap[:, 0:32], pm.bufs)
            pend = V(pm.ap[:, 32:64], pm.bufs)
            S.mm(pcs, tri[d][:], adt)
            S.mm(pend, c.ones_f[:], adt)
            S.copy("act", cs[:], pcs)
            S.tt("dve", df[:], pend, cs[:], ALU.subtract)
            S.act(df[:], df[:], AF.Exp)
            S.tt("dve", dte[:], df[:], DTV[:, ch, d * 32:(d + 1) * 32], ALU.mult)
            S.act(dch[:], pend, AF.Exp)
            xv = V(x.t[:, :].rearrange("p (h q) -> p h q", h=32), [x.buf])
            xo = V(xdd.t[:, :].rearrange("p (h q) -> p h q", h=32), [xdd.buf])
            S.tt("dve", xo, xv, V(dte.t[:, :].unsqueeze(2).to_broadcast([128, 32, 64]), [dte.buf]), ALU.mult)
            S.copy("act", sb_[:], St[:])
            r0 = (d * NT + ch) * 128
            S.dma("sp", g.ST.v(g.ST.t[r0:r0 + 128, :]), sb_[:])
            ps = S.bank(0, 4)
            for gi in range(4):
                S.mm(V(ps.ap[:, gi * 512:(gi + 1) * 512], ps.bufs[gi:gi + 1]), b[:, gi * 128:(gi + 1) * 128], xdd[:, gi * 512:(gi + 1) * 512])
            sv = V(St.t[:, :].rearrange("p (h q) -> p h q", h=32), [St.buf])
            S.tt("pool", sv, sv, V(dch.t[:, :].unsqueeze(2).to_broadcast([128, 32, 64]), [dch.buf]), ALU.mult)
            S.tt("dve", St[:], St[:], ps, ALU.add)
    S.barrier()
    S.arena_reset(base)


def phase_ssd_y(S, g, c, l, DTV, ADT):
    base = S.arena_off
    YST = S.sb("YSTres", [128, KC, T], BF16)
    tri = [S.sb("tri%d" % d, [128, 128], F32) for d in range(2)]
    negm = [S.sb("negm%d" % d, [128, 128], F32) for d in range(2)]
    for d in range(2):
        S.dma("sp", tri[d][:], g.tri.v(g.tri.t[d]))
        S.dma("sp", negm[d][:], g.negm.v(g.negm.t[d]))
    BT = S.sb("BTres", [128, 4, T], BF16)
    CT = S.sb("CTres", [128, 4, T], BF16)
    for gi in range(4):
        S.dma("sp", BT[:, gi, :], g.BT.v(g.BT.t[gi * 128:(gi + 1) * 128, :]))
        S.dma("sp", CT[:, gi, :], g.CT.v(g.CT.t[gi * 128:(gi + 1) * 128, :]))
    dsk = S.sb("dsk", [128, 32], F32)
    S.dma("sp", dsk[:], g.ssmd.v(g.ssmd.t[l].partition_broadcast(128)))
    ng = S.sb("normg", [128, D], F32)
    S.dma("sp", ng[:], g.normg.v(g.normg.t[l].partition_broadcast(128)))
    xs = [S.sb("yx%d" % i, [128, D], BF16) for i in range(2)]
    sts = [[S.sb("yst%d_%d" % (d, i), [128, D], BF16) for i in range(2)] for d in range(2)]
    zs = [S.sb("yz%d" % i, [128, D], F32) for i in range(2)]
    xdt = [S.sb("xdt%d" % d, [128, D], BF16) for d in range(2)]
    cs = S.sb("ycs", [128, 64], F32)
    ecs = S.sb("yecs", [128, 64], F32)
    CBT = S.sb("CBT", [128, 4, 128], F32)
    R = [S.sb("R%d" % i, [128, 4, 128], F32) for i in range(2)]
    Dm = [S.sb("Dm%d" % i, [128, 4, 128], F32) for i in range(2)]
    MT = [S.sb("MT%d" % i, [128, 4, 128], BF16) for i in range(2)]
    yacc = S.sb("yacc", [128, D], F32)
    t2 = S.sb("yt2", [128, 1024], F32)
    ybf = S.sb("ybf", [128, D], BF16)
    ss = S.sb("yss", [128, 4], F32)
    junk = S.sb("yjunk", [128, 512], F32)

    def loads(ch, i):
        S.dma("sp", xs[i][:], g.Xtok.v(g.Xtok.t[ch * 128:(ch + 1) * 128, :]))
        for d in range(2):
            r0 = (d * NT + ch) * 128
            S.dma("sp", sts[d][i][:], g.ST.v(g.ST.t[r0:r0 + 128, :]))
        S.dma("sp", zs[i][:], g.Z.v(g.Z.t[ch * 128:(ch + 1) * 128, :]))

    loads(0, 0)
    gcnt = 0
    for ch in range(NT):
        i = ch % 2
        if ch + 1 < NT:
            loads(ch + 1, (ch + 1) % 2)
        x = xs[i]
        z = zs[i]
        tsl = slice(ch * 128, (ch + 1) * 128)
        pm = S.bank(6)
        for d in range(2):
            S.mm(V(pm.ap[:, d * 32:(d + 1) * 32], pm.bufs), tri[d][:], ADT[:, ch, d * 32:(d + 1) * 32])
        S.copy("act", cs[:], V(pm.ap[:, 0:64], pm.bufs))
        S.act(ecs[:], V(pm.ap[:, 0:64], pm.bufs), AF.Exp)
        pc = S.bank(7)
        for gi in range(4):
            S.mm(V(pc.ap[:, gi * 128:(gi + 1) * 128], pc.bufs), BT[:, gi, tsl], CT[:, gi, tsl])
        S.copy("dve", V(CBT.t[:, :, :].rearrange("p a b -> p (a b)"), [CBT.buf]), pc)
        xv = V(x.t[:, :].rearrange("p (h q) -> p h q", h=32), [x.buf])
        for d in range(2):
            xo = V(xdt[d].t[:, :].rearrange("p (h q) -> p h q", h=32), [xdt[d].buf])
            dv = V(DTV.t[:, ch, d * 32:(d + 1) * 32].unsqueeze(2).to_broadcast([128, 32, 64]), [DTV.buf])
            S.tt("pool", xo, xv, dv, ALU.mult)
        for half in range(2):
            yp = S.bank(0, 2)
            for jj in range(4):
                j = half * 4 + jj
                gi = j // 2
                for d in range(2):
                    k = gcnt % 2
                    gcnt += 1
                    a4 = V(ADT.t[:, ch, d * 32 + 4 * j:d * 32 + 4 * j + 4].unsqueeze(2).to_broadcast([128, 4, 128]), [ADT.buf])
                    tb = V(tri[d].t[:, :].unsqueeze(1).to_broadcast([128, 4, 128]), [tri[d].buf])
                    S.tt("dve", R[k][:], tb, a4, ALU.mult)
                    dp = S.bank(4 + k)
                    S.mm(dp, c.ones_f[:], V(R[k].t[:, :, :].rearrange("p a b -> p (a b)"), [R[k].buf]))
                    for ii in range(4):
                        h = 4 * j + ii
                        S.stt("dve", Dm[k][:, ii, :], V(dp.ap[:, ii * 128:(ii + 1) * 128], dp.bufs),
                              cs[:, d * 32 + h:d * 32 + h + 1], negm[d][:], ALU.subtract, ALU.add)
                    S.act(Dm[k][:], Dm[k][:], AF.Exp)
                    cb = V(CBT.t[:, gi, :].unsqueeze(1).to_broadcast([128, 4, 128]), [CBT.buf])
                    S.tt("pool", MT[d][:], Dm[k][:], cb, ALU.mult)
                for ii in range(4):
                    h = 4 * j + ii
                    hl = h - half * 16
                    for d in range(2):
                        S.mm(V(yp.ap[:, hl * 64:(hl + 1) * 64], yp.bufs[hl // 8:hl // 8 + 1]), MT[d][:, ii, :],
                             xdt[d][:, h * 64:(h + 1) * 64], start=(d == 0), stop=(d == 1))
            ya = yacc[:, half * 1024:(half + 1) * 1024]
            ya3 = V(yacc.t[:, half * 1024:(half + 1) * 1024].rearrange("p (h q) -> p h q", h=16), [yacc.buf])
            for d in range(2):
                yo = S.bank(2, 2)
                for gg in range(2):
                    gi = half * 2 + gg
                    S.mm(V(yo.ap[:, gg * 512:(gg + 1) * 512], yo.bufs[gg:gg + 1]), CT[:, gi, tsl],
                         sts[d][i][:, gi * 512:(gi + 1) * 512])
                ev = V(ecs.t[:, d * 32 + half * 16:d * 32 + half * 16 + 16].unsqueeze(2).to_broadcast([128, 16, 64]), [ecs.buf])
                yo3 = V(yo.ap.rearrange("p (h q) -> p h q", h=16), yo.bufs)
                if d == 0:
                    S.tt("dve", ya3, yo3, ev, ALU.mult)
                else:
                    t23 = V(t2.t[:, :].rearrange("p (h q) -> p h q", h=16), [t2.buf])
                    S.tt("dve", t23, yo3, ev, ALU.mult)
                    S.tt("pool", ya, ya, t2[:], ALU.add)
            S.tt("dve", ya, ya, yp, ALU.add)
        if "YDBG" in DBG_EXT:
            S.dma("sp", g.YDBG.v(g.YDBG.t[ch * 128:(ch + 1) * 128, :]), yacc[:])
        y3 = V(yacc.t[:, :].rearrange("p (h q) -> p h q", h=32), [yacc.buf])
        dk = V(dsk.t[:, :].unsqueeze(2).to_broadcast([128, 32, 64]), [dsk.buf])
        xf = V(z.t[:, :].rearrange("p (h q) -> p h q", h=32), [z.buf])
        S.act(z[:], z[:], AF.Silu)
        S.tt("dve", yacc[:], yacc[:], z[:], ALU.mult)
        t3 = V(junk.t[:, :], [junk.buf])
        for q4 in range(4):
            xq = V(x.t[:, q4 * 512:(q4 + 1) * 512].rearrange("p (h q) -> p h q", h=8), [x.buf])
            dq = V(dsk.t[:, q4 * 8:(q4 + 1) * 8].unsqueeze(2).to_broadcast([128, 8, 64]), [dsk.buf])
            j3 = V(junk.t[:, :].rearrange("p (h q) -> p h q", h=8), [junk.buf])
            S.tt("pool", j3, xq, dq, ALU.mult)
            S.tt("pool", junk[:], junk[:], z[:, q4 * 512:(q4 + 1) * 512], ALU.mult)
            S.tt("dve", yacc[:, q4 * 512:(q4 + 1) * 512], yacc[:, q4 * 512:(q4 + 1) * 512], junk[:], ALU.add)
        S.memset("dve", ss[:], 0.0)
        for gi in range(4):
            S.act(junk[:], yacc[:, gi * 512:(gi + 1) * 512], AF.Square, accum_out=ss[:, gi:gi + 1])
        S.act(ss[:], ss[:], AF.Sqrt, bias=c.eps_rms[:], scale=1.0 / 512)
        S.op("dve", lambda e: e.reciprocal(ss.t[:], ss.t[:]), [ss[:]], [ss[:]])
        for gi in range(4):
            S.act(yacc[:, gi * 512:(gi + 1) * 512], yacc[:, gi * 512:(gi + 1) * 512], AF.Copy, scale=ss[:, gi:gi + 1])
        S.tt("dve", ybf[:], yacc[:], ng[:], ALU.mult)
        for j in range(4):
            pb = S.bank(6 + (j % 2), dt=BF16)
            for ii in range(4):
                kc = j * 4 + ii
                S.tr(V(pb.ap[:, ii * 128:(ii + 1) * 128], pb.bufs), ybf[:, kc * 128:(kc + 1) * 128], c.identb[:])
            S.copy("act", YST[:, j * 4:(j + 1) * 4, tsl], V(pb.ap[:, 0:512].rearrange("p (a b) -> p a b", a=4), pb.bufs))
    for kc in range(KC):
        S.dma("sp", g.YST.v(g.YST.t[kc * 128:(kc + 1) * 128, :]), YST[:, kc, :])
    S.barrier()
    S.arena_reset(base)


def phase_attn(S, g, c, l, last=False):
    import math as _m
    base = S.arena_off
    lam_init = 0.8 - 0.6 * _m.exp(-0.3 * l)
    lv = S.sb("lamv", [128, 256], F32)
    S.dma("sp", lv[:], g.lamv.v(g.lamv.t[l].partition_broadcast(128)))
    pr = S.sb("lampr", [128, 128], F32)
    sm = S.sb("lamsm", [128, 2], F32)
    neglam = S.sb("neglam", [128, 1], F32)
    gsc = S.sb("gsc", [128, 1], F32)
    S.tt("dve", pr[:, 0:64], lv[:, 0:64], lv[:, 64:128], ALU.mult)
    S.tt("dve", pr[:, 64:128], lv[:, 128:192], lv[:, 192:256], ALU.mult)
    for i in range(2):
        S.op("dve", lambda e, i=i: e.reduce_sum(sm.t[:, i:i + 1], pr.t[:, i * 64:(i + 1) * 64], AX.X), [pr[:]], [sm[:]])
    S.act(sm[:], sm[:], AF.Exp)
    S.tt("dve", neglam[:], sm[:, 1:2], sm[:, 0:1], ALU.subtract)
    S.ts("dve", neglam[:], neglam[:], -lam_init, None, ALU.add)
    S.dma("sp", gsc[:], g.subg.v(g.subg.t[l]))
    S.ts("dve", gsc[:], gsc[:], 1.0 - lam_init, None, ALU.mult)

    KTh = [S.sb("KTh%d" % i, [128, T], BF16) for i in range(2)]
    QTh = [S.sb("QTh%d" % i, [128, T], BF16) for i in range(2)]
    QNh = [S.sb("QNh%d" % i, [128, T], BF16) for i in range(2)]
    Vh = [S.sb("Vh%d" % i, [128, NT, 128], BF16) for i in range(2)]
    YA = [S.sb("YAh%d" % i, [128, T], BF16) for i in range(2)]
    psum_acc = [S.sb("pacc%d" % m, [128, 512], F32) for m in range(2)]
    psum_acc2 = [S.sb("paccp%d" % m, [128, 512], F32) for m in range(2)]
    PT = [S.sb("PT%d" % i, [128, 512], BF16) for i in range(9)]
    r = [S.sb("ar%d" % i, [128, 512], F32) for i in range(2)]
    tt_ = [S.sb("at%d" % i, [128, 512], F32) for i in range(2)]
    o2 = [S.sb("ao%d" % i, [128, 512], F32) for i in range(2)]
    tt2 = [[S.sb("at%d_%d" % (i, j), [128, 512], F32) for j in range(2)] for i in range(2)]
    pending = []
    qcnt = 0
    sq = S.sb("asq", [128, 512], BF16)
    rs = S.sb("ars", [128, 512], F32)

    def loads(h, i):
        S.dma("sp", KTh[i][:], g.KT.v(g.KT.t[h]))
        S.dma("sp", QTh[i][:], g.QT.v(g.QT.t[h]))
        S.dma("sp", QNh[i][:], g.QN.v(g.QN.t[h]))
        S.dma("sp", Vh[i][:], g.Vd.v(g.Vd.t[:, h * 128:(h + 1) * 128].rearrange("(kc p) e -> p kc e", p=128)))

    loads(0, 0)
    pcnt = 0
    bcnt = 0
    qgroups = [(NCTX + i * 512, 512, list(range(NT))) for i in range(4)] + ([] if last else [(0, NCTX, [0, 1])])
    for h in range(16):
        i = h % 2
        if h + 1 < 16:
            loads(h + 1, (h + 1) % 2)
        for (q0, nq, kcs) in qgroups:
            o = o2[qcnt % 2]
            tt_ = tt2[qcnt % 2]
            qcnt += 1
            BS = 3
            batches = []
            for m in range(2):
                for b0 in range(0, len(kcs), BS):
                    batches.append([(m, ki, kcs[ki]) for ki in range(b0, min(b0 + BS, len(kcs)))])
            obv = []
            for m in range(2):
                ob = S.bank(6 + m)
                obv.append(V(ob.ap[:, 0:nq], ob.bufs))
            pv_order = {0: [], 1: []}
            for bt in batches:
                for (m, ki, kc) in reversed(bt):
                    pv_order[m].append(ki)
            pts = {}

            def norm_map(m, nq=nq, obv=obv, tt_=tt_):
                sbk = S.bank(m)
                sv = V(sbk.ap[:, 0:nq], sbk.bufs)
                S.mm(sv, c.ones_f[:], psum_acc[m][:, 0:nq], start=True, stop=False)
                S.mm(sv, c.ones_f[:], psum_acc2[m][:, 0:nq], start=False, stop=True)
                S.op("dve", lambda e, m=m, sv=sv, nq=nq: e.reciprocal(r[m].t[:, 0:nq], sv.ap), [sv], [r[m][:]])
                S.tt("dve", tt_[m][:, 0:nq], obv[m], r[m][:, 0:nq], ALU.mult)

            def emit_pv(bt):
                for (m, ki, kc) in reversed(bt):
                    p = pts.pop((m, ki))
                    S.mm(obv[m], Vh[i][:, kc, :], p[:, 0:nq], start=(ki == pv_order[m][0]), stop=(ki == pv_order[m][-1]))

            for bi, bt in enumerate(batches):
                sset = (bcnt % 2) * 3
                bcnt += 1
                stvs = {}
                for j, (m, ki, kc) in reversed(list(enumerate(bt))):
                    ps_ = slice(m * 64, (m + 1) * 64)
                    st = S.bank(sset + j)
                    stv = V(st.ap[:, 0:nq], st.bufs)
                    stvs[j] = stv
                    qsrc = QNh[i] if kc < 2 else QTh[i]
                    S.mm(stv, KTh[i][ps_, kc * 128:(kc + 1) * 128], qsrc[ps_, q0:q0 + nq])
                for j, (m, ki, kc) in enumerate(bt):
                    p = PT[pcnt % 9]
                    pcnt += 1
                    S.act(p[:, 0:nq], stvs[j], AF.Exp, scale=0.125)
                    pts[(m, ki)] = p
                    if ki == 0:
                        S.copy("dve", psum_acc[m][:, 0:nq], p[:, 0:nq])
                    elif ki == 1:
                        S.copy("pool", psum_acc2[m][:, 0:nq], p[:, 0:nq])
                    elif ki % 3 == 1:
                        S.tt("pool", psum_acc2[m][:, 0:nq], psum_acc2[m][:, 0:nq], p[:, 0:nq], ALU.add)
                    else:
                        S.tt("dve", psum_acc[m][:, 0:nq], psum_acc[m][:, 0:nq], p[:, 0:nq], ALU.add)
                if bi >= 1:
                    emit_pv(batches[bi - 1])
                    if batches[bi - 1][-1][0] == 0 and bt[0][0] == 1:
                        norm_map(0)
                if bi == 1 and pending:
                    pending.pop(0)()
            emit_pv(batches[-1])
            norm_map(1)
            if pending:
                pending.pop(0)()
            S.stt("dve", o[:, 0:nq], tt_[1][:, 0:nq], neglam[:], tt_[0][:, 0:nq], ALU.mult, ALU.add)

            def tail(o=o, nq=nq, q0=q0, i=i):
                S.act(sq[:, 0:nq], o[:, 0:nq], AF.Square)
                mb = S.bank(2)
                mbv = V(mb.ap[:, 0:nq], mb.bufs)
                S.mm(mbv, c.ones_b[:], sq[:, 0:nq])
                S.act(rs[:, 0:nq], mbv, AF.Sqrt, bias=c.eps_rms[:], scale=1.0 / 128)
                S.op("dve", lambda e, nq=nq: e.reciprocal(rs.t[:, 0:nq], rs.t[:, 0:nq]), [rs[:]], [rs[:]])
                S.tt("pool", o[:, 0:nq], o[:, 0:nq], rs[:, 0:nq], ALU.mult)
                S.act(YA[i][:, q0:q0 + nq], o[:, 0:nq], AF.Copy, scale=gsc[:])

            pending.append(tail)
        while pending:
            pending.pop(0)()
        S.dma("sp", g.YAT.v(g.YAT.t[h * 128:(h + 1) * 128, :]), YA[i][:])
    S.barrier()
    S.arena_reset(base)


def declare_merge_scratch(S, g):
    g.MTd = S.dram("MTd", [D, T], BF16)
    g.U2T = S.dram("U2T", [D, T], BF16)
    g.YM = S.dram("YM", [T, D], F32)


def phase_merge(S, g, c, l):
    base = S.arena_off
    YS = S.sb("YSr", [128, KC, T], BF16)
    YA = S.sb("YAr", [128, KC, T], BF16)
    for kc in range(KC):
        S.dma("sp", YS[:, kc, :], g.YST.v(g.YST.t[kc * 128:(kc + 1) * 128, :]))
        S.dma("sp", YA[:, kc, :], g.YAT.v(g.YAT.t[kc * 128:(kc + 1) * 128, :]))
    ws = [S.sb("wbs%d" % i, [128, KC, 128], BF16) for i in range(2)]
    wa = [S.sb("wba%d" % i, [128, KC, 128], BF16) for i in range(2)]
    gs = S.sb("ggs", [128, T], F32)
    ga = S.sb("gga", [128, T], F32)
    m1 = [S.sb("mm1_%d" % i, [128, 512], F32) for i in range(2)]
    m2 = [S.sb("mm2_%d" % i, [128, 512], F32) for i in range(2)]
    mrow = [S.sb("mrow%d" % i, [128, T], BF16) for i in range(2)]
    wsv = g.w_br_ssm.t[l].rearrange("(kc p) n -> p kc n", p=128)
    wav = g.w_br_att.t[l].rearrange("(kc p) n -> p kc n", p=128)

    def loadw(cc):
        S.dma("pool", ws[cc % 2][:], g.w_br_ssm.v(wsv[:, :, cc * 128:(cc + 1) * 128]))
        S.dma("pool", wa[cc % 2][:], g.w_br_att.v(wav[:, :, cc * 128:(cc + 1) * 128]))

    loadw(0)
    cnt = 0
    for cc in range(16):
        if cc + 1 < 16:
            loadw(cc + 1)
        S.dma("sp", gs[:], g.GT.v(g.GT.t[cc * 128:(cc + 1) * 128, :]))
        S.dma("sp", ga[:], g.GT.v(g.GT.t[D + cc * 128:D + (cc + 1) * 128, :]))
        mr = mrow[cc % 2]
        for (t0, tn) in TG:
            k = cnt % 2
            cnt += 1
            p1 = S.bank(2 * k)
            p2 = S.bank(2 * k + 1)
            p1v = V(p1.ap[:, 0:tn], p1.bufs)
            p2v = V(p2.ap[:, 0:tn], p2.bufs)
            for kc in range(KC):
                S.mm(p1v, ws[cc % 2][:, kc, :], YS[:, kc, t0:t0 + tn], start=(kc == 0), stop=(kc == KC - 1))
            for kc in range(KC):
                S.mm(p2v, wa[cc % 2][:, kc, :], YA[:, kc, t0:t0 + tn], start=(kc == 0), stop=(kc == KC - 1))
            S.tt("dve", m1[k][:, 0:tn], p1v, gs[:, t0:t0 + tn], ALU.mult)
            S.tt("dve", m2[k][:, 0:tn], p2v, ga[:, t0:t0 + tn], ALU.mult)
            S.tt("pool", mr[:, t0:t0 + tn], m1[k][:, 0:tn], m2[k][:, 0:tn], ALU.add)
        S.dma("sp", g.MTd.v(g.MTd.t[cc * 128:(cc + 1) * 128, :]), mr[:])
    S.barrier()
    S.arena_reset(base)


def bcast_tile(S, name, dram_tl, ap1d):
    t = S.sb(name, [128, D], F32)
    S.dma("sp", t[:], dram_tl.v(ap1d.partition_broadcast(128)))
    return t


def phase_outproj(S, g, c, l, src):
    base = S.arena_off
    wo = S.sb("wout", [128, KC, D], BF16)
    wov = g.w_out.t[l].rearrange("(kc p) n -> p kc n", p=128)
    for n in range(4):
        S.dma("pool", wo[:, :, n * 512:(n + 1) * 512], g.w_out.v(wov[:, :, n * 512:(n + 1) * 512]))
    g1 = load_mod_tiles(S, g, l, 2, False)
    lg = bcast_tile(S, "ln1g", g.ln1g, g.ln1g.t[l])
    lb = bcast_tile(S, "ln1b", g.ln1b, g.ln1b.t[l])
    mts = [S.sb("mt%d" % i, [128, KC, 128], BF16) for i in range(2)]
    hs = [S.sb("hh%d" % i, [128, D], F32) for i in range(2)]
    t1s = [S.sb("ot1_%d" % i, [128, D], F32) for i in range(2)]
    xns = [S.sb("oxn%d" % i, [128, D], F32) for i in range(2)]
    stats = S.sb("ostats", [128, 4, 6], F32)
    mv = S.sb("omv", [128, 2], F32)
    rstd = S.sb("orstd", [128, 1], F32)
    nmr = S.sb("onmr", [128, 1], F32)
    mtv = g.MTd.t.rearrange("(cc p) t -> p cc t", p=128)

    def loads(t):
        S.dma("sp", mts[t % 2][:], g.MTd.v(mtv[:, :, t * 128:(t + 1) * 128]))
        S.dma("sp", hs[t % 2][:], src.v(src.t[t * 128:(t + 1) * 128, :]))

    loads(0)
    for t in range(NT):
        if t + 1 < NT:
            loads(t + 1)
        row = 1 if t < 2 else 0
        mt = mts[t % 2]
        h = hs[t % 2]
        t1 = t1s[t % 2]
        xn = xns[t % 2]
        po = S.bank((t % 2) * 4, 4)
        for n in range(4):
            for cc in range(KC):
                S.mm(V(po.ap[:, n * 512:(n + 1) * 512], po.bufs[n:n + 1]), mt[:, cc, :], wo[:, cc, n * 512:(n + 1) * 512],
                     start=(cc == 0), stop=(cc == KC - 1))
        S.tt("dve", t1[:], po, g1[row][:], ALU.mult)
        S.stt("dve", t1[:], h[:], float(ALPHA), t1[:], ALU.mult, ALU.add)
        layer_norm_tile(S, c, t1, xn, stats, mv, rstd, nmr)
        S.tt("pool", xn[:], xn[:], lg[:], ALU.mult)
        S.tt("pool", xn[:], xn[:], lb[:], ALU.add)
        S.dma("sp", g.H.v(g.H.t[t * 128:(t + 1) * 128, :]), xn[:])
    S.barrier()
    S.arena_reset(base)


def router_tile(S, g, c, rt, xn, t):
    u2Tf = rt.u2Tf
    for j in range(4):
        pb = S.bank(4 + (j % 2))
        for i in range(4):
            kc = j * 4 + i
            S.tr(V(pb.ap[:, i * 128:(i + 1) * 128], pb.bufs), xn[:, kc * 128:(kc + 1) * 128], c.identf[:])
        S.copy("dve", u2Tf[:, j * 4:(j + 1) * 4, :], V(pb.ap[:, 0:512].rearrange("p (a b) -> p a b", a=4), pb.bufs))
    pl = S.bank(3)
    plv = V(pl.ap[:, 0:36], pl.bufs)
    for kc in range(KC):
        S.mm(plv, u2Tf[:, kc, :], rt.wr[:, kc, :], start=(kc == 0), stop=(kc == KC - 1))
    lg = rt.lg
    S.tt("dve", lg[:], plv, rt.brb[:], ALU.add)
    mx = rt.s[:, 0:1]
    s4 = rt.s[:, 1:2]
    pg = rt.s[:, 2:3]
    m1 = rt.s[:, 3:4]
    m2 = rt.s[:, 4:5]
    e2 = rt.s[:, 5:6]
    w1 = rt.s[:, 6:7]
    w2 = rt.s[:, 7:8]
    nmx = rt.s[:, 8:9]
    S.op("dve", lambda e: e.reduce_max(rt.s.t[:, 0:1], lg.t[:, 0:4], AX.X), [lg[:]], [rt.s[:]])
    S.ts("dve", nmx, mx, -1.0, None, ALU.mult)
    S.memset("dve", s4, 0.0)
    S.act(rt.e4[:], lg[:, 0:4], AF.Exp, bias=nmx, accum_out=s4)
    S.op("dve", lambda e: e.reciprocal(rt.s.t[:, 2:3], rt.s.t[:, 1:2]), [rt.s[:]], [rt.s[:]])
    S.ts("dve", rt.oh4[:], lg[:, 0:4], mx, None, ALU.is_equal)
    le = V(lg.t[:, 4:36].rearrange("p (g e) -> p g e", g=4), [lg.buf])
    ohb = V(rt.oh4.t[:, :].unsqueeze(2).to_broadcast([128, 4, 8]), [rt.oh4.buf])
    S.tt("dve", V(rt.t32.t[:, :].rearrange("p (g e) -> p g e", g=4), [rt.t32.buf]), le, ohb, ALU.mult)
    S.op("dve", lambda e: e.reduce_sum(rt.l8.t[:, :], rt.t32.t[:, :].rearrange("p (g e) -> p e g", g=4), AX.X), [rt.t32[:]], [rt.l8[:]])
    S.op("dve", lambda e: e.reduce_max(rt.s.t[:, 3:4], rt.l8.t[:, :], AX.X), [rt.l8[:]], [rt.s[:]])
    S.ts("dve", rt.oh1[:], rt.l8[:], m1, None, ALU.is_equal)
    S.stt("dve", rt.l8b[:], rt.oh1[:], -1.0e30, rt.l8[:], ALU.mult, ALU.add)
    S.op("dve", lambda e: e.reduce_max(rt.s.t[:, 4:5], rt.l8b.t[:, :], AX.X), [rt.l8b[:]], [rt.s[:]])
    S.ts("dve", rt.oh2[:], rt.l8b[:], m2, None, ALU.is_equal)
    S.tt("dve", e2, m2, m1, ALU.subtract)
    S.act(e2, e2, AF.Exp)
    S.ts("dve", w1, e2, 1.0, None, ALU.add)
    S.op("dve", lambda e: e.reciprocal(rt.s.t[:, 6:7], rt.s.t[:, 6:7]), [rt.s[:]], [rt.s[:]])
    S.tt("dve", w1, w1, pg, ALU.mult)
    S.tt("dve", w2, w1, e2, ALU.mult)
    S.ts("dve", rt.oh1[:], rt.oh1[:], w1, None, ALU.mult)
    S.stt("dve", rt.w8[:], rt.oh2[:], w2, rt.oh1[:], ALU.mult, ALU.add)
    w8b = V(rt.w8.t[:, :].unsqueeze(1).to_broadcast([128, 4, 8]), [rt.w8.buf])
    S.tt("dve", V(rt.WR.t[:, t, :].rearrange("p (g e) -> p g e", g=4), [rt.WR.buf]), ohb, w8b, ALU.mult)


def router_setup(S, g, c, l, WR):
    rt = Ctx()
    rt.WR = WR
    rt.u2Tf = S.sb("u2Tf", [128, KC, 128], F32)
    rt.wr = S.sb("wr", [128, KC, 36], F32)
    S.dma("sp", rt.wr[:], g.wr.v(g.wr.t[l].rearrange("(kc p) n -> p kc n", p=128)))
    rt.brb = S.sb("brb", [128, 36], F32)
    S.dma("sp", rt.brb[:], g.br.v(g.br.t[l].partition_broadcast(128)))
    rt.lg = S.sb("rlg", [128, 36], F32)
    rt.s = S.sb("rs", [128, 16], F32)
    rt.e4 = S.sb("re4", [128, 4], F32)
    rt.oh4 = S.sb("roh4", [128, 4], F32)
    rt.t32 = S.sb("rt32", [128, 32], F32)
    rt.l8 = S.sb("rl8", [128, 8], F32)
    rt.l8b = S.sb("rl8b", [128, 8], F32)
    rt.oh1 = S.sb("roh1", [128, 8], F32)
    rt.oh2 = S.sb("roh2", [128, 8], F32)
    rt.w8 = S.sb("rw8", [128, 8], F32)
    return rt


def phase_moe(S, g, c, l, WR, last=False):
    base = S.arena_off
    wg = S.sb("wg", [128, KC, 1024], BF16)
    wu = S.sb("wu", [128, KC, 1024], BF16)
    wd = S.sb("wd", [128, 8, D], BF16)
    hT = S.sb("hT", [128, 8, T], BF16)
    ut = [S.sb("ut%d" % i, [128, KC, 512], BF16) for i in range(2)]
    sg = [S.sb("sg%d" % i, [128, 512], F32) for i in range(2)]
    ysb = [S.sb("ysb%d" % i, [128, D], F32) for i in range(2)]
    u2v = g.U2T.t.rearrange("(kc p) t -> p kc t", p=128)

    def load_gu(e):
        gv = g.w_eg.t[l, e].rearrange("(kc p) f -> p kc f", p=128)
        uv = g.w_eu.t[l, e].rearrange("(kc p) f -> p kc f", p=128)
        for hf in range(2):
            S.dma("pool", wg[:, :, hf * 512:(hf + 1) * 512], g.w_eg.v(gv[:, :, hf * 512:(hf + 1) * 512]))
            S.dma("pool", wu[:, :, hf * 512:(hf + 1) * 512], g.w_eu.v(uv[:, :, hf * 512:(hf + 1) * 512]))

    def load_d(e):
        dv = g.w_ed.t[l, e].rearrange("(fc p) d -> p fc d", p=128)
        for hf in range(2):
            S.dma("pool", wd[:, hf * 4:(hf + 1) * 4, :], g.w_ed.v(dv[:, hf * 4:(hf + 1) * 4, :]))

    load_gu(0)
    load_d(0)
    ucnt = 0
    pcnt = 0
    ycnt = 0
    tgs = TG[0:4] if not last else [(NCTX + i * 512, 512) for i in range(4)]
    tiles = list(range(NT)) if not last else list(range(2, NT))
    if not last:
        tgs = TG
    for e in range(32):
        for (t0, tn) in tgs:
            u = ut[ucnt % 2]
            ucnt += 1
            S.dma("sp", u[:, :, 0:tn], g.U2T.v(u2v[:, :, t0:t0 + tn]))
            for fc in range(8):
                k = pcnt % 2
                pcnt += 1
                pg = S.bank(2 * k)
                pu = S.bank(2 * k + 1)
                pgv = V(pg.ap[:, 0:tn], pg.bufs)
                puv = V(pu.ap[:, 0:tn], pu.bufs)
                for kc in range(KC):
                    S.mm(pgv, wg[:, kc, fc * 128:(fc + 1) * 128], u[:, kc, 0:tn], start=(kc == 0), stop=(kc == KC - 1))
                for kc in range(KC):
                    S.mm(puv, wu[:, kc, fc * 128:(fc + 1) * 128], u[:, kc, 0:tn], start=(kc == 0), stop=(kc == KC - 1))
                S.act(sg[k][:, 0:tn], pgv, AF.Silu)
                S.tt("dve", hT[:, fc, t0:t0 + tn], puv, sg[k][:, 0:tn], ALU.mult)
        if e + 1 < 32:
            load_gu(e + 1)
        for t in tiles:
            py = S.bank(4, 4)
            for n in range(4):
                for fc in range(8):
                    S.mm(V(py.ap[:, n * 512:(n + 1) * 512], py.bufs[n:n + 1]), hT[:, fc, t * 128:(t + 1) * 128],
                         wd[:, fc, n * 512:(n + 1) * 512], start=(fc == 0), stop=(fc == 7))
            y = ysb[ycnt % 2]
            ycnt += 1
            wsc = WR[:, t, e:e + 1]
            S.act(y[:, 0:1024], V(py.ap[:, 0:1024], py.bufs[0:2]), AF.Copy, scale=wsc)
            S.ts("dve", y[:, 1024:2048], V(py.ap[:, 1024:2048], py.bufs[2:4]), wsc, None, ALU.mult)
            if e == 0:
                S.dma("pool", g.YM.v(g.YM.t[t * 128:(t + 1) * 128, :]), y[:])
            else:
                S.dma("pool", g.YM.v(g.YM.t[t * 128:(t + 1) * 128, :]), y[:], accum_op=ALU.add)
        if e + 1 < 32:
            load_d(e + 1)
    S.barrier()
    S.arena_reset(base)


def phase_final(S, g, c, l, last):
    base = S.arena_off
    g2 = load_mod_tiles(S, g, l, 5, False)
    lg = bcast_tile(S, "ln2g", g.ln2g, g.ln2g.t[l])
    lb = bcast_tile(S, "ln2b", g.ln2b, g.ln2b.t[l])
    hs = [S.sb("fh%d" % i, [128, D], F32) for i in range(2)]
    ys = [S.sb("fy%d" % i, [128, D], F32) for i in range(2)]
    xn = [S.sb("fxn%d" % i, [128, D], F32) for i in range(2)]
    stats = S.sb("fstats", [128, 4, 6], F32)
    mv = S.sb("fmv", [128, 2], F32)
    rstd = S.sb("frstd", [128, 1], F32)
    nmr = S.sb("fnmr", [128, 1], F32)
    toks = []
    tiles = list(range(2, NT)) if last else list(range(NT))
    for t in tiles:
        row = 1 if t < 2 else 0
        h = hs[t % 2]
        y = ys[t % 2]
        x = xn[t % 2]
        S.dma("sp", h[:], g.H.v(g.H.t[t * 128:(t + 1) * 128, :]))
        S.dma("sp", y[:], g.YM.v(g.YM.t[t * 128:(t + 1) * 128, :]))
        S.tt("dve", y[:], y[:], g2[row][:], ALU.mult)
        S.stt("dve", y[:], h[:], float(ALPHA), y[:], ALU.mult, ALU.add)
        layer_norm_tile(S, c, y, x, stats, mv, rstd, nmr)
        S.tt("pool", x[:], x[:], lg[:], ALU.mult)
        S.tt("pool", x[:], x[:], lb[:], ALU.add)
        if last:
            toks.append(S.dma("sp", g.out.v(g.out.t[(t - 2) * 128:(t - 1) * 128, :]), x[:]))
        else:
            S.dma("sp", g.H.v(g.H.t[t * 128:(t + 1) * 128, :]), x[:])
    S.barrier()
    S.arena_reset(base)
    return toks


DBG_EXT = set()


def build_program(stop_after="all", dbg=()):
    global DBG_EXT
    DBG_EXT = set(dbg)
    nc = bass.Bass("TRN2", target_bir_lowering=False)
    S = Sched(nc)
    _orig_dram = S.dram

    def dram(name, shape, dt, kind="Internal"):
        if name in DBG_EXT and kind == "Internal":
            kind = "ExternalOutput"
        return _orig_dram(name, shape, dt, kind=kind)

    S.dram = dram
    stages = ["mod", "lnmod", "inproj", "ssdprep", "ssd", "attn", "merge", "moe", "all"]
    lim = stages.index(stop_after)
    g = declare_io(S, lim)
    declare_ssd_scratch(S, g)
    declare_merge_scratch(S, g)
    g.WRd = S.dram("WRd", [128, NT * 32], F32)
    c = load_consts(S, g)
    final = []
    for l in range(NL):
        phase_mod(S, g, c, l)
        if lim == 0:
            break
        base = S.arena_off
        UT = S.sb("UT", [128, KC, T], BF16)
        phase_ln_mod(S, g, c, l, UT, 0, 1, g.xin if l == 0 else g.H)
        if lim >= 2:
            phase_inproj(S, g, c, l, UT)
        S.arena_reset(base)
        if lim <= 2:
            break
        DTV = S.sb("DTV", [128, NT, 64], F32)
        ADT = S.sb("ADT", [128, NT, 64], F32)
        phase_ssdprep(S, g, c, l, DTV, ADT)
        if lim >= 4:
            phase_ssd_states(S, g, c, l, DTV, ADT)
            phase_ssd_y(S, g, c, l, DTV, ADT)
        S.arena_reset(base)
        if lim <= 4:
            break
        phase_attn(S, g, c, l, last=(l == NL - 1))
        if lim <= 5:
            break
        phase_merge(S, g, c, l)
        phase_outproj(S, g, c, l, g.xin if l == 0 else g.H)
        if lim <= 6:
            break
        WR = S.sb("WR", [128, NT, 32], F32)
        base2 = S.arena_off
        UT2 = S.sb("UT2", [128, KC, T], BF16)
        rt = router_setup(S, g, c, l, WR)
        phase_ln_mod(S, g, c, l, UT2, 3, 4, g.H, router=rt, flush=g.U2T)
        if "WRd" in DBG_EXT:
            S.dma("sp", g.WRd[:], V(WR.t[:, :, :].rearrange("p a b -> p (a b)"), [WR.buf]))
        S.arena_reset(base2)
        phase_moe(S, g, c, l, WR, last=(l == NL - 1))
        toks = phase_final(S, g, c, l, l == NL - 1)
        final.extend(toks)
        S.arena_reset(base)
        if lim <= 7:
            break
    if not final:
        z = S.sb("zout", [128, 64], F32)
        S.memset("dve", z[:], 0.0)
        final.append(S.dma("sp", g.out.v(g.out.t[0:128, 0:64]), z[:]))
    S.barrier()
    S.finish(final)
    return nc


def host_consts():
    nf = 16
    inv = (10000.0 ** (-(np.arange(nf, dtype=np.float32) / nf))).astype(np.float32)
    s = np.arange(2048)
    row = (s // 64).astype(np.float32)
    col = (s % 64).astype(np.float32)
    ang = np.concatenate([row[:, None] * inv[None, :], col[:, None] * inv[None, :]], axis=1).astype(np.float32)
    cos = np.ones((T, 32), np.float32)
    sin = np.zeros((T, 32), np.float32)
    cos[NCTX:] = np.cos(ang)
    sin[NCTX:] = np.sin(ang)
    k = np.arange(128)
    tri = np.stack([(k[:, None] <= k[None, :]), (k[:, None] >= k[None, :])]).astype(np.float32)
    negm = np.stack([np.where(k[None, :] >= k[:, None], 0.0, -30000.0),
                     np.where(k[None, :] <= k[:, None], 0.0, -30000.0)]).astype(np.float32)
    return dict(costab=cos, sintab=sin, identf=np.eye(128, dtype=np.float32), tri=tri, negm=negm)


def prep_core_inputs(inp, b, consts):
    f = np.ascontiguousarray
    m = {}
    m["xin"] = f(np.concatenate([inp["ctx"][b], inp["x"][b]], axis=0))
    cc = np.stack([inp["c"][b], inp["c_ctx"]], axis=1)
    m["cT"] = f(cc.reshape(KC, 128, 2).transpose(1, 0, 2).reshape(128, 32))
    m["w_ada"] = inp["w_ada"]
    m["b_ada"] = inp["b_ada"]
    m["w_in"] = inp["w_in"]
    m["convw"] = f(inp["conv_w"].reshape(NL, 5, 24, 128).transpose(0, 3, 2, 1).reshape(NL, 128, 120))
    m["convb"] = f(inp["conv_b"].reshape(NL, 24, 128).transpose(0, 2, 1))
    m["dtb"] = f(inp["ssm_dt_bias"].reshape(NL, 64))
    m["alog"] = f(inp["ssm_a_log"].reshape(NL, 64))
    m["ssmd"] = inp["ssm_d"]
    m["normg"] = inp["ssm_norm_g"]
    m["lamv"] = f(np.concatenate([inp["lam_q1"], inp["lam_k1"], inp["lam_q2"], inp["lam_k2"]], axis=1))
    m["subg"] = f(inp["attn_subln_g"].reshape(NL, 128, 1))
    for k_ in ("w_br_ssm", "w_br_att", "w_out"):
        m[k_] = inp[k_]
    m["ln1g"], m["ln1b"], m["ln2g"], m["ln2b"] = inp["ln1_g"], inp["ln1_b"], inp["ln2_g"], inp["ln2_b"]
    m["wr"] = f(np.concatenate([inp["w_router_group"], inp["w_router_expert"]], axis=2))
    m["br"] = f(np.concatenate([inp["b_router_group"], inp["b_router_expert"]], axis=1))
    m["w_eg"], m["w_eu"], m["w_ed"] = inp["w_exp_gate"], inp["w_exp_up"], inp["w_exp_down"]
    m.update(consts)
    return m


def kernel(**inputs):
    inp = {k: np.asarray(v) for k, v in inputs.items()}
    nc = build_program(stop_after="all")
    consts = host_consts()
    names = set()
    for a in nc.allocations:
        try:
            if a.kind == "ExternalInput":
                names.add(a.memorylocations[0].name)
        except Exception:
            pass
    in_maps = []
    for b in range(8):
        m = prep_core_inputs(inp, b, consts)
        in_maps.append({k: v for k, v in m.items() if k in names})
    res = run_bass_kernel_spmd(nc, in_maps, core_ids=list(range(8)))
    out = np.stack([np.asarray(res.results[b]["out"]) for b in range(8)], axis=0)
    return out.astype(np.float32)
```
